# Optimizing a Trainium2 kernel written in Bass

```python
import math
import jax, jax.numpy as jnp
from jax import lax
import numpy as np

D_MODEL = 1024
BATCH = 4
SEQ = 4096
DEPTH = 1

N_MLSTM_HEADS = 4
MLSTM_HEAD_DIM = 128
D_MLSTM = N_MLSTM_HEADS * MLSTM_HEAD_DIM
MLSTM_CHUNK = 64
CONV_WIDTH = 3
N_MLA_HEADS = 8
MLA_NOPE_DIM = 64
MLA_ROPE_DIM = 32
MLA_QK_DIM = MLA_NOPE_DIM + MLA_ROPE_DIM
MLA_V_DIM = 64
D_MLA = N_MLA_HEADS * MLA_V_DIM
Q_LORA_RANK = 256
KV_LORA_RANK = 128
ROPE_THETA = 10000.0
Q_BLOCK = 128
D_MIX = D_MLSTM + D_MLA
N_GATE = 4 * N_MLSTM_HEADS
IN_SIZES = [D_MLSTM, D_MLSTM, D_MLSTM, D_MLSTM, N_GATE, Q_LORA_RANK, KV_LORA_RANK, MLA_ROPE_DIM]
IN_OFFSETS = np.cumsum(IN_SIZES)[:-1].tolist()
D_IN = int(sum(IN_SIZES))
N_MEM = 256
N_MEM_HEADS = 4
MEM_HEAD_DIM = 256
D_MEM_INNER = N_MEM_HEADS * MEM_HEAD_DIM
N_EXPERTS = 16
EC_CAPACITY_FACTOR = 2
D_FF_EXPERT = 2048
EPS = 1e-6

kernel_name = "hybrid_mlstm_mla_memxattn_ec_moe"


def rmsnorm(x, g):
    xf = x.astype(jnp.float32)
    y = xf * lax.rsqrt(jnp.mean(xf * xf, axis=-1, keepdims=True) + EPS)
    return (y * g).astype(x.dtype)


def head_rmsnorm(t, g):
    B, S, H, d = t.shape
    tf = t.astype(jnp.float32)
    y = tf * lax.rsqrt(jnp.mean(tf * tf, axis=-1, keepdims=True) + EPS)
    return (y.reshape(B, S, H * d) * g).astype(t.dtype)


def rope_tables(positions):
    inv_freq = ROPE_THETA ** (-jnp.arange(0, MLA_ROPE_DIM, 2, dtype=jnp.float32) / MLA_ROPE_DIM)
    ang = positions.astype(jnp.float32)[..., None] * inv_freq
    return jnp.cos(ang), jnp.sin(ang)


def apply_rope(t, cos, sin):
    tf = t.astype(jnp.float32)
    t1, t2 = jnp.split(tf, 2, axis=-1)
    return jnp.concatenate([t1 * cos - t2 * sin, t2 * cos + t1 * sin], axis=-1).astype(t.dtype)


def centred_conv(u, w):
    S = u.shape[1]
    pad = CONV_WIDTH // 2
    up = jnp.pad(u, ((0, 0), (pad, pad), (0, 0)))
    out = up[:, 0:S] * w[0]
    for j in range(1, CONV_WIDTH):
        out = out + up[:, j:j + S] * w[j]
    return out


def mlstm_chunkwise(q, k, v, log_i, log_f):
    B, H, S, d = q.shape
    T = MLSTM_CHUNK
    NC = S // T
    chunk = lambda t: jnp.moveaxis(t.reshape(B, H, NC, T, *t.shape[3:]), 2, 0)
    mask = jnp.tril(jnp.ones((T, T), dtype=bool))

    def step(carry, inp):
        C, n, m = carry
        qc, kc, vc, li, lf = inp
        b = jnp.cumsum(lf, axis=-1)
        g = b[..., -1]
        Dm = b[..., :, None] - b[..., None, :] + li[..., None, :]
        Dm = jnp.where(mask, Dm, -jnp.inf)
        inter = b + m[..., None]
        m_t = jnp.maximum(inter, jnp.max(Dm, axis=-1))
        Pw = jnp.exp(Dm - m_t[..., None]) * jnp.einsum('bhtd,bhsd->bhts', qc, kc)
        inter_w = jnp.exp(inter - m_t)
        num = inter_w[..., None] * jnp.einsum('bhtd,bhde->bhte', qc, C) + jnp.einsum('bhts,bhse->bhte', Pw, vc)
        den = inter_w * jnp.einsum('bhtd,bhd->bht', qc, n) + jnp.sum(Pw, axis=-1)
        h = num / jnp.maximum(jnp.abs(den), jnp.exp(-m_t))[..., None]
        w_s = g[..., None] - b + li
        m_new = jnp.maximum(g + m, jnp.max(w_s, axis=-1))
        decay = jnp.exp(g + m - m_new)
        ws = jnp.exp(w_s - m_new[..., None])
        C_new = decay[..., None, None] * C + jnp.einsum('bhs,bhsd,bhse->bhde', ws, kc, vc)
        n_new = decay[..., None] * n + jnp.einsum('bhs,bhsd->bhd', ws, kc)
        return (C_new, n_new, m_new), h

    init = (jnp.zeros((B, H, d, d), jnp.float32), jnp.zeros((B, H, d), jnp.float32), jnp.zeros((B, H), jnp.float32))
    _, hs = lax.scan(step, init, (chunk(q), chunk(k), chunk(v), chunk(log_i), chunk(log_f)))
    return jnp.moveaxis(hs, 0, 2).reshape(B, H, S, d)


def mla_attention(qn, qr, kn, kr, v):
    B, S, H, _ = qn.shape
    nb = S // Q_BLOCK
    scale = MLA_QK_DIM ** -0.5
    blk = lambda t: t.reshape(B, nb, Q_BLOCK, *t.shape[2:]).swapaxes(0, 1)

    def one(args):
        qn_b, qr_b = args
        s = jnp.einsum('bqhd,bkhd->bhqk', qn_b, kn) + jnp.einsum('bqhr,bkr->bhqk', qr_b, kr)
        p = jax.nn.softmax(s.astype(jnp.float32) * scale, axis=-1).astype(v.dtype)
        return jnp.einsum('bhqk,bkhd->bqhd', p, v)

    out = lax.map(one, (blk(qn), blk(qr)))
    return out.swapaxes(0, 1).reshape(B, S, H, MLA_V_DIM)


def hybrid_mixer(h, cos, sin, w_in, b_gates, conv_qk, g_q_a, w_q_b, g_kv_a, w_kv_b,
                 g_head_mlstm, g_head_mla, w_out):
    B, S, _ = h.shape
    proj = h @ w_in
    q_m, k_m, v_m, o_m, gates, c_q, c_kv, k_rope = jnp.split(proj, IN_OFFSETS, axis=-1)

    qk = jax.nn.silu(centred_conv(jnp.concatenate([q_m, k_m], axis=-1), conv_qk))
    q_m, k_m = jnp.split(qk, 2, axis=-1)
    to_heads = lambda t: t.reshape(B, S, N_MLSTM_HEADS, MLSTM_HEAD_DIM).transpose(0, 2, 1, 3).astype(jnp.float32)
    qh = to_heads(q_m)
    kh = to_heads(k_m) * (MLSTM_HEAD_DIM ** -0.5)
    vh = to_heads(v_m)
    gts = (gates + b_gates).astype(jnp.float32).reshape(B, S, 4, N_MLSTM_HEADS).transpose(2, 0, 3, 1)
    i_f, f_f, i_b, f_b = gts[0], gts[1], gts[2], gts[3]
    h_fwd = mlstm_chunkwise(qh, kh, vh, i_f, jax.nn.log_sigmoid(f_f))
    flip = lambda t: jnp.flip(t, axis=2)
    h_bwd = flip(mlstm_chunkwise(flip(qh), flip(kh), flip(vh), flip(i_b), flip(jax.nn.log_sigmoid(f_b))))
    h_m = (h_fwd + h_bwd).transpose(0, 2, 1, 3).astype(h.dtype)
    y_m = jax.nn.sigmoid(o_m) * head_rmsnorm(h_m, g_head_mlstm)

    q = (rmsnorm(c_q, g_q_a) @ w_q_b).reshape(B, S, N_MLA_HEADS, MLA_QK_DIM)
    qn, qr = q[..., :MLA_NOPE_DIM], q[..., MLA_NOPE_DIM:]
    qr = apply_rope(qr, cos[:, :, None], sin[:, :, None])
    kv = (rmsnorm(c_kv, g_kv_a) @ w_kv_b).reshape(B, S, N_MLA_HEADS, MLA_NOPE_DIM + MLA_V_DIM)
    kn, v = kv[..., :MLA_NOPE_DIM], kv[..., MLA_NOPE_DIM:]
    kr = apply_rope(k_rope, cos, sin)
    y_a = head_rmsnorm(mla_attention(qn, qr, kn, kr, v), g_head_mla)

    return jnp.concatenate([y_m, y_a], axis=-1) @ w_out


def memory_cross_attention(h, m, w_q, w_k, w_v, w_o):
    B, S, _ = h.shape
    M = m.shape[1]
    q = (h @ w_q).reshape(B, S, N_MEM_HEADS, MEM_HEAD_DIM)
    k = (m @ w_k).reshape(B, M, N_MEM_HEADS, MEM_HEAD_DIM)
    v = (m @ w_v).reshape(B, M, N_MEM_HEADS, MEM_HEAD_DIM)
    s = jnp.einsum('bqhd,bkhd->bhqk', q, k).astype(jnp.float32) * (MEM_HEAD_DIM ** -0.5)
    p = jax.nn.softmax(s, axis=-1).astype(v.dtype)
    o = jnp.einsum('bhqk,bkhd->bqhd', p, v).reshape(B, S, D_MEM_INNER)
    return o @ w_o


def expert_choice_ffn(h, w_router, w_gate, w_up, w_down):
    B, S, D = h.shape
    cap = EC_CAPACITY_FACTOR * S // N_EXPERTS
    aff = jax.nn.softmax((h @ w_router).astype(jnp.float32), axis=-1)
    gate_vals, idx = lax.top_k(aff.transpose(0, 2, 1), cap)
    xe = jax.vmap(lambda t, i: t[i])(h, idx)
    hid = jax.nn.silu(jnp.einsum('becd,edf->becf', xe, w_gate)) * jnp.einsum('becd,edf->becf', xe, w_up)
    ye = jnp.einsum('becf,efd->becd', hid, w_down) * gate_vals[..., None].astype(h.dtype)
    return jax.vmap(lambda y, i: jnp.zeros((S, D), y.dtype).at[i.reshape(-1)].add(y.reshape(-1, D)))(ye, idx)


def setup_inputs(seed: int = 0) -> dict:
    key = jax.random.key(seed)
    ks = jax.random.split(key, 32)
    f32 = jnp.float32

    def w(k, shape, fan_in):
        return jax.random.normal(k, (DEPTH,) + shape, f32) * (fan_in ** -0.5)

    def gain(k, n, depth=True):
        shape = (DEPTH, n) if depth else (n,)
        return 1.0 + 0.05 * jax.random.normal(k, shape, f32)

    x = jax.random.normal(ks[0], (BATCH, SEQ, D_MODEL), f32)
    mem = jax.random.normal(ks[1], (BATCH, N_MEM, D_MODEL), f32)
    offsets = jax.random.randint(ks[2], (BATCH, 1), 0, 1024, dtype=jnp.int32)
    positions = offsets + jnp.arange(SEQ, dtype=jnp.int32)[None, :]
    forget_bias = jnp.linspace(3.0, 6.0, N_MLSTM_HEADS, dtype=f32)
    zeros_h = jnp.zeros((N_MLSTM_HEADS,), f32)
    gate_base = jnp.concatenate([zeros_h, forget_bias, zeros_h, forget_bias])
    b_gates = gate_base[None] + 0.1 * jax.random.normal(ks[3], (DEPTH, N_GATE), f32)
    return {
        "x": x,
        "mem": mem,
        "positions": positions,
        "g_mix": gain(ks[4], D_MODEL),
        "w_in": w(ks[5], (D_MODEL, D_IN), D_MODEL),
        "b_gates": b_gates,
        "conv_qk": w(ks[6], (CONV_WIDTH, 2 * D_MLSTM), CONV_WIDTH),
        "g_q_a": gain(ks[7], Q_LORA_RANK),
        "w_q_b": w(ks[8], (Q_LORA_RANK, N_MLA_HEADS * MLA_QK_DIM), Q_LORA_RANK),
        "g_kv_a": gain(ks[9], KV_LORA_RANK),
        "w_kv_b": w(ks[10], (KV_LORA_RANK, N_MLA_HEADS * (MLA_NOPE_DIM + MLA_V_DIM)), KV_LORA_RANK),
        "g_head_mlstm": gain(ks[11], D_MLSTM),
        "g_head_mla": gain(ks[12], D_MLA),
        "w_out": w(ks[13], (D_MIX, D_MODEL), D_MIX),
        "g_mem_x": gain(ks[14], D_MODEL),
        "g_mem_kv": gain(ks[15], D_MODEL),
        "w_mem_q": w(ks[16], (D_MODEL, D_MEM_INNER), D_MODEL),
        "w_mem_k": w(ks[17], (D_MODEL, D_MEM_INNER), D_MODEL),
        "w_mem_v": w(ks[18], (D_MODEL, D_MEM_INNER), D_MODEL),
        "w_mem_o": w(ks[19], (D_MEM_INNER, D_MODEL), D_MEM_INNER),
        "g_ffn": gain(ks[20], D_MODEL),
        "w_router": w(ks[21], (D_MODEL, N_EXPERTS), D_MODEL),
        "w_exp_gate": w(ks[22], (N_EXPERTS, D_MODEL, D_FF_EXPERT), D_MODEL),
        "w_exp_up": w(ks[23], (N_EXPERTS, D_MODEL, D_FF_EXPERT), D_MODEL),
        "w_exp_down": w(ks[24], (N_EXPERTS, D_FF_EXPERT, D_MODEL), D_FF_EXPERT),
        "g_final": gain(ks[25], D_MODEL, depth=False),
    }


def reference(x, mem, positions, g_mix, w_in, b_gates, conv_qk, g_q_a, w_q_b, g_kv_a, w_kv_b,
              g_head_mlstm, g_head_mla, w_out, g_mem_x, g_mem_kv, w_mem_q, w_mem_k, w_mem_v, w_mem_o,
              g_ffn, w_router, w_exp_gate, w_exp_up, w_exp_down, g_final):
    cos, sin = rope_tables(positions)
    for l in range(DEPTH):
        x = x + hybrid_mixer(rmsnorm(x, g_mix[l]), cos, sin, w_in[l], b_gates[l], conv_qk[l],
                             g_q_a[l], w_q_b[l], g_kv_a[l], w_kv_b[l],
                             g_head_mlstm[l], g_head_mla[l], w_out[l])
        x = x + memory_cross_attention(rmsnorm(x, g_mem_x[l]), rmsnorm(mem, g_mem_kv[l]),
                                       w_mem_q[l], w_mem_k[l], w_mem_v[l], w_mem_o[l])
        x = x + expert_choice_ffn(rmsnorm(x, g_ffn[l]), w_router[l], w_exp_gate[l], w_exp_up[l], w_exp_down[l])
    return rmsnorm(x, g_final)
```

```python
from contextlib import ExitStack
import numpy as np
import ml_dtypes
import concourse.bass as bass
import concourse.mybir as mybir
from concourse.bass_utils import run_bass_kernel_spmd

F32 = mybir.dt.float32
BF16 = mybir.dt.bfloat16
I32 = mybir.dt.int32
AF = mybir.ActivationFunctionType
ALU = mybir.AluOpType
AX = mybir.AxisListType

SEM_CH = 1000
NDMA_SEM = 12
ARENA_WORDS = 53000
EPS = 1e-6


class Op:
    __slots__ = ("eng", "fn", "deps", "signal", "semval", "idx", "dma", "dsem", "dval", "phase")


class Prog:
    ENGS = ("pe", "act", "dve", "pool", "sp")

    def __init__(self, nc):
        self.nc = nc
        self.streams = {e: [] for e in self.ENGS}
        self.last_w = {}
        self.readers = {}
        self.ndma = {"sp": 0, "pool": 0}
        self.dma_ops = {"sp": [], "pool": []}
        self.seen = {e: {p: -1 for p in self.ENGS} for e in self.ENGS}
        self.seen_dma = {e: {} for e in self.ENGS}
        self.stack = ExitStack()
        self.n_ops = 0
        self.arena = self.stack.enter_context(nc.sbuf_tensor("arena", [128, ARENA_WORDS], F32))
        self.top = 0
        self.marks = []
        self.peak = 0
        self.phase = "p0"
        self.scopes = False

    def alloc(self, shape, dtype):
        esz = 2 if dtype == BF16 else 4
        n = 1
        for s in shape[1:]:
            n *= s
        words = (n * esz + 3) // 4
        off = self.top
        self.top += (words + 7) // 8 * 8
        self.peak = max(self.peak, self.top)
        assert self.top <= ARENA_WORDS, f"SBUF arena overflow {self.top}"
        ap = self.arena[0:shape[0], off:off + words]
        if dtype != F32:
            ap = ap.bitcast(dtype)
        if ap.shape[1] != n:
            ap = ap[:, 0:n]
        if len(shape) == 3:
            ap = ap.rearrange("p (a b) -> p a b", a=shape[1])
        elif len(shape) == 4:
            ap = ap.rearrange("p (a b c) -> p a b c", a=shape[1], b=shape[2])
        return ap

    def mark(self):
        self.marks.append(self.top)

    def release(self):
        self.barrier()
        self.top = self.marks.pop()

    def psum(self, name, shape, dtype=F32):
        return self.stack.enter_context(self.nc.psum_tensor(name, list(shape), dtype))

    def op(self, eng, fn, reads=(), writes=(), dma=False, extra=()):
        xr = [k for k in reads if isinstance(k, tuple) and k[0] == "B"]
        if xr:
            writes = list(writes) + [k for k in xr if k not in writes]
        o = Op()
        o.eng, o.fn, o.dma, o.signal, o.semval = eng, fn, dma, False, 0
        o.dsem = o.dval = None
        o.phase = self.phase
        stream = self.streams[eng]
        o.idx = len(stream)
        deps = {}
        cand = list(extra)
        for k in reads:
            w = self.last_w.get(k)
            if w is not None:
                cand.append(w)
        for k in writes:
            w = self.last_w.get(k)
            if w is not None:
                cand.append(w)
            cand.extend(self.readers.get(k, ()))
        if dma:
            q = eng
            n = self.ndma[q]
            self.ndma[q] = n + 1
            o.dsem = n % NDMA_SEM
            o.dval = 16 * (n // NDMA_SEM + 1)
            if n >= NDMA_SEM:
                cand.append(self.dma_ops[q][n - NDMA_SEM])
            self.dma_ops[q].append(o)
        for d in cand:
            if d is o or d.fn is None:
                continue
            if d.dma:
                key = (d.eng, d.dsem)
                if self.seen_dma[eng].get(key, 0) >= d.dval:
                    continue
                self.seen_dma[eng][key] = d.dval
                deps[("dma",) + key] = d
            else:
                if d.eng == "pe" and eng == "pe" and not dma:
                    continue
                if self.seen[eng][d.eng] >= d.idx:
                    continue
                cur = deps.get(("c", d.eng))
                if cur is None or cur.idx < d.idx:
                    deps[("c", d.eng)] = d
        for k, d in deps.items():
            if k[0] == "c":
                self.seen[eng][d.eng] = d.idx
                d.signal = True
        o.deps = list(deps.values())
        stream.append(o)
        self.n_ops += 1
        for k in reads:
            lst = self.readers.setdefault(k, [])
            if not dma:
                lst[:] = [r for r in lst if r.dma or r.eng != eng]
            lst.append(o)
        for k in writes:
            self.last_w[k] = o
            self.readers[k] = []
        return o

    def pe(self, fn, reads=(), writes=()):
        return self.op("pe", fn, reads, writes)

    def act(self, fn, reads=(), writes=()):
        return self.op("act", fn, reads, writes)

    def dve(self, fn, reads=(), writes=()):
        return self.op("dve", fn, reads, writes)

    def pool(self, fn, reads=(), writes=()):
        return self.op("pool", fn, reads, writes)

    def dma(self, out, in_, reads=(), writes=(), q="sp", **kw):
        return self.op(q, lambda e: e.dma_start(out=out, in_=in_, **kw), reads, writes, dma=True)

    def barrier(self):
        allops = set()
        for w in self.last_w.values():
            allops.add(w)
        for lst in self.readers.values():
            allops.update(lst)
        for q in self.dma_ops:
            allops.update(self.dma_ops[q][-NDMA_SEM:])
        allops = [o for o in allops if o.fn is not None]
        for e in self.ENGS:
            self.op(e, None, extra=allops)
        self.last_w.clear()
        self.readers.clear()

    def emit(self):
        nc = self.nc
        st = self.stack
        csem = {}
        for e in ("pe", "act", "dve", "pool"):
            cnt = 0
            for o in self.streams[e]:
                if o.signal and not o.dma:
                    cnt += 1
                    o.semval = cnt
            nsem = max(1, (cnt + SEM_CH - 1) // SEM_CH)
            csem[e] = [st.enter_context(nc.semaphore(f"s_{e}{i}")) for i in range(nsem)]
        dsem = {}
        for q in ("sp", "pool"):
            dsem[q] = [st.enter_context(nc.semaphore(f"d_{q}{i}")) for i in range(NDMA_SEM)]

        def emit_stream(ename, eng):
            if self.scopes:
                cur = None
                cm = None
                for o in self.streams[ename]:
                    if o.phase != cur:
                        if cm is not None:
                            cm.__exit__(None, None, None)
                        cur = o.phase
                        cm = nc.named_scope(cur)
                        cm.__enter__()
                    emit_one(ename, eng, o)
                if cm is not None:
                    cm.__exit__(None, None, None)
                return
            for o in self.streams[ename]:
                emit_one(ename, eng, o)

        def emit_one(ename, eng, o):
            if True:
                for d in o.deps:
                    if d.dma:
                        eng.wait_ge(dsem[d.eng][d.dsem], d.dval)
                    else:
                        v = d.semval - 1
                        eng.wait_ge(csem[d.eng][v // SEM_CH], v % SEM_CH + 1)
                if o.fn is None:
                    return
                ins = o.fn(eng)
                if o.dma:
                    ins.then_inc(dsem[ename][o.dsem], 16)
                elif o.signal:
                    v = o.semval - 1
                    ins.then_inc(csem[ename][v // SEM_CH], 1)

        with nc.Block() as block:
            @block.tensor
            def _(eng):
                emit_stream("pe", eng)

            @block.scalar
            def _(eng):
                emit_stream("act", eng)

            @block.vector
            def _(eng):
                emit_stream("dve", eng)

            @block.gpsimd
            def _(eng):
                emit_stream("pool", eng)

            @block.sync
            def _(eng):
                emit_stream("sp", eng)


LANE_PART = [0, 1, 2, 3, 32, 33, 34, 35]
NHEADS_MLA = 8
PV_NOACC = False
OB_BASE = 6
SCOPES = False


def build(stop=99, taps=False):
    nc = bass.Bass("TRN2", target_bir_lowering=False)
    P = Prog(nc)
    P.scopes = SCOPES

    def din(n, shp, dt=F32):
        return nc.dram_tensor(n, list(shp), dt, kind="ExternalInput")

    def dscr(n, shp, dt):
        return nc.dram_tensor(n, list(shp), dt, kind="Internal")

    x = din("x", [4096, 1024]); mem = din("mem", [256, 1024]); pos = din("pos", [32, 4096], I32)
    c_identb = din("c_identb", [128, 128], BF16); c_identf = din("c_identf", [128, 128])
    c_onesb = din("c_onesb", [128, 128], BF16)
    c_bmask = din("c_bmask", [128, 2, 128]); c_sel36 = din("c_sel36", [36, 8, 128])
    c_nsel = din("c_nsel", [128, 64]); c_invf = din("c_invf", [128, 1])
    c_reset = din("c_reset", [36, 4096])
    c_iota = din("c_iota", [128, 512]); c_tokid = din("c_tokid", [128, 32]); c_U = din("c_U", [128, 128])
    c_onesf = din("c_onesf", [128, 128])
    gmix_d = din("gmix", [128, 8])
    wq_d = din("w_q", [1024, 512]); wk_d = din("w_k", [1024, 512]); wv_d = din("w_v", [1024, 512]); wo_d = din("w_o", [1024, 512])
    wgI_d = din("w_gI", [1024, 36]); wgF_d = din("w_gF", [1024, 36]); bI_d = din("b_I", [36, 1]); bF_d = din("b_F", [36, 1])
    wcq_d = din("w_cq", [1024, 256]); wckv_d = din("w_ckv", [1024, 128])
    wkr_d = din("w_kr", [1024, 96]); wkrr_d = din("w_krr", [1024, 96])
    convq_d = din("convq", [128, 4, 3]); convk_d = din("convk", [128, 4, 3])
    gqa_d = din("g_qa", [128, 2]); gkva_d = din("g_kva", [128, 1])
    wqb_d = din("w_qb", [256, 768]); wqbr_d = din("w_qbr", [256, 768])
    wkn_d = din("w_kn", [128, 512]); wv2_d = din("w_v2", [128, 512])
    ghm_d = din("g_hm", [128, 4]); gha_d = din("g_ha", [64, 8])
    woutm_d = din("w_outm", [512, 1024]); wouta_d = din("w_outa", [8, 64, 1024])
    gmx_d = din("g_mx", [128, 8]); gmkv_d = din("g_mkv", [128, 8])
    wmq_d = din("w_mq", [1024, 1024]); wmk_d = din("w_mk", [1024, 1024]); wmv_d = din("w_mv", [1024, 1024]); wmo_d = din("w_mo", [1024, 1024])
    gffn_d = din("g_ffn", [1, 1024]); gfin_d = din("g_fin", [1, 1024]); gffnp_d = din("g_ffnp", [128, 8]); c_jp = din("c_jp", [128, 32, 2])
    wr_d = din("w_r", [1024, 16])
    if stop > 4:
        weg_d = din("w_eg", [16, 1024, 2048]); weu_d = din("w_eu", [16, 1024, 2048]); wed_d = din("w_ed", [16, 2048, 1024])
    else:
        weg_d = din("w_eg", [1, 1, 1]); weu_d = din("w_eu", [1, 1, 1]); wed_d = din("w_ed", [1, 1, 1])
    out = nc.dram_tensor("out", [4096, 1024], F32, kind="ExternalOutput")
    ymT_d = dscr("ymT_d", [512, 4096], BF16)
    yaT_d = dscr("yaT_d", [8, 64, 4096], BF16)
    acc_d = dscr("acc_d", [4096, 1024], F32)
    h_d = dscr("h_d", [4096, 1024], BF16)
    tapd = {}

    def tap(name, ap_sb, shape, dt, reads):
        if not taps:
            return
        t = nc.dram_tensor("tap_" + name, list(shape), dt, kind="ExternalOutput")
        tapd[name] = t
        P.dma(t.ap(), ap_sb, reads=reads, writes=[("tap", name)])

    def mm(o, lhsT, rhs, st, sp, R, W):
        P.pe(lambda e: e.matmul(o, lhsT=lhsT, rhs=rhs, start=st, stop=sp), R, W)

    def tr(o, i, idn, R, W):
        P.pe(lambda e: e.transpose(out=o, in_=i, identity=idn), R, W)

    def actf(o, i, func, R, W, bias=None, scale=None, accum=None):
        kw = {}
        if bias is not None:
            kw["bias"] = bias
        if scale is not None:
            kw["scale"] = scale
        if accum is not None:
            kw["accum_out"] = accum
        P.act(lambda e: e.activation(out=o, in_=i, func=func, **kw), R, W)

    def ts(eng, o, i0, s1, s2, op0, op1, R, W):
        if op1 is None:
            P.op(eng, lambda e: e.tensor_scalar(out=o, in0=i0, scalar1=s1, scalar2=None, op0=op0), R, W)
        else:
            P.op(eng, lambda e: e.tensor_scalar(out=o, in0=i0, scalar1=s1, scalar2=s2, op0=op0, op1=op1), R, W)

    def tt(eng, o, i0, i1, op, R, W):
        P.op(eng, lambda e: e.tensor_tensor(out=o, in0=i0, in1=i1, op=op), R, W)

    def stt(eng, o, i0, sc, i1, op0, op1, R, W):
        P.op(eng, lambda e: e.scalar_tensor_tensor(out=o, in0=i0, scalar=sc, in1=i1, op0=op0, op1=op1), R, W)

    def cp(eng, o, i, R, W):
        P.op(eng, lambda e: e.tensor_copy(out=o, in_=i), R, W)

    def recip(o, i, R, W):
        P.dve(lambda e: e.reciprocal(out=o, in_=i), R, W)

    def memset(eng, o, v, W):
        P.op(eng, lambda e: e.memset(o, v), (), W)

    def load(dst, src, key, q="sp"):
        P.dma(dst, src, writes=[key], q=q)

    TP = [P.psum(f"pp{i}", [128, 1024], F32) for i in range(4)]
    B = []
    for t_ in TP:
        B += [t_[:, 0:512], t_[:, 512:1024]]
    Bb = [b.bitcast(BF16) for b in B]
    BK = [("B", i) for i in range(8)]

    identb = P.alloc([128, 128], BF16); load(identb, c_identb.ap(), "identb")
    identf = P.alloc([128, 128], F32); load(identf, c_identf.ap(), "identf")
    onesb = P.alloc([128, 128], BF16); load(onesb, c_onesb.ap(), "onesb")

    def rms_rstd(ssum, rs, n, key):
        actf(rs, ssum, AF.Sqrt, [key + "ss"], [key + "rs"], bias=EPS, scale=1.0 / n)
        recip(rs, rs, [key + "rs"], [key + "rs"])

    xnT = P.alloc([128, 8, 4096], BF16)
    P.mark()
    gmix = P.alloc([128, 8], F32); load(gmix, gmix_d.ap(), "gmix")
    xs = [P.alloc([128, 4, 1024], F32) for _ in range(2)]
    xnb = [P.alloc([128, 4, 1024], BF16) for _ in range(2)]
    junk = P.alloc([128, 1024], BF16)
    ss = P.alloc([128, 32], F32); rs = P.alloc([128, 32], F32)
    for s in range(8):
        b = s % 2
        P.dma(xs[b], x[s * 512:(s + 1) * 512, :].rearrange("(j p) d -> p j d", p=128), writes=[("xs", b)])
        for j in range(4):
            t = s * 4 + j
            actf(junk, xs[b][:, j, :], AF.Square, [("xs", b)], ["junk", ("ss", t)], accum=ss[:, t:t + 1])
            actf(rs[:, t:t + 1], ss[:, t:t + 1], AF.Sqrt, [("ss", t)], [("rs", t)], bias=EPS, scale=1.0 / 1024)
            recip(rs[:, t:t + 1], rs[:, t:t + 1], [("rs", t)], [("rs", t)])
            ts("dve", xnb[b][:, j, :], xs[b][:, j, :], rs[:, t:t + 1], None, ALU.mult, None, [("xs", b), ("rs", t)], [("xnb", b, j)])
        for kc in range(8):
            bk = 6 + kc % 2
            for j in range(4):
                tr(Bb[bk][:, j * 128:(j + 1) * 128], xnb[b][:, j, kc * 128:(kc + 1) * 128], identb, [("xnb", b, j), "identb"], [BK[bk]])
            if kc % 2 == 0:
                ts("dve", xnT[:, kc, s * 512:(s + 1) * 512], Bb[bk][:, 0:512], gmix[:, kc:kc + 1], None, ALU.mult, None, [BK[bk], "gmix"], [("xnT", s)])
            else:
                actf(xnT[:, kc, s * 512:(s + 1) * 512], Bb[bk][:, 0:512], AF.Copy, [BK[bk], "gmix"], [("xnT", s)], scale=gmix[:, kc:kc + 1])
    XNT = [("xnT", s) for s in range(8)]
    tap("xnT", xnT, [128, 8, 4096], BF16, XNT)
    P.release()
    if stop <= 0:
        return finish(nc, P, out, tapd)

    P.phase = "p1a"
    P.mark()
    WS128 = P.alloc([128, 32, 36], F32)
    LB64 = P.alloc([64, 64, 36], F32)
    DEC = P.alloc([128, 8, 64], F32)
    P.mark()
    wgI = P.alloc([128, 8, 36], BF16); load(wgI, wgI_d.ap().rearrange("(k p) n -> p k n", p=128), "wgI", q="pool")
    wgF = P.alloc([128, 8, 36], BF16); load(wgF, wgF_d.ap().rearrange("(k p) n -> p k n", p=128), "wgF", q="pool")
    bI = P.alloc([36, 1], F32); load(bI, bI_d.ap(), "bI")
    nbF = P.alloc([36, 1], F32); load(nbF, bF_d.ap(), "nbF")
    ts("dve", nbF, nbF, -1.0, None, ALU.mult, None, ["nbF"], ["nbF"])
    reset = P.alloc([36, 4096], F32); load(reset, c_reset.ap(), "reset")
    sel36 = P.alloc([36, 8, 128], F32); load(sel36, c_sel36.ap(), "sel36")
    IT = P.alloc([36, 4096], F32); SP = P.alloc([36, 4096], F32); NB = P.alloc([36, 4096], F32)
    Gt = P.alloc([36, 64], F32); Gn = P.alloc([36, 64], F32); amax = P.alloc([36, 64], F32)
    Mm = P.alloc([36, 64], F32); Mend = P.alloc([36, 64], F32); MP = P.alloc([36, 64], F32); DECs = P.alloc([36, 64], F32)
    for s in range(8):
        sl = slice(s * 512, (s + 1) * 512)
        for kc in range(8):
            mm(B[0][0:36, :], wgI[:, kc, :], xnT[:, kc, sl], kc == 0, kc == 7, ["wgI"] + XNT, [BK[0]])
        actf(IT[:, sl], B[0][0:36, :], AF.Identity, [BK[0], "bI"], ["IT"], bias=bI[:, 0:1])
        for kc in range(8):
            mm(B[1][0:36, :], wgF[:, kc, :], xnT[:, kc, sl], kc == 0, kc == 7, ["wgF"] + XNT, [BK[1]])
        actf(SP[:, sl], B[1][0:36, :], AF.Exp, [BK[1], "nbF"], ["SP"], bias=nbF[:, 0:1], scale=-1.0)
    actf(SP, SP, AF.Ln, ["SP"], ["SP"], bias=1.0)
    P.dve(lambda e: e.tensor_tensor_scan(out=NB, data0=reset, data1=SP, initial=0.0, op0=ALU.mult, op1=ALU.add), ["reset", "SP"], ["NB"])
    NB3 = NB.rearrange("p (c t) -> p c t", t=64); IT3 = IT.rearrange("p (c t) -> p c t", t=64); SP3 = SP.rearrange("p (c t) -> p c t", t=64)
    cp("dve", Gt, NB3[:, :, 63], ["NB"], ["Gt"])
    Gt3 = Gt.rearrange("p (c o) -> p c o", o=1)
    tt("dve", NB3[32:36], Gt3[32:36].to_broadcast([4, 64, 64]), NB3[32:36], ALU.subtract, ["Gt", "NB"], ["NB"])
    tt("dve", NB3[32:36], NB3[32:36], SP3[32:36], ALU.add, ["NB", "SP"], ["NB"])
    tt("dve", IT, IT, NB, ALU.add, ["IT", "NB"], ["IT"])
    P.dve(lambda e: e.tensor_reduce(out=amax, in_=IT3, axis=AX.X, op=ALU.max), ["IT"], ["amax"])
    ts("dve", Gn, Gt, -1.0, None, ALU.mult, None, ["Gt"], ["Gn"])
    P.dve(lambda e: e.tensor_tensor_scan(out=Mm[0:4, :], data0=amax[0:4, :], data1=Gn[0:4, :], initial=0.0, op0=ALU.max, op1=ALU.add), ["amax", "Gn"], ["Mm"])
    P.dve(lambda e: e.tensor_tensor_scan(out=Mm[32:36, ::-1], data0=amax[32:36, ::-1], data1=Gn[32:36, ::-1], initial=0.0, op0=ALU.max, op1=ALU.add), ["amax", "Gn"], ["Mm"])
    tt("dve", Mend, Mm, Gt, ALU.add, ["Mm", "Gt"], ["Mend"])
    memset("dve", MP, 0.0, ["MP"])
    cp("dve", MP[0:4, 1:64], Mm[0:4, 0:63], ["Mm", "MP"], ["MP"])
    cp("dve", MP[32:36, 0:63], Mm[32:36, 1:64], ["Mm", "MP"], ["MP"])
    tt("dve", DECs, MP, Mend, ALU.subtract, ["MP", "Mend"], ["DECs"])
    actf(DECs, DECs, AF.Exp, ["DECs"], ["DECs"])
    Mend3 = Mend.rearrange("p (c o) -> p c o", o=1).to_broadcast([36, 64, 64])
    tt("dve", IT3, IT3, Mend3, ALU.subtract, ["IT", "Mend"], ["IT"])
    actf(IT, IT, AF.Exp, ["IT"], ["IT"])
    tt("dve", NB3, NB3, Mend3, ALU.subtract, ["NB", "Mend"], ["NB"])
    actf(NB, NB, AF.Exp, ["NB"], ["NB"])
    for g8 in range(4):
        bk = g8 % 2
        for jj in range(8):
            j = g8 * 8 + jj
            tr(B[bk][:, jj * 36:(jj + 1) * 36], IT[0:36, j * 128:(j + 1) * 128], identf[0:36, 0:36], ["IT", "identf"], [BK[bk]])
        cp("dve", WS128[:, g8 * 8:(g8 + 1) * 8, :], B[bk][:, 0:288].rearrange("p (a b) -> p a b", a=8), [BK[bk]], ["WS128"])
    for g8 in range(8):
        bk = 2 + g8 % 2
        for cc in range(8):
            c = g8 * 8 + cc
            tr(B[bk][0:64, cc * 36:(cc + 1) * 36], NB[0:36, c * 64:(c + 1) * 64], identf[0:36, 0:36], ["NB", "identf"], [BK[bk]])
        cp("dve", LB64[:, g8 * 8:(g8 + 1) * 8, :], B[bk][0:64, 0:288].rearrange("p (a b) -> p a b", a=8), [BK[bk]], ["LB64"])
    for l in range(8):
        mm(B[4][:, l * 64:(l + 1) * 64], sel36[:, l, :], DECs, True, True, ["sel36", "DECs"], [BK[4]])
    cp("dve", DEC, B[4][:, 0:512].rearrange("p (a b) -> p a b", a=8), [BK[4]], ["DEC"])
    tap("WS128", WS128, [128, 32, 36], F32, ["WS128"])
    tap("LB64", LB64, [64, 64, 36], F32, ["LB64"])
    tap("DEC", DEC, [128, 8, 64], F32, ["DEC"])
    P.release()
    if stop <= 1:
        return finish(nc, P, out, tapd)

    P.phase = "p1b"
    P.mark()
    wq = P.alloc([128, 8, 128], BF16); wk = P.alloc([128, 8, 128], BF16)
    wv = P.alloc([128, 8, 128], BF16); wo = P.alloc([128, 8, 128], BF16)
    convq = P.alloc([128, 4, 3], F32); load(convq, convq_d.ap(), "convq")
    convk = P.alloc([128, 4, 3], F32); load(convk, convk_d.ap(), "convk")
    ghm = P.alloc([128, 4], F32); load(ghm, ghm_d.ap(), "ghm")
    bmask = P.alloc([128, 2, 128], F32); load(bmask, c_bmask.ap(), "bmask")
    preq = P.alloc([128, 4098], BF16); prek = P.alloc([128, 4098], BF16)
    ctmp = [P.alloc([128, 512], F32) for _ in range(2)]
    qT = P.alloc([128, 4096], BF16); kT = P.alloc([128, 4096], BF16)
    k_tm = P.alloc([128, 32, 128], BF16); v_tm = P.alloc([128, 32, 129], BF16)
    sigT = P.alloc([128, 4096], BF16)
    PT = P.alloc([128, 2, 32, 128], BF16)
    hacc = P.alloc([64, 64, 128], F32)
    hn = [P.alloc([64, 8, 128], BF16) for _ in range(2)]
    ymT = qT
    C32 = [[P.alloc([128, 129], F32) for _ in range(2)] for _ in range(2)]
    Cbf = [[P.alloc([128, 129], BF16) for _ in range(2)] for _ in range(2)]
    vp = [P.alloc([128, 129], BF16) for _ in range(4)]
    dtmp = [P.alloc([64, 2], F32) for _ in range(4)]
    hsq = P.alloc([64, 128], BF16)
    hss = P.alloc([64, 64], F32)
    memset("dve", v_tm[:, :, 128:129], 1.0, ["v_tm"])
    for h in range(4):
        hs = slice(0, 128)
        P.phase = "p1b_proj"
        for wt, wd, wkey_ in ((wq, wq_d, "wq"), (wk, wk_d, "wk"), (wv, wv_d, "wv"), (wo, wo_d, "wo")):
            P.dma(wt, wd[:, h * 128:(h + 1) * 128].rearrange("(k p) n -> p k n", p=128), writes=[wkey_], q="pool")
        qk = (("convq", wq, "wq", convq, qT, "qT", preq, "preq"), ("convk", wk, "wk", convk, kT, "kT", prek, "prek"))
        for cwk, wmat, wkey, cw, dst, dkey, pre, pk in qk:
            if h == 0:
                memset("dve", pre[:, 0:1], 0.0, [pk]); memset("dve", pre[:, 4097:4098], 0.0, [pk])
            for s in range(8):
                sl = slice(s * 512, (s + 1) * 512)
                bk = s % 2
                for kc in range(8):
                    mm(B[bk][:, :], wmat[:, kc, hs], xnT[:, kc, sl], kc == 0, kc == 7, [wkey] + XNT, [BK[bk]])
                actf(pre[:, 1 + s * 512:1 + (s + 1) * 512], B[bk][:, :], AF.Copy, [BK[bk]], [pk])
        for s in range(8):
            sl = slice(s * 512, (s + 1) * 512)
            bk = s % 2
            for kc in range(8):
                mm(B[bk][:, :], wo[:, kc, hs], xnT[:, kc, sl], kc == 0, kc == 7, ["wo"] + XNT, [BK[bk]])
            actf(sigT[:, sl], B[bk][:, :], AF.Sigmoid, [BK[bk]], ["sigT"])
        for cwk, wmat, wkey, cw, dst, dkey, pre, pk in qk:
            for s in range(8):
                c0 = s * 512
                tb = ctmp[s % 2]
                tk = ("ctmp", s % 2)
                ts("dve", tb, pre[:, c0:c0 + 512], cw[:, h, 0:1], None, ALU.mult, None, [pk, cwk], [tk])
                stt("dve", tb, pre[:, c0 + 1:c0 + 513], cw[:, h, 1:2], tb, ALU.mult, ALU.add, [pk, tk], [tk])
                stt("dve", tb, pre[:, c0 + 2:c0 + 514], cw[:, h, 2:3], tb, ALU.mult, ALU.add, [pk, tk], [tk])
                actf(dst[:, c0:c0 + 512], tb, AF.Silu, [tk], [dkey])
        for j in range(32):
            bk = 2 + j % 2
            for kc in range(8):
                mm(B[bk][:, 0:128], xnT[:, kc, j * 128:(j + 1) * 128], wv[:, kc, hs], kc == 0, kc == 7, ["wv"] + XNT, [BK[bk]])
            cp("dve", v_tm[:, j, 0:128], B[bk][:, 0:128], [BK[bk]], ["v_tm"])
        for j in range(32):
            bk = 6 + j % 2
            tr(Bb[bk][:, 0:128], kT[:, j * 128:(j + 1) * 128], identb, ["kT", "identb"], [BK[bk]])
            ts("dve", k_tm[:, j, :], Bb[bk][:, 0:128], 128 ** -0.5, None, ALU.mult, None, [BK[bk]], ["k_tm"])
        P.phase = "p1b_scan"
        for j in range(32):
            bk = j % 2
            mm(B[bk][:, 0:128], kT[:, j * 128:(j + 1) * 128], qT[:, j * 128:(j + 1) * 128], True, True, ["kT", "qT"], [BK[bk]])
            for d in range(2):
                lc = h + 32 * d
                stt("dve", PT[:, d, j, :], B[bk][:, 0:128], WS128[:, j, lc:lc + 1], bmask[:, d, :], ALU.mult, ALU.mult,
                    [BK[bk], "WS128", "bmask"], [("PT", d, j)])
        for d in range(2):
            memset("dve", C32[d][0], 0.0, [("C32", d, 0)])
        units = []
        for i in range(64):
            for d in range(2):
                units.append((i, d, i if d == 0 else 63 - i))

        def emit_dc(n):
            i, d, c = units[n]
            j, r = c // 2, c % 2
            rs_ = slice(r * 64, (r + 1) * 64)
            lc = h + 32 * d
            vpb = vp[n % 4]; vk = ("vp", n % 4)
            actf(vpb[rs_, :], v_tm[rs_, j, :], AF.Copy, ["v_tm", "WS128"], [vk], scale=WS128[rs_, j, lc:lc + 1])
            db = 4 + n % 2
            mm(B[db][:, 0:129], k_tm[rs_, j, :], vpb[rs_, :], True, True, ["k_tm", vk], [BK[db]])

        def emit_norm(n):
            i, d, c = units[n]
            lc = h + 32 * d
            nb_ = n % 4
            dt_ = dtmp[n % 4]; dk = ("dtmp", n % 4)
            actf(dt_[:, 0:1], B[nb_][0:64, 128:129], AF.Abs, [BK[nb_]], [dk])
            ts("dve", dt_[:, 0:1], dt_[:, 0:1], LB64[:, c, lc:lc + 1], None, ALU.max, None, [dk, "LB64"], [dk])
            recip(dt_[:, 1:2], dt_[:, 0:1], [dk], [dk])
            if i < 32:
                actf(hacc[:, c, :], B[nb_][0:64, 0:128], AF.Copy, [BK[nb_], dk], [("hacc", c)], scale=dt_[:, 1:2])
            else:
                stt("dve", hacc[:, c, :], B[nb_][0:64, 0:128], dt_[:, 1:2], hacc[:, c, :], ALU.mult, ALU.add, [BK[nb_], dk, ("hacc", c)], [("hacc", c)])

        emit_dc(0)
        for n in range(128):
            i, d, c = units[n]
            j, r = c // 2, c % 2
            rs_ = slice(r * 64, (r + 1) * 64)
            l = h + 4 * d
            if n >= 4:
                emit_norm(n - 4)
            if n + 1 < 128:
                emit_dc(n + 1)
            cur, nxt = C32[d][i % 2], C32[d][(i + 1) % 2]
            ck, nk = ("C32", d, i % 2), ("C32", d, (i + 1) % 2)
            cb = Cbf[d][i % 2]; cbk = ("Cbf", d, i % 2)
            actf(cb, cur, AF.Copy, [ck, "DEC"], [cbk], scale=DEC[:, l, c:c + 1])
            db = 4 + n % 2
            stt("dve", nxt, cur, DEC[:, l, c:c + 1], B[db][:, 0:129], ALU.mult, ALU.add, [ck, "DEC", BK[db]], [nk])
            nb_ = n % 4
            mm(B[nb_][0:64, 0:129], qT[:, c * 64:(c + 1) * 64], cb, True, False, ["qT", cbk], [BK[nb_]])
            mm(B[nb_][0:64, 0:129], PT[rs_, d, j, rs_], v_tm[rs_, j, :], False, True, [("PT", d, j), "v_tm"], [BK[nb_]])
        for n in range(124, 128):
            emit_norm(n)
        P.phase = "p1b_post"
        HA = [("hacc", c) for c in range(64)]
        for c in range(64):
            actf(hsq, hacc[:, c, :], AF.Square, [("hacc", c)], ["hsq", "hssss"], accum=hss[:, c:c + 1])
        rms_rstd(hss, hss, 128, "hss")
        for g8 in range(8):
            hb = hn[g8 % 2]; hk = ("hn", g8 % 2)
            for cc in range(8):
                c = g8 * 8 + cc
                ts("dve", hb[:, cc, :], hacc[:, c, :], hss[:, c:c + 1], None, ALU.mult, None, [("hacc", c), "hssrs"], [hk])
            bk = 6 + g8 % 2
            for cc in range(8):
                tr(Bb[bk][:, cc * 64:(cc + 1) * 64], hb[:, cc, :], identb[0:64, 0:64], [hk, "identb"], [BK[bk]])
            sl = slice(g8 * 512, (g8 + 1) * 512)
            stt("dve", ymT[:, sl], Bb[bk][:, 0:512], ghm[:, h:h + 1], sigT[:, sl], ALU.mult, ALU.mult, [BK[bk], "ghm", "sigT"], ["qT"])
        P.dma(ymT_d[h * 128:(h + 1) * 128, :], ymT, reads=["qT"], writes=[("ymT_d", h)])
    P.release()
    P.release()
    if stop <= 2:
        return finish(nc, P, out, tapd, extra_out=[("ymT_d", ymT_d, [512, 4096], BF16)])
    P.phase = "p1c"
    P.mark()
    sinT = P.alloc([96, 4096], BF16); cosT = P.alloc([96, 4096], BF16)
    cqnT = P.alloc([128, 2, 4096], BF16); ckvnT = P.alloc([128, 4096], BF16); krT = P.alloc([96, 4096], BF16)
    R_ = slice(64, 96)
    P.mark()
    posi = P.alloc([96, 4096], I32); load(posi[R_, :], pos.ap(), "posi")
    invf = P.alloc([128, 1], F32); load(invf, c_invf.ap(), "invf")
    ang = P.alloc([96, 4096], F32); kf = P.alloc([96, 4096], F32); fw = P.alloc([96, 4096], F32)
    cp("dve", ang[R_], posi[R_], ["posi"], ["ang"])
    ts("dve", ang[R_], ang[R_], invf[R_, 0:1], 1.0 / (2 * np.pi), ALU.mult, ALU.mult, ["ang", "invf"], ["ang"])
    cp("dve", posi[R_], ang[R_], ["ang"], ["posi"])
    cp("dve", kf[R_], posi[R_], ["posi"], ["kf"])
    tt("dve", ang[R_], ang[R_], kf[R_], ALU.subtract, ["ang", "kf"], ["ang"])
    for dstT, shift, dk_ in ((sinT, 0.0, "sinT"), (cosT, 0.25, "cosT")):
        ts("dve", fw[R_], ang[R_], shift, None, ALU.add, None, ["ang"], ["fw"])
        ts("dve", kf[R_], fw[R_], 0.5, None, ALU.is_gt, None, ["fw"], ["kf"])
        tt("dve", fw[R_], fw[R_], kf[R_], ALU.subtract, ["fw", "kf"], ["fw"])
        ts("dve", kf[R_], fw[R_], -0.5, None, ALU.is_lt, None, ["fw"], ["kf"])
        tt("dve", fw[R_], fw[R_], kf[R_], ALU.add, ["fw", "kf"], ["fw"])
        actf(dstT[R_], fw[R_], AF.Sin, ["fw"], [dk_], scale=6.283185)
    P.release()
    if stop <= 2.3:
        tap("sinT", sinT[64:96], [32, 4096], BF16, ["sinT"]); tap("cosT", cosT[64:96], [32, 4096], BF16, ["cosT"])
        return finish(nc, P, out, tapd)
    def wload(shape, src, key):
        t_ = P.alloc(shape, BF16); load(t_, src, key, q="pool"); return t_
    wcq = wload([128, 8, 256], wcq_d.ap().rearrange("(k p) n -> p k n", p=128), "wcq")
    wckv = wload([128, 8, 128], wckv_d.ap().rearrange("(k p) n -> p k n", p=128), "wckv")
    wkr = wload([128, 8, 96], wkr_d.ap().rearrange("(k p) n -> p k n", p=128), "wkr")
    wkrr = wload([128, 8, 96], wkrr_d.ap().rearrange("(k p) n -> p k n", p=128), "wkrr")
    wqb = wload([128, 2, 768], wqb_d.ap().rearrange("(k p) n -> p k n", p=128), "wqb")
    wqbr = wload([128, 2, 768], wqbr_d.ap().rearrange("(k p) n -> p k n", p=128), "wqbr")
    wkn = wload([128, 512], wkn_d.ap(), "wkn"); wv2 = wload([128, 512], wv2_d.ap(), "wv2")
    ts("dve", wkrr[:, :, 64:80], wkrr[:, :, 64:80], -1.0, None, ALU.mult, None, ["wkrr"], ["wkrr"])
    wqbr4 = wqbr.rearrange("p k (h c) -> p k h c", c=96)
    for k2 in range(2):
        ts("dve", wqbr4[:, k2, :, 64:80], wqbr4[:, k2, :, 64:80], -1.0, None, ALU.mult, None, ["wqbr"], ["wqbr"])
    gqa = P.alloc([128, 2], F32); load(gqa, gqa_d.ap(), "gqa")
    gkva = P.alloc([128, 1], F32); load(gkva, gkva_d.ap(), "gkva")
    gha = P.alloc([64, 8], F32); load(gha, gha_d.ap(), "gha")
    nsel = P.alloc([128, 64], BF16); load(nsel, c_nsel.ap(), "nsel", q="pool")
    v_allf = P.alloc([128, 32, 592], BF16)
    v_all = v_allf[:, :, 0:528].rearrange("p j (h c) -> p j h c", c=66)
    memset("pool", v_allf, 0.0, ["v_all"]); memset("dve", v_all[:, :, :, 64:65], 1.0, ["v_all"])
    t1 = P.alloc([96, 512], F32); t2 = P.alloc([96, 512], F32)
    P.mark()
    sqb = P.alloc([128, 2, 512], BF16); cqf = P.alloc([128, 2, 512], F32); rstd = P.alloc([128, 512], F32)
    sq2 = P.alloc([128, 512], BF16); ckf = P.alloc([128, 512], F32); rstd2 = P.alloc([128, 512], F32)
    for s in range(8):
        sl = slice(s * 512, (s + 1) * 512)
        for blk in range(2):
            for kc in range(8):
                mm(B[blk][:, :], wcq[:, kc, blk * 128:(blk + 1) * 128], xnT[:, kc, sl], kc == 0, kc == 7, ["wcq"] + XNT, [BK[blk]])
            actf(sqb[:, blk, :], B[blk][:, :], AF.Square, [BK[blk]], [("sqb", blk)])
            actf(cqf[:, blk, :], B[blk][:, :], AF.Copy, [BK[blk]], [("cqf", blk)])
        mm(B[2][:, :], onesb, sqb[:, 0, :], True, False, ["onesb", ("sqb", 0)], [BK[2]])
        mm(B[2][:, :], onesb, sqb[:, 1, :], False, True, ["onesb", ("sqb", 1)], [BK[2]])
        actf(rstd, B[2][:, :], AF.Sqrt, [BK[2]], ["rstd"], bias=EPS, scale=1.0 / 256)
        recip(rstd, rstd, ["rstd"], ["rstd"])
        for blk in range(2):
            stt("dve", cqnT[:, blk, sl], cqf[:, blk, :], gqa[:, blk:blk + 1], rstd, ALU.mult, ALU.mult, [("cqf", blk), "gqa", "rstd"], ["cqnT"])
        for kc in range(8):
            mm(B[3][:, :], wckv[:, kc, :], xnT[:, kc, sl], kc == 0, kc == 7, ["wckv"] + XNT, [BK[3]])
        actf(sq2, B[3][:, :], AF.Square, [BK[3]], ["sq2"])
        actf(ckf, B[3][:, :], AF.Copy, [BK[3]], ["ckf"])
        mm(B[4][:, :], onesb, sq2, True, True, ["onesb", "sq2"], [BK[4]])
        actf(rstd2, B[4][:, :], AF.Sqrt, [BK[4]], ["rstd2"], bias=EPS, scale=1.0 / 128)
        recip(rstd2, rstd2, ["rstd2"], ["rstd2"])
        stt("dve", ckvnT[:, sl], ckf, gkva[:, 0:1], rstd2, ALU.mult, ALU.mult, ["ckf", "gkva", "rstd2"], ["ckvnT"])
        for kc in range(8):
            mm(B[5][0:96, :], wkr[:, kc, :], xnT[:, kc, sl], kc == 0, kc == 7, ["wkr"] + XNT, [BK[5]])
        for kc in range(8):
            mm(B[6][0:96, :], wkrr[:, kc, :], xnT[:, kc, sl], kc == 0, kc == 7, ["wkrr"] + XNT, [BK[6]])
        tt("dve", t1[R_], B[5][R_, :], cosT[R_, sl], ALU.mult, [BK[5], "cosT"], ["t1"])
        tt("dve", t2[R_], B[6][R_, :], sinT[R_, sl], ALU.mult, [BK[6], "sinT"], ["t2"])
        tt("dve", krT[R_, sl], t1[R_], t2[R_], ALU.add, ["t1", "t2"], ["krT"])
    for j in range(32):
        bk = j % 2
        mm(B[bk][:, :], ckvnT[:, j * 128:(j + 1) * 128], wv2, True, True, ["ckvnT", "wv2"], [BK[bk]])
        cp("dve", v_all[:, j, :, 0:64], B[bk][:, :].rearrange("p (h d) -> p h d", h=8), [BK[bk]], ["v_all"])
    P.release()
    if stop <= 2.6:
        tap("cqnT", cqnT, [128, 2, 4096], BF16, ["cqnT"]); tap("ckvnT", ckvnT, [128, 4096], BF16, ["ckvnT"]); tap("krT", krT[64:96], [32, 4096], BF16, ["krT"])
        return finish(nc, P, out, tapd, extra_out=[("ymT_d", ymT_d, [512, 4096], BF16)])
    P.barrier()
    qTh_ = [P.alloc([128, 4096], BF16), xnT[:, 0, :]]
    kTh_ = [P.alloc([128, 4096], BF16), xnT[:, 1, :]]
    vh_ = [P.alloc([128, 32, 128], BF16), xnT[:, 2, :].rearrange("p (j c) -> p j c", c=128)]
    for pb in range(2):
        memset("dve", qTh_[pb], 0.0, [("qTh", pb)]); memset("pool", kTh_[pb], 0.0, [("kTh", pb)])
        memset("pool", vh_[pb], 0.0, [("vh", pb)]); memset("dve", vh_[pb][:, :, 64:65], 1.0, [("vh", pb)])
    PT2 = [P.alloc([128, 1024], BF16) for _ in range(2)]
    osq = P.alloc([128, 512], BF16); memset("dve", osq, 0.0, ["osq"]); ocp = P.alloc([64, 512], F32); rden = P.alloc([64, 512], F32)
    yab = [P.alloc([64, 512], BF16) for _ in range(2)]

    def pro_groups(h, s):
        pb = h % 2
        qTh, kTh = qTh_[pb], kTh_[pb]
        hc = slice(h * 96, (h + 1) * 96)
        sl = slice(s * 512, (s + 1) * 512)

        def g0():
            ph = P.phase; P.phase = "p1c_pro"
            for k2 in range(2):
                mm(B[5][0:96, :], wqb[:, k2, hc], cqnT[:, k2, sl], k2 == 0, k2 == 1, ["wqb", "cqnT"], [BK[5]])
            cp("dve", qTh[0:64, sl], B[5][0:64, :], [BK[5]], [("qTh", pb)])
            tt("dve", t1[R_], B[5][R_, :], cosT[R_, sl], ALU.mult, [BK[5], "cosT"], ["t1"])
            P.phase = ph

        def g1():
            ph = P.phase; P.phase = "p1c_pro"
            for k2 in range(2):
                mm(B[5][0:96, :], wqbr[:, k2, hc], cqnT[:, k2, sl], k2 == 0, k2 == 1, ["wqbr", "cqnT"], [BK[5]])
            tt("dve", t2[R_], B[5][R_, :], sinT[R_, sl], ALU.mult, [BK[5], "sinT"], ["t2"])
            tt("dve", qTh[R_, sl], t1[R_], t2[R_], ALU.add, ["t1", "t2"], [("qTh", pb)])
            P.phase = ph

        def g2():
            ph = P.phase; P.phase = "p1c_pro"
            mm(B[5][0:64, :], wkn[:, h * 64:(h + 1) * 64], ckvnT[:, sl], True, True, ["wkn", "ckvnT"], [BK[5]])
            cp("dve", kTh[0:64, sl], B[5][0:64, :], [BK[5]], [("kTh", pb)])
            P.phase = ph
        return [g0, g1, g2]

    def pro_step(h, s):
        for g in pro_groups(h, s):
            g()

    def pro_fin(h):
        pb = h % 2
        cp("dve", kTh_[pb][R_, :], krT[R_, :], ["krT"], [("kTh", pb)])
        cp("dve", vh_[pb][:, :, 0:64], v_all[:, :, h, 0:64], ["v_all"], [("vh", pb)])

    def attn(h, Q, inserts=()):
        inserts = list(inserts)
        pb_ = h % 2
        qTh, kTh, vh = qTh_[pb_], kTh_[pb_], vh_[pb_]
        ql = slice(Q * 512, (Q + 1) * 512)
        ob = OB_BASE + Q % 2

        def st_(kp):
            pb = kp % 2
            for u in range(2):
                kt = 2 * kp + u
                mm(B[2 * pb + u][:, :], kTh[:, kt * 128:(kt + 1) * 128], qTh[:, ql], True, True, [("kTh", pb_), ("qTh", pb_)], [BK[2 * pb + u]])
            actf(PT2[pb], TP[pb][:, :], AF.Exp, [BK[2 * pb], BK[2 * pb + 1]], [("PT2", pb)], scale=96 ** -0.5)
        st_(0)
        for kp in range(16):
            if kp + 1 < 16:
                st_(kp + 1)
            for u in range(2):
                kt = 2 * kp + u
                mm(B[ob][:, :], vh[:, kt, :], PT2[kp % 2][:, u * 512:(u + 1) * 512], kt == 0, kt == 31, [("vh", pb_), ("PT2", kp % 2)], [BK[ob]])
            if kp in (3, 8, 13) and inserts:
                inserts.pop(0)()
        actf(osq, B[ob][:, :], AF.Square, [BK[ob]], ["osq"])
        cp("dve", ocp, B[ob][0:64, :], [BK[ob]], ["ocp"])
        mm(B[4][0:64, :], nsel[:, :], osq[:, :], True, True, ["nsel", "osq"], [BK[4]])
        actf(rden, B[4][0:64, :], AF.Sqrt, [BK[4]], ["rden"])
        recip(rden, rden, ["rden"], ["rden"])
        yb = yab[Q % 2]; yk = ("yab", Q % 2)
        stt("dve", yb, ocp, gha[:, h:h + 1], rden, ALU.mult, ALU.mult, ["ocp", "gha", "rden"], [yk])
        P.dma(yaT_d[h, :, ql], yb, reads=[yk], writes=[("yaT_d", h, Q)])

    for s_ in range(8):
        pro_step(0, s_)
    pro_fin(0)
    P.phase = "p1c_attn"
    for h in range(NHEADS_MLA):
        for Q in range(8):
            attn(h, Q, pro_groups(h + 1, Q) if h + 1 < NHEADS_MLA else ())
        if h + 1 < NHEADS_MLA:
            pro_fin(h + 1)
    P.release()
    P.barrier()
    P.top = 0
    identb = P.alloc([128, 128], BF16); load(identb, c_identb.ap(), "identb")
    identf = P.alloc([128, 128], F32); load(identf, c_identf.ap(), "identf")
    if stop <= 3:
        return finish(nc, P, out, tapd, extra_out=[("ymT_d", ymT_d, [512, 4096], BF16), ("yaT_d", yaT_d, [8, 64, 4096], BF16)])

    P.phase = "p2"
    AFF = P.alloc([128, 32, 16], F32)
    P.mark()
    woutm = wload([128, 4, 1024], woutm_d.ap().rearrange("(k p) n -> p k n", p=128), "woutm")
    wouta = wload([128, 4, 1024], wouta_d.ap().rearrange("h p n -> (h p) n").rearrange("(k p) n -> p k n", p=128), "wouta")
    wmq = wload([128, 8, 1024], wmq_d.ap().rearrange("(k p) n -> p k n", p=128), "wmq")
    wmo = wload([128, 8, 1024], wmo_d.ap().rearrange("(k p) n -> p k n", p=128), "wmo")
    gmx = P.alloc([128, 8], F32); load(gmx, gmx_d.ap(), "gmx")
    gffn_b = P.alloc([128, 1024], F32); load(gffn_b, gffn_d.ap().to_broadcast([128, 1024]), "gffn_b")
    gffn_p = P.alloc([128, 8], F32); load(gffn_p, gffnp_d.ap(), "gffn_p")
    wr = P.alloc([128, 8, 16], F32); load(wr, wr_d.ap().rearrange("(k p) n -> p k n", p=128), "wr")
    memKT = P.alloc([128, 8, 256], BF16); memV = P.alloc([128, 2, 4, 257], BF16)
    memset("dve", memV[:, :, :, 256:257], 1.0, ["memV"])
    P.mark()
    wmk = wload([128, 8, 1024], wmk_d.ap().rearrange("(k p) n -> p k n", p=128), "wmk")
    wmv = wload([128, 8, 1024], wmv_d.ap().rearrange("(k p) n -> p k n", p=128), "wmv")
    gmkv = P.alloc([128, 8], F32); load(gmkv, gmkv_d.ap(), "gmkv")
    mx_ = P.alloc([128, 2, 1024], F32); load(mx_, mem.ap().rearrange("(j p) d -> p j d", p=128), "mx")
    mnb = P.alloc([128, 2, 1024], BF16); memnT = P.alloc([128, 8, 256], BF16)
    mss = P.alloc([128, 2], F32); mjunk = P.alloc([128, 1024], BF16)
    for j in range(2):
        actf(mjunk, mx_[:, j, :], AF.Square, ["mx"], ["mjunk", ("mss", j)], accum=mss[:, j:j + 1])
        actf(mss[:, j:j + 1], mss[:, j:j + 1], AF.Sqrt, [("mss", j)], [("mss", j)], bias=EPS, scale=1.0 / 1024)
        recip(mss[:, j:j + 1], mss[:, j:j + 1], [("mss", j)], [("mss", j)])
        ts("dve", mnb[:, j, :], mx_[:, j, :], mss[:, j:j + 1], None, ALU.mult, None, ["mx", ("mss", j)], [("mnb", j)])
    for kc in range(8):
        bk = 6 + kc % 2
        for j in range(2):
            tr(Bb[bk][:, j * 128:(j + 1) * 128], mnb[:, j, kc * 128:(kc + 1) * 128], identb, [("mnb", j), "identb"], [BK[bk]])
        ts("dve", memnT[:, kc, :], Bb[bk][:, 0:256], gmkv[:, kc:kc + 1], None, ALU.mult, None, [BK[bk], "gmkv"], ["memnT"])
    for blk in range(8):
        bk = blk % 2
        for kc in range(8):
            mm(B[bk][:, 0:256], wmk[:, kc, blk * 128:(blk + 1) * 128], memnT[:, kc, :], kc == 0, kc == 7, ["wmk", "memnT"], [BK[bk]])
        cp("dve", memKT[:, blk, :], B[bk][:, 0:256], [BK[bk]], ["memKT"])
    for mt in range(2):
        for hf in range(2):
            bk = 2 + hf
            for kc in range(8):
                mm(B[bk][:, :], memnT[:, kc, mt * 128:(mt + 1) * 128], wmv[:, kc, hf * 512:(hf + 1) * 512], kc == 0, kc == 7, ["wmv", "memnT"], [BK[bk]])
            cp("dve", memV[:, mt, 2 * hf:2 * hf + 2, 0:256], B[bk][:, :].rearrange("p (h d) -> p h d", h=2), [BK[bk]], ["memV"])
    P.release()
    xs2_ = [P.alloc([128, 4, 1024], F32) for _ in range(2)]; ymTs_ = [P.alloc([128, 4, 512], BF16) for _ in range(2)]; yaTs_ = [P.alloc([128, 4, 512], BF16) for _ in range(2)]
    xn2 = P.alloc([128, 4, 1024], BF16); xn2T = P.alloc([128, 8, 512], BF16); qmT = P.alloc([128, 8, 512], BF16)
    PmT = P.alloc([128, 4, 2, 512], BF16); om = P.alloc([128, 4, 1024], BF16); omT = P.alloc([128, 8, 512], BF16)
    x2T = P.alloc([128, 8, 128], F32); xn3 = P.alloc([128, 4, 1024], BF16)
    st2_ = [P.alloc([128, 16], F32) for _ in range(2)]; rtmp = P.alloc([128, 16], F32); lg = P.alloc([128, 16], F32); ex = P.alloc([128, 16], F32)
    junk2 = P.alloc([128, 1024], BF16)
    ymT_v = ymT_d.ap().rearrange("(k p) t -> p k t", p=128)
    yaT_v = yaT_d.ap().rearrange("h p t -> (h p) t").rearrange("(k p) t -> p k t", p=128)
    YD = [("ymT_d", h_) for h_ in range(4)]
    def stageA(s):
        pb = s % 2
        xs2, ymTs, yaTs, st2 = xs2_[pb], ymTs_[pb], yaTs_[pb], st2_[pb]
        sl = slice(s * 512, (s + 1) * 512)
        P.dma(xs2, x[s * 512:(s + 1) * 512, :].rearrange("(j p) d -> p j d", p=128), writes=[("xs2", pb)])
        P.dma(ymTs, ymT_v[:, :, sl], reads=YD, writes=[("ymTs", pb)])
        P.dma(yaTs, yaT_v[:, :, sl], reads=[("yaT_d", h_, s) for h_ in range(8)], writes=[("yaTs", pb)])
        for j in range(4):
            tl = slice(j * 128, (j + 1) * 128)
            for hf in range(2):
                fl = slice(hf * 512, (hf + 1) * 512)
                bk = (2 * j + hf) % 4
                for blk in range(4):
                    mm(B[bk][:, :], ymTs[:, blk, tl], woutm[:, blk, fl], blk == 0, False, [("ymTs", pb), "woutm"], [BK[bk]])
                for blk in range(4):
                    mm(B[bk][:, :], yaTs[:, blk, tl], wouta[:, blk, fl], False, blk == 3, [("yaTs", pb), "wouta"], [BK[bk]])
                tt("dve", xs2[:, j, fl], xs2[:, j, fl], B[bk][:, :], ALU.add, [("xs2", pb), BK[bk]], [("xs2", pb)])
        for j in range(4):
            actf(junk2, xs2[:, j, :], AF.Square, [("xs2", pb)], ["junk2", ("st2", pb, j)], accum=st2[:, j:j + 1])
            actf(st2[:, j:j + 1], st2[:, j:j + 1], AF.Sqrt, [("st2", pb, j)], [("st2", pb, j)], bias=EPS, scale=1.0 / 1024)
            recip(st2[:, j:j + 1], st2[:, j:j + 1], [("st2", pb, j)], [("st2", pb, j)])
            ts("dve", xn2[:, j, :], xs2[:, j, :], st2[:, j:j + 1], None, ALU.mult, None, [("xs2", pb), ("st2", pb, j)], ["xn2"])
        for kc in range(8):
            bk = 6 + kc % 2
            for j in range(4):
                tr(Bb[bk][:, j * 128:(j + 1) * 128], xn2[:, j, kc * 128:(kc + 1) * 128], identb, ["xn2", "identb"], [BK[bk]])
            if kc % 2:
                ts("dve", xn2T[:, kc, :], Bb[bk][:, 0:512], gmx[:, kc:kc + 1], None, ALU.mult, None, [BK[bk], "gmx"], ["xn2T"])
            else:
                actf(xn2T[:, kc, :], Bb[bk][:, 0:512], AF.Copy, [BK[bk], "gmx"], ["xn2T"], scale=gmx[:, kc:kc + 1])
        for blk in range(8):
            bk = blk % 2
            for kc in range(8):
                mm(B[bk][:, :], wmq[:, kc, blk * 128:(blk + 1) * 128], xn2T[:, kc, :], kc == 0, kc == 7, ["wmq", "xn2T"], [BK[bk]])
            if blk % 2:
                cp("dve", qmT[:, blk, :], B[bk][:, :], [BK[bk]], ["qmT"])
            else:
                actf(qmT[:, blk, :], B[bk][:, :], AF.Copy, [BK[bk]], ["qmT"])
    def stageB(s):
        pb = s % 2
        xs2, ymTs, yaTs, st2 = xs2_[pb], ymTs_[pb], yaTs_[pb], st2_[pb]
        for h_ in range(4):
            for mc in range(2):
                bk = 2 + (2 * h_ + mc) % 2
                ml = slice(mc * 128, (mc + 1) * 128)
                mm(B[bk][:, :], memKT[:, 2 * h_, ml], qmT[:, 2 * h_, :], True, False, ["memKT", "qmT"], [BK[bk]])
                mm(B[bk][:, :], memKT[:, 2 * h_ + 1, ml], qmT[:, 2 * h_ + 1, :], False, True, ["memKT", "qmT"], [BK[bk]])
                actf(PmT[:, h_, mc, :], B[bk][:, :], AF.Exp, [BK[bk]], ["PmT"], scale=1.0 / 16)
        for j in range(4):
            tl = slice(j * 128, (j + 1) * 128)
            for h_ in range(4):
                i_ = 4 * j + h_
                bk = 4 + i_ % 2
                mm(B[bk][:, 0:257], PmT[:, h_, 0, tl], memV[:, 0, h_, :], True, False, ["PmT", "memV"], [BK[bk]])
                mm(B[bk][:, 0:257], PmT[:, h_, 1, tl], memV[:, 1, h_, :], False, True, ["PmT", "memV"], [BK[bk]])
                recip(rtmp[:, i_:i_ + 1], B[bk][:, 256:257], [BK[bk]], [("rtmp", i_)])
                ts("dve", om[:, j, h_ * 256:(h_ + 1) * 256], B[bk][:, 0:256], rtmp[:, i_:i_ + 1], None, ALU.mult, None, [BK[bk], ("rtmp", i_)], ["om"])
        for kc in range(8):
            bk = 6 + kc % 2
            for j in range(4):
                tr(Bb[bk][:, j * 128:(j + 1) * 128], om[:, j, kc * 128:(kc + 1) * 128], identb, ["om", "identb"], [BK[bk]])
            if kc % 2:
                cp("dve", omT[:, kc, :], Bb[bk][:, 0:512], [BK[bk]], ["omT"])
            else:
                actf(omT[:, kc, :], Bb[bk][:, 0:512], AF.Copy, [BK[bk]], ["omT"])
        for j in range(4):
            tl = slice(j * 128, (j + 1) * 128)
            for hf in range(2):
                fl = slice(hf * 512, (hf + 1) * 512)
                bk = (2 * j + hf) % 4
                for kc in range(8):
                    mm(B[bk][:, :], omT[:, kc, tl], wmo[:, kc, fl], kc == 0, kc == 7, ["omT", "wmo"], [BK[bk]])
                tt("dve", xs2[:, j, fl], xs2[:, j, fl], B[bk][:, :], ALU.add, [("xs2", pb), BK[bk]], [("xs2", pb)])
    def stageC(s):
        pb = s % 2
        xs2, ymTs, yaTs, st2 = xs2_[pb], ymTs_[pb], yaTs_[pb], st2_[pb]
        P.dma(acc_d[s * 512:(s + 1) * 512, :].rearrange("(j p) d -> p j d", p=128), xs2, reads=[("xs2", pb)], writes=["acc_d"])
        for j in range(4):
            t = s * 4 + j
            q_ = 4 + j
            actf(junk2, xs2[:, j, :], AF.Square, [("xs2", pb)], ["junk2", ("st2", pb, q_)], accum=st2[:, q_:q_ + 1])
            actf(st2[:, q_:q_ + 1], st2[:, q_:q_ + 1], AF.Sqrt, [("st2", pb, q_)], [("st2", pb, q_)], bias=EPS, scale=1.0 / 1024)
            recip(st2[:, q_:q_ + 1], st2[:, q_:q_ + 1], [("st2", pb, q_)], [("st2", pb, q_)])
            stt("dve", xn3[:, j, :], xs2[:, j, :], st2[:, q_:q_ + 1], gffn_b, ALU.mult, ALU.mult, [("xs2", pb), ("st2", pb, q_), "gffn_b"], ["xn3"])
            for kc in range(8):
                bk = kc % 2
                tr(B[bk][:, 0:128], xs2[:, j, kc * 128:(kc + 1) * 128], identf, [("xs2", pb), "identf"], [BK[bk]])
                if kc % 2:
                    ts("dve", x2T[:, kc, :], B[bk][:, 0:128], gffn_p[:, kc:kc + 1], None, ALU.mult, None, [BK[bk], "gffn_p"], [("x2T", kc)])
                else:
                    actf(x2T[:, kc, :], B[bk][:, 0:128], AF.Copy, [BK[bk], "gffn_p"], [("x2T", kc)], scale=gffn_p[:, kc:kc + 1])
            for kc in range(8):
                mm(B[2][:, 0:16], x2T[:, kc, :], wr[:, kc, :], kc == 0, kc == 7, [("x2T", kc), "wr"], [BK[2]])
            ts("dve", lg, B[2][:, 0:16], st2[:, q_:q_ + 1], None, ALU.mult, None, [BK[2], ("st2", pb, q_)], ["lg"])
            P.dve(lambda e: e.tensor_reduce(out=ex[:, 0:1], in_=lg, axis=AX.X, op=ALU.max), ["lg"], ["exm"])
            ts("dve", ex[:, 0:1], ex[:, 0:1], -1.0, None, ALU.mult, None, ["exm"], ["exm"])
            actf(lg, lg, AF.Exp, ["lg", "exm"], ["lg", "exs"], bias=ex[:, 0:1], accum=ex[:, 1:2])
            recip(ex[:, 1:2], ex[:, 1:2], ["exs"], ["exs"])
            ts("dve", AFF[:, t, :], lg, ex[:, 1:2], None, ALU.mult, None, ["lg", "exs"], ["AFF"])
        P.dma(h_d[s * 512:(s + 1) * 512, :].rearrange("(j p) d -> p j d", p=128), xn3, reads=["xn3"], writes=["h_d"])
    stageA(0)
    for s in range(8):
        stageB(s)
        if s + 1 < 8:
            stageA(s + 1)
        stageC(s)
    tap("AFF", AFF, [128, 32, 16], F32, ["AFF"])
    P.release()
    if stop <= 4:
        return finish(nc, P, out, tapd, extra_out=[("acc_d", acc_d, [4096, 1024], F32)])

    P.phase = "p3"
    P.mark()
    iota = P.alloc([128, 512], F32); load(iota, c_iota.ap(), "iota")
    posm = P.alloc([128, 32, 16], F32)
    RH = P.alloc([128, 32, 16, 4], BF16)
    P.mark()
    onesf = P.alloc([128, 128], F32); load(onesf, c_onesf.ap(), "onesf")
    Uf = P.alloc([128, 128], F32); load(Uf, c_U.ap(), "Uf")
    jp = P.alloc([128, 32, 2], F32); load(jp, c_jp.ap(), "jp")
    lo = P.alloc([128, 16], F32); mid = P.alloc([128, 16], F32); tot = P.alloc([128, 16], F32); tq = P.alloc([128, 16], F32)
    sel = P.alloc([128, 32, 16], F32); cum = P.alloc([128, 32, 16], F32)
    ones32 = P.alloc([128, 32], F32); memset("dve", ones32, 1.0, ["ones32"])
    afh = P.alloc([128, 32, 16], BF16); afl = P.alloc([128, 32, 16], F32)
    memset("dve", lo, 0.0, ["lo"])
    mid3 = mid.rearrange("p (o e) -> p o e", o=1).to_broadcast([128, 32, 16])
    lo3 = lo.rearrange("p (o e) -> p o e", o=1).to_broadcast([128, 32, 16])
    self2 = sel.rearrange("p j e -> p (j e)")
    for it in range(24):
        step = 0.5 ** (it + 1)
        ts("dve", mid, lo, step, None, ALU.add, None, ["lo"], ["mid"])
        tt("dve", sel, AFF, mid3, ALU.is_ge, ["AFF", "mid"], ["sel"])
        mm(B[0][:, :], onesf, self2, True, True, ["onesf", "sel"], [BK[0]])
        P.dve(lambda e: e.tensor_reduce(out=tot, in_=B[0][:, :].rearrange("p (j e) -> p e j", e=16), axis=AX.X, op=ALU.add), [BK[0]], ["tot"])
        stt("dve", tq, tot, 512.0, mid, ALU.is_ge, ALU.mult, ["tot", "mid"], ["tq"])
        tt("dve", lo, lo, tq, ALU.max, ["lo", "tq"], ["lo"])
    tt("dve", sel, AFF, lo3, ALU.is_ge, ["AFF", "lo"], ["sel"])
    for e_ in range(16):
        P.dve(lambda e, e_=e_: e.tensor_tensor_scan(out=cum[:, :, e_], data0=ones32, data1=sel[:, :, e_], initial=0.0, op0=ALU.mult, op1=ALU.add), ["ones32", "sel"], ["cum"])
    tt("dve", cum, cum, sel, ALU.subtract, ["cum", "sel"], ["cum"])
    mm(B[1][:, :], onesf, cum.rearrange("p j e -> p (j e)"), True, False, ["onesf", "cum"], [BK[1]])
    mm(B[1][:, :], Uf, self2, False, True, ["Uf", "sel"], [BK[1]])
    stt("dve", posm.rearrange("p j e -> p (j e)"), B[1][:, :], 1.0, self2, ALU.add, ALU.mult, [BK[1], "sel"], ["posm"])
    ts("dve", posm, posm, -1.0, None, ALU.add, None, ["posm"], ["posm"])
    cp("dve", afh, AFF, ["AFF"], ["afh"])
    tt("dve", afl, AFF, afh, ALU.subtract, ["AFF", "afh"], ["afl"])
    cp("dve", RH[:, :, :, 2], afh, ["afh"], ["RH"])
    cp("dve", RH[:, :, :, 3], afl, ["afl", "RH"], ["RH"])
    for e_ in range(16):
        cp("dve", RH[:, :, e_, 0:2], jp, ["jp", "RH"], ["RH"])
    P.release()
    P.phase = "p3_alloc"
    OH = P.alloc([128, 32, 512], BF16)
    res = P.alloc([128, 16], F32); idxf = [P.alloc([128, 4], F32) for _ in range(2)]; idxi = [P.alloc([128, 4], I32) for _ in range(2)]
    gate_ = [P.alloc([128, 4], F32) for _ in range(2)]
    xe = [P.alloc([128, 4, 1024], BF16) for _ in range(2)]
    xeT = P.alloc([128, 8, 512], BF16); hidT = P.alloc([128, 16, 512], BF16)
    sg = [P.alloc([128, 512], F32) for _ in range(2)]
    yacc = [P.alloc([128, 1024], F32) for _ in range(4)]
    NGU, ND = 4, 4
    gub = [P.alloc([128, 2, 8, 512], BF16) for _ in range(NGU)]
    dbf = [P.alloc([128, 4, 1024], BF16) for _ in range(ND)]

    def route_oh(e_, j0, j1):
        ph = P.phase; P.phase = "p3_route"
        for j in range(j0, j1):
            ts("dve", OH[:, j, :], iota, posm[:, j, e_:e_ + 1], None, ALU.is_equal, None, ["iota", "posm"], [("OH", j)])
        P.phase = ph

    def route_rest(e_):
        ph = P.phase; P.phase = "p3_route"
        p_ = e_ % 2
        for sc in range(4):
            for j in range(32):
                mm(B[0][:, sc * 4:(sc + 1) * 4], OH[:, j, sc * 128:(sc + 1) * 128], RH[:, j, e_, :], j == 0, j == 31, [("OH", j), "RH"], [BK[0]])
        cp("dve", res, B[0][:, 0:16], [BK[0]], ["res"])
        r3 = res.rearrange("p (s c) -> p s c", c=4)
        stt("dve", idxf[p_], r3[:, :, 0], 128.0, r3[:, :, 1], ALU.mult, ALU.add, ["res"], [("idxf", p_)])
        tt("dve", gate_[p_], r3[:, :, 2], r3[:, :, 3], ALU.add, ["res"], [("gate", p_)])
        cp("dve", idxi[p_], idxf[p_], [("idxf", p_)], [("idxi", p_)])
        for sc in range(4):
            P.op("pool", lambda e, sc=sc, p_=p_: e.indirect_dma_start(out=xe[p_][:, sc, :], out_offset=None, in_=h_d[:, :],
                                                                in_offset=bass.IndirectOffsetOnAxis(ap=idxi[p_][:, sc:sc + 1], axis=0)),
                 reads=[("idxi", p_), "h_d"], writes=[("xe", p_, sc)], dma=True)
        P.phase = ph

    def wload_gu(e_, q4):
        ph = P.phase; P.phase = "p3_wload"
        g_ = gub[q4 % NGU]
        P.dma(g_[:, 0, :, :], weg_d[e_, :, q4 * 512:(q4 + 1) * 512].rearrange("(k p) n -> p k n", p=128), writes=[("gub", q4 % NGU, 0)], q="pool")
        P.dma(g_[:, 1, :, :], weu_d[e_, :, q4 * 512:(q4 + 1) * 512].rearrange("(k p) n -> p k n", p=128), writes=[("gub", q4 % NGU, 1)], q="pool")
        P.phase = ph

    def wload_d(e_, q4):
        ph = P.phase; P.phase = "p3_wload"
        P.dma(dbf[q4 % ND], wed_d[e_, q4 * 512:(q4 + 1) * 512, :].rearrange("(k p) n -> p k n", p=128), writes=[("dbf", q4 % ND)], q="pool")
        P.phase = ph

    def wloads(e_):
        for q4 in range(4):
            wload_gu(e_, q4)
        for q4 in range(4):
            wload_d(e_, q4)

    def compute(e_):
        P.phase = "p3_xT"
        p_ = e_ % 2
        for kc in range(8):
            for sc in range(4):
                tr(Bb[7][:, sc * 128:(sc + 1) * 128], xe[p_][:, sc, kc * 128:(kc + 1) * 128], identb, [("xe", p_, sc), "identb"], [BK[7]])
            if kc % 2:
                cp("dve", xeT[:, kc, :], Bb[7][:, 0:512], [BK[7]], ["xeT"])
            else:
                actf(xeT[:, kc, :], Bb[7][:, 0:512], AF.Copy, [BK[7]], ["xeT"])
        P.phase = "p3_gu"
        for fc in range(16):
            q4, f4 = fc // 4, fc % 4
            g_ = gub[q4 % NGU]
            bg, bu = 1 + (fc % 2) * 2, 2 + (fc % 2) * 2
            for kc in range(8):
                mm(B[bg][:, :], g_[:, 0, kc, f4 * 128:(f4 + 1) * 128], xeT[:, kc, :], kc == 0, kc == 7, [("gub", q4 % NGU, 0), "xeT"], [BK[bg]])
            for kc in range(8):
                mm(B[bu][:, :], g_[:, 1, kc, f4 * 128:(f4 + 1) * 128], xeT[:, kc, :], kc == 0, kc == 7, [("gub", q4 % NGU, 1), "xeT"], [BK[bu]])
            actf(sg[fc % 2], B[bg][:, :], AF.Silu, [BK[bg]], [("sg", fc % 2)])
            tt("dve", hidT[:, fc, :], sg[fc % 2], B[bu][:, :], ALU.mult, [("sg", fc % 2), BK[bu]], [("hidT", fc)])
            if e_ + 1 < 16:
                route_oh(e_ + 1, 2 * fc, 2 * fc + 2)
            if f4 == 3 and e_ + 1 < 16:
                wload_gu(e_ + 1, q4)
        if e_ + 1 < 16:
            route_rest(e_ + 1)
        P.phase = "p3_down"
        gi = 0
        for q4 in range(4):
            for st_ in range(4):
                for hf in range(2):
                    bk = 5 + gi % 2
                    gi += 1
                    fl = slice(hf * 512, (hf + 1) * 512)
                    for f4 in range(4):
                        fc = q4 * 4 + f4
                        mm(B[bk][:, :], hidT[:, fc, st_ * 128:(st_ + 1) * 128], dbf[q4 % ND][:, f4, fl], f4 == 0, f4 == 3,
                           [("hidT", fc), ("dbf", q4 % ND)], [BK[bk]])
                    yk = ("ya", st_, hf)
                    if q4 == 0:
                        actf(yacc[st_][:, fl], B[bk][:, :], AF.Copy, [BK[bk], ("gate", p_)], [yk], scale=gate_[p_][:, st_:st_ + 1])
                    else:
                        stt("dve", yacc[st_][:, fl], B[bk][:, :], gate_[p_][:, st_:st_ + 1], yacc[st_][:, fl], ALU.mult, ALU.add,
                            [BK[bk], ("gate", p_), yk], [yk])
            if e_ + 1 < 16:
                wload_d(e_ + 1, q4)
        for st_ in range(4):
            P.op("pool", lambda e, st_=st_, p_=p_: e.indirect_dma_start(out=acc_d[:, :], out_offset=bass.IndirectOffsetOnAxis(ap=idxi[p_][:, st_:st_ + 1], axis=0),
                                                                in_=yacc[st_], in_offset=None, compute_op=ALU.add),
                 reads=[("idxi", p_), ("ya", st_, 0), ("ya", st_, 1), "acc_d"], writes=["acc_d"], dma=True)

    route_oh(0, 0, 32)
    route_rest(0)
    wloads(0)
    for e_ in range(16):
        compute(e_)
    P.release()
    if stop <= 5:
        return finish(nc, P, out, tapd, extra_out=[("acc_d", acc_d, [4096, 1024], F32)])

    P.phase = "p4"
    gfin_b = P.alloc([128, 1024], F32); load(gfin_b, gfin_d.ap().to_broadcast([128, 1024]), "gfin_b")
    xf = [P.alloc([128, 4, 1024], F32) for _ in range(2)]
    of = [P.alloc([128, 4, 1024], F32) for _ in range(2)]
    fs = P.alloc([128, 32], F32); junk3 = P.alloc([128, 1024], BF16)
    for s in range(8):
        b = s % 2
        P.dma(xf[b], acc_d[s * 512:(s + 1) * 512, :].rearrange("(j p) d -> p j d", p=128), reads=["acc_d"], writes=[("xf", b)])
        for j in range(4):
            t = s * 4 + j
            actf(junk3, xf[b][:, j, :], AF.Square, [("xf", b)], ["junk3", ("fs", t)], accum=fs[:, t:t + 1])
            actf(fs[:, t:t + 1], fs[:, t:t + 1], AF.Sqrt, [("fs", t)], [("fs", t)], bias=EPS, scale=1.0 / 1024)
            recip(fs[:, t:t + 1], fs[:, t:t + 1], [("fs", t)], [("fs", t)])
            stt("dve", of[b][:, j, :], xf[b][:, j, :], fs[:, t:t + 1], gfin_b, ALU.mult, ALU.mult, [("xf", b), ("fs", t), "gfin_b"], [("of", b)])
        P.dma(out[s * 512:(s + 1) * 512, :].rearrange("(j p) d -> p j d", p=128), of[b], reads=[("of", b)], writes=[("out", s)])
    return finish(nc, P, out, tapd)


def finish(nc, P, out, tapd, extra_out=()):
    for name, src, shp, dt in extra_out:
        t = nc.dram_tensor("tap_" + name, list(shp), dt, kind="ExternalOutput")
        tapd[name] = t
        P.barrier()
        P.dma(t.ap(), src.ap(), writes=[("tap", name)])
    P.barrier()
    P.emit()
    return nc, P, tapd


def _bf(a):
    return np.ascontiguousarray(a).astype(ml_dtypes.bfloat16)


def _pk(v, k):
    return np.ascontiguousarray(np.asarray(v, np.float32).reshape(k, 128).T)


def const_inputs():
    c = {}
    c["c_identb"] = _bf(np.eye(128, dtype=np.float32))
    c["c_identf"] = np.eye(128, dtype=np.float32)
    c["c_onesb"] = _bf(np.ones((128, 128), np.float32))
    c["c_onesf"] = np.ones((128, 128), np.float32)
    s = np.arange(128)[:, None]; t = np.arange(128)[None, :]
    same = (s // 64) == (t // 64)
    bm = np.zeros((128, 2, 128), np.float32)
    bm[:, 0, :] = (same & (s <= t)); bm[:, 1, :] = (same & (s >= t))
    bm *= np.float32(128 ** -0.5)
    c["c_bmask"] = bm
    sel = np.zeros((36, 8, 128), np.float32)
    for l, p in enumerate(LANE_PART):
        sel[p, l, :] = 1.0
    c["c_sel36"] = sel
    ns = np.zeros((128, 64), np.float32); ns[0:64, :] = 1.0 / 64; ns[64, :] = EPS
    c["c_nsel"] = ns
    invf = np.zeros((128, 1), np.float32)
    f = (10000.0 ** (-np.arange(0, 32, 2, dtype=np.float32) / 32)).astype(np.float32)
    invf[64:80, 0] = f; invf[80:96, 0] = f
    c["c_invf"] = invf
    r = np.ones((36, 4096), np.float32); r[:, ::64] = 0.0
    c["c_reset"] = r
    c["c_iota"] = np.tile(np.arange(512, dtype=np.float32)[None, :], (128, 1))
    c["c_tokid"] = (np.arange(32)[None, :] * 128 + np.arange(128)[:, None]).astype(np.float32)
    c["c_U"] = (s < t).astype(np.float32)
    jp = np.zeros((128, 32, 2), np.float32); jp[:, :, 0] = np.arange(32)[None, :]; jp[:, :, 1] = np.arange(128)[:, None]
    c["c_jp"] = jp
    return c


def weight_inputs(I):
    g = lambda k: np.asarray(I[k], np.float32)
    w = {}
    w_in = g("w_in")[0]
    w["gmix"] = _pk(g("g_mix")[0], 8)
    w["w_q"] = np.ascontiguousarray(w_in[:, 0:512]); w["w_k"] = np.ascontiguousarray(w_in[:, 512:1024])
    w["w_v"] = np.ascontiguousarray(w_in[:, 1024:1536]); w["w_o"] = np.ascontiguousarray(w_in[:, 1536:2048])
    gt = w_in[:, 2048:2064]; bg = g("b_gates")[0]
    wI = np.zeros((1024, 36), np.float32); wF = np.zeros((1024, 36), np.float32)
    bI = np.zeros((36, 1), np.float32); bF = np.zeros((36, 1), np.float32)
    wI[:, 0:4] = gt[:, 0:4]; wI[:, 32:36] = gt[:, 8:12]; wF[:, 0:4] = gt[:, 4:8]; wF[:, 32:36] = gt[:, 12:16]
    bI[0:4, 0] = bg[0:4]; bI[32:36, 0] = bg[8:12]; bF[0:4, 0] = bg[4:8]; bF[32:36, 0] = bg[12:16]
    w["w_gI"], w["w_gF"], w["b_I"], w["b_F"] = wI, wF, bI, bF
    w["w_cq"] = np.ascontiguousarray(w_in[:, 2064:2320]); w["w_ckv"] = np.ascontiguousarray(w_in[:, 2320:2448])
    kr = w_in[:, 2448:2480]
    wkr = np.zeros((1024, 96), np.float32); wkr[:, 64:96] = kr
    wkrr = np.zeros((1024, 96), np.float32); wkrr[:, 64:80] = kr[:, 16:32]; wkrr[:, 80:96] = kr[:, 0:16]
    w["w_kr"], w["w_krr"] = wkr, wkrr
    cv = g("conv_qk")[0]
    w["convq"] = np.ascontiguousarray(cv[:, 0:512].reshape(3, 4, 128).transpose(2, 1, 0))
    w["convk"] = np.ascontiguousarray(cv[:, 512:1024].reshape(3, 4, 128).transpose(2, 1, 0))
    w["g_qa"] = _pk(g("g_q_a")[0], 2); w["g_kva"] = _pk(g("g_kv_a")[0], 1)
    wqb = g("w_q_b")[0]
    w["w_qb"] = wqb
    wqbr = np.zeros_like(wqb).reshape(256, 8, 96); q3 = wqb.reshape(256, 8, 96)
    wqbr[:, :, 64:80] = q3[:, :, 80:96]; wqbr[:, :, 80:96] = q3[:, :, 64:80]
    w["w_qbr"] = np.ascontiguousarray(wqbr.reshape(256, 768))
    kv3 = g("w_kv_b")[0].reshape(128, 8, 128)
    w["w_kn"] = np.ascontiguousarray(kv3[:, :, 0:64].reshape(128, 512)); w["w_v2"] = np.ascontiguousarray(kv3[:, :, 64:128].reshape(128, 512))
    w["g_hm"] = _pk(g("g_head_mlstm")[0], 4)
    w["g_ha"] = np.ascontiguousarray(g("g_head_mla")[0].reshape(8, 64).T)
    wout = g("w_out")[0]
    w["w_outm"] = np.ascontiguousarray(wout[0:512]); w["w_outa"] = np.ascontiguousarray(wout[512:1024].reshape(8, 64, 1024))
    w["g_mx"] = _pk(g("g_mem_x")[0], 8); w["g_mkv"] = _pk(g("g_mem_kv")[0], 8)
    w["w_mq"], w["w_mk"], w["w_mv"], w["w_mo"] = g("w_mem_q")[0], g("w_mem_k")[0], g("w_mem_v")[0], g("w_mem_o")[0]
    w["g_ffnp"] = _pk(g("g_ffn")[0], 8); w["g_ffn"] = g("g_ffn")[0].reshape(1, 1024); w["g_fin"] = g("g_final").reshape(1, 1024)
    w["w_r"] = g("w_router")[0]
    w["w_eg"], w["w_eu"], w["w_ed"] = g("w_exp_gate")[0], g("w_exp_up")[0], g("w_exp_down")[0]
    return w


def core_inputs(I, b, shared):
    m = dict(shared)
    m["x"] = np.ascontiguousarray(np.asarray(I["x"], np.float32)[b])
    m["mem"] = np.ascontiguousarray(np.asarray(I["mem"], np.float32)[b])
    m["pos"] = np.ascontiguousarray(np.tile(np.asarray(I["positions"], np.int32)[b][None, :], (32, 1)))
    return m


_CACHE = {}


def kernel(**inputs):
    if "nc" not in _CACHE:
        _CACHE["nc"] = build()[0]
    nc = _CACHE["nc"]
    shared = const_inputs()
    shared.update(weight_inputs(inputs))
    in_maps = [core_inputs(inputs, c % 4, shared) for c in range(8)]
    res = run_bass_kernel_spmd(nc, in_maps, core_ids=list(range(8)))
    return np.stack([np.asarray(res.results[b]["out"], np.float32) for b in range(4)], axis=0)
```

```python
from contextlib import ExitStack
import numpy as np
import ml_dtypes
import concourse.bass as bass
import concourse.mybir as mybir
from concourse.bass_utils import run_bass_kernel_spmd

F32 = mybir.dt.float32
BF16 = mybir.dt.bfloat16
I32 = mybir.dt.int32
AF = mybir.ActivationFunctionType
ALU = mybir.AluOpType
AX = mybir.AxisListType

SEM_CH = 1000
NDMA_SEM = 12
ARENA_WORDS = 53000
EPS = 1e-6


class Op:
    __slots__ = ("eng", "fn", "deps", "signal", "semval", "idx", "dma", "dsem", "dval", "phase")


class Prog:
    ENGS = ("pe", "act", "dve", "pool", "sp")

    def __init__(self, nc):
        self.nc = nc
        self.streams = {e: [] for e in self.ENGS}
        self.last_w = {}
        self.readers = {}
        self.ndma = {"sp": 0, "pool": 0}
        self.dma_ops = {"sp": [], "pool": []}
        self.seen = {e: {p: -1 for p in self.ENGS} for e in self.ENGS}
        self.seen_dma = {e: {} for e in self.ENGS}
        self.stack = ExitStack()
        self.n_ops = 0
        self.arena = self.stack.enter_context(nc.sbuf_tensor("arena", [128, ARENA_WORDS], F32))
        self.top = 0
        self.marks = []
        self.peak = 0
        self.phase = "p0"
        self.scopes = False

    def alloc(self, shape, dtype):
        esz = 2 if dtype == BF16 else 4
        n = 1
        for s in shape[1:]:
            n *= s
        words = (n * esz + 3) // 4
        off = self.top
        self.top += (words + 7) // 8 * 8
        self.peak = max(self.peak, self.top)
        assert self.top <= ARENA_WORDS, f"SBUF arena overflow {self.top}"
        ap = self.arena[0:shape[0], off:off + words]
        if dtype != F32:
            ap = ap.bitcast(dtype)
        if ap.shape[1] != n:
            ap = ap[:, 0:n]
        if len(shape) == 3:
            ap = ap.rearrange("p (a b) -> p a b", a=shape[1])
        elif len(shape) == 4:
            ap = ap.rearrange("p (a b c) -> p a b c", a=shape[1], b=shape[2])
        return ap

    def mark(self):
        self.marks.append(self.top)

    def release(self):
        self.barrier()
        self.top = self.marks.pop()

    def psum(self, name, shape, dtype=F32):
        return self.stack.enter_context(self.nc.psum_tensor(name, list(shape), dtype))

    def op(self, eng, fn, reads=(), writes=(), dma=False, extra=()):
        xr = [k for k in reads if isinstance(k, tuple) and k[0] == "B"]
        if xr:
            writes = list(writes) + [k for k in xr if k not in writes]
        o = Op()
        o.eng, o.fn, o.dma, o.signal, o.semval = eng, fn, dma, False, 0
        o.dsem = o.dval = None
        o.phase = self.phase
        stream = self.streams[eng]
        o.idx = len(stream)
        deps = {}
        cand = list(extra)
        for k in reads:
            w = self.last_w.get(k)
            if w is not None:
                cand.append(w)
        for k in writes:
            w = self.last_w.get(k)
            if w is not None:
                cand.append(w)
            cand.extend(self.readers.get(k, ()))
        if dma:
            q = eng
            n = self.ndma[q]
            self.ndma[q] = n + 1
            o.dsem = n % NDMA_SEM
            o.dval = 16 * (n // NDMA_SEM + 1)
            if n >= NDMA_SEM:
                cand.append(self.dma_ops[q][n - NDMA_SEM])
            self.dma_ops[q].append(o)
        for d in cand:
            if d is o or d.fn is None:
                continue
            if d.dma:
                key = (d.eng, d.dsem)
                if self.seen_dma[eng].get(key, 0) >= d.dval:
                    continue
                self.seen_dma[eng][key] = d.dval
                deps[("dma",) + key] = d
            else:
                if d.eng == "pe" and eng == "pe" and not dma:
                    continue
                if self.seen[eng][d.eng] >= d.idx:
                    continue
                cur = deps.get(("c", d.eng))
                if cur is None or cur.idx < d.idx:
                    deps[("c", d.eng)] = d
        for k, d in deps.items():
            if k[0] == "c":
                self.seen[eng][d.eng] = d.idx
                d.signal = True
        o.deps = list(deps.values())
        stream.append(o)
        self.n_ops += 1
        for k in reads:
            lst = self.readers.setdefault(k, [])
            if not dma:
                lst[:] = [r for r in lst if r.dma or r.eng != eng]
            lst.append(o)
        for k in writes:
            self.last_w[k] = o
            self.readers[k] = []
        return o

    def pe(self, fn, reads=(), writes=()):
        return self.op("pe", fn, reads, writes)

    def act(self, fn, reads=(), writes=()):
        return self.op("act", fn, reads, writes)

    def dve(self, fn, reads=(), writes=()):
        return self.op("dve", fn, reads, writes)

    def pool(self, fn, reads=(), writes=()):
        return self.op("pool", fn, reads, writes)

    def dma(self, out, in_, reads=(), writes=(), q="sp", **kw):
        return self.op(q, lambda e: e.dma_start(out=out, in_=in_, **kw), reads, writes, dma=True)

    def barrier(self):
        allops = set()
        for w in self.last_w.values():
            allops.add(w)
        for lst in self.readers.values():
            allops.update(lst)
        for q in self.dma_ops:
            allops.update(self.dma_ops[q][-NDMA_SEM:])
        allops = [o for o in allops if o.fn is not None]
        for e in self.ENGS:
            self.op(e, None, extra=allops)
        self.last_w.clear()
        self.readers.clear()

    def emit(self):
        nc = self.nc
        st = self.stack
        csem = {}
        for e in ("pe", "act", "dve", "pool"):
            cnt = 0
            for o in self.streams[e]:
                if o.signal and not o.dma:
                    cnt += 1
                    o.semval = cnt
            nsem = max(1, (cnt + SEM_CH - 1) // SEM_CH)
            csem[e] = [st.enter_context(nc.semaphore(f"s_{e}{i}")) for i in range(nsem)]
        dsem = {}
        for q in ("sp", "pool"):
            dsem[q] = [st.enter_context(nc.semaphore(f"d_{q}{i}")) for i in range(NDMA_SEM)]

        def emit_stream(ename, eng):
            if self.scopes:
                cur = None
                cm = None
                for o in self.streams[ename]:
                    if o.phase != cur:
                        if cm is not None:
                            cm.__exit__(None, None, None)
                        cur = o.phase
                        cm = nc.named_scope(cur)
                        cm.__enter__()
                    emit_one(ename, eng, o)
                if cm is not None:
                    cm.__exit__(None, None, None)
                return
            for o in self.streams[ename]:
                emit_one(ename, eng, o)

        def emit_one(ename, eng, o):
            if True:
                for d in o.deps:
                    if d.dma:
                        eng.wait_ge(dsem[d.eng][d.dsem], d.dval)
                    else:
                        v = d.semval - 1
                        eng.wait_ge(csem[d.eng][v // SEM_CH], v % SEM_CH + 1)
                if o.fn is None:
                    return
                ins = o.fn(eng)
                if o.dma:
                    ins.then_inc(dsem[ename][o.dsem], 16)
                elif o.signal:
                    v = o.semval - 1
                    ins.then_inc(csem[ename][v // SEM_CH], 1)

        with nc.Block() as block:
            @block.tensor
            def _(eng):
                emit_stream("pe", eng)

            @block.scalar
            def _(eng):
                emit_stream("act", eng)

            @block.vector
            def _(eng):
                emit_stream("dve", eng)

            @block.gpsimd
            def _(eng):
                emit_stream("pool", eng)

            @block.sync
            def _(eng):
                emit_stream("sp", eng)


LANE_PART = [0, 1, 2, 3, 32, 33, 34, 35]
NHEADS_MLA = 8
PV_NOACC = False
OB_BASE = 6
SCOPES = False


def build(stop=99, taps=False):
    nc = bass.Bass("TRN2", target_bir_lowering=False)
    P = Prog(nc)
    P.scopes = SCOPES

    def din(n, shp, dt=F32):
        return nc.dram_tensor(n, list(shp), dt, kind="ExternalInput")

    def dscr(n, shp, dt):
        return nc.dram_tensor(n, list(shp), dt, kind="Internal")

    x = din("x", [4096, 1024]); mem = din("mem", [256, 1024]); pos = din("pos", [32, 4096], I32)
    c_identb = din("c_identb", [128, 128], BF16); c_identf = din("c_identf", [128, 128])
    c_onesb = din("c_onesb", [128, 128], BF16)
    c_bmask = din("c_bmask", [128, 2, 128]); c_sel36 = din("c_sel36", [36, 8, 128])
    c_nsel = din("c_nsel", [128, 64]); c_invf = din("c_invf", [128, 1])
    c_reset = din("c_reset", [36, 4096])
    c_iota = din("c_iota", [128, 512]); c_tokid = din("c_tokid", [128, 32]); c_U = din("c_U", [128, 128])
    c_onesf = din("c_onesf", [128, 128])
    gmix_d = din("gmix", [128, 8])
    wq_d = din("w_q", [1024, 512]); wk_d = din("w_k", [1024, 512]); wv_d = din("w_v", [1024, 512]); wo_d = din("w_o", [1024, 512])
    wgI_d = din("w_gI", [1024, 36]); wgF_d = din("w_gF", [1024, 36]); bI_d = din("b_I", [36, 1]); bF_d = din("b_F", [36, 1])
    wcq_d = din("w_cq", [1024, 256]); wckv_d = din("w_ckv", [1024, 128])
    wkr_d = din("w_kr", [1024, 96]); wkrr_d = din("w_krr", [1024, 96])
    convq_d = din("convq", [128, 4, 3]); convk_d = din("convk", [128, 4, 3])
    gqa_d = din("g_qa", [128, 2]); gkva_d = din("g_kva", [128, 1])
    wqb_d = din("w_qb", [256, 768]); wqbr_d = din("w_qbr", [256, 768])
    wkn_d = din("w_kn", [128, 512]); wv2_d = din("w_v2", [128, 512])
    ghm_d = din("g_hm", [128, 4]); gha_d = din("g_ha", [64, 8])
    woutm_d = din("w_outm", [512, 1024]); wouta_d = din("w_outa", [8, 64, 1024])
    gmx_d = din("g_mx", [128, 8]); gmkv_d = din("g_mkv", [128, 8])
    wmq_d = din("w_mq", [1024, 1024]); wmk_d = din("w_mk", [1024, 1024]); wmv_d = din("w_mv", [1024, 1024]); wmo_d = din("w_mo", [1024, 1024])
    gffn_d = din("g_ffn", [1, 1024]); gfin_d = din("g_fin", [1, 1024]); gffnp_d = din("g_ffnp", [128, 8]); c_jp = din("c_jp", [128, 32, 2])
    wr_d = din("w_r", [1024, 16])
    if stop > 4:
        weg_d = din("w_eg", [16, 1024, 2048]); weu_d = din("w_eu", [16, 1024, 2048]); wed_d = din("w_ed", [16, 2048, 1024])
    else:
        weg_d = din("w_eg", [1, 1, 1]); weu_d = din("w_eu", [1, 1, 1]); wed_d = din("w_ed", [1, 1, 1])
    out = nc.dram_tensor("out", [4096, 1024], F32, kind="ExternalOutput")
    ymT_d = dscr("ymT_d", [512, 4096], BF16)
    yaT_d = dscr("yaT_d", [8, 64, 4096], BF16)
    acc_d = dscr("acc_d", [4096, 1024], F32)
    h_d = dscr("h_d", [4096, 1024], BF16)
    tapd = {}

    def tap(name, ap_sb, shape, dt, reads):
        if not taps:
            return
        t = nc.dram_tensor("tap_" + name, list(shape), dt, kind="ExternalOutput")
        tapd[name] = t
        P.dma(t.ap(), ap_sb, reads=reads, writes=[("tap", name)])

    def mm(o, lhsT, rhs, st, sp, R, W):
        P.pe(lambda e: e.matmul(o, lhsT=lhsT, rhs=rhs, start=st, stop=sp), R, W)

    def tr(o, i, idn, R, W):
        P.pe(lambda e: e.transpose(out=o, in_=i, identity=idn), R, W)

    def actf(o, i, func, R, W, bias=None, scale=None, accum=None):
        kw = {}
        if bias is not None:
            kw["bias"] = bias
        if scale is not None:
            kw["scale"] = scale
        if accum is not None:
            kw["accum_out"] = accum
        P.act(lambda e: e.activation(out=o, in_=i, func=func, **kw), R, W)

    def ts(eng, o, i0, s1, s2, op0, op1, R, W):
        if op1 is None:
            P.op(eng, lambda e: e.tensor_scalar(out=o, in0=i0, scalar1=s1, scalar2=None, op0=op0), R, W)
        else:
            P.op(eng, lambda e: e.tensor_scalar(out=o, in0=i0, scalar1=s1, scalar2=s2, op0=op0, op1=op1), R, W)

    def tt(eng, o, i0, i1, op, R, W):
        P.op(eng, lambda e: e.tensor_tensor(out=o, in0=i0, in1=i1, op=op), R, W)

    def stt(eng, o, i0, sc, i1, op0, op1, R, W):
        P.op(eng, lambda e: e.scalar_tensor_tensor(out=o, in0=i0, scalar=sc, in1=i1, op0=op0, op1=op1), R, W)

    def cp(eng, o, i, R, W):
        P.op(eng, lambda e: e.tensor_copy(out=o, in_=i), R, W)

    def recip(o, i, R, W):
        P.dve(lambda e: e.reciprocal(out=o, in_=i), R, W)

    def memset(eng, o, v, W):
        P.op(eng, lambda e: e.memset(o, v), (), W)

    def load(dst, src, key, q="sp"):
        P.dma(dst, src, writes=[key], q=q)

    TP = [P.psum(f"pp{i}", [128, 1024], F32) for i in range(4)]
    B = []
    for t_ in TP:
        B += [t_[:, 0:512], t_[:, 512:1024]]
    Bb = [b.bitcast(BF16) for b in B]
    BK = [("B", i) for i in range(8)]

    identb = P.alloc([128, 128], BF16); load(identb, c_identb.ap(), "identb")
    identf = P.alloc([128, 128], F32); load(identf, c_identf.ap(), "identf")
    onesb = P.alloc([128, 128], BF16); load(onesb, c_onesb.ap(), "onesb")

    def rms_rstd(ssum, rs, n, key):
        actf(rs, ssum, AF.Sqrt, [key + "ss"], [key + "rs"], bias=EPS, scale=1.0 / n)
        recip(rs, rs, [key + "rs"], [key + "rs"])

    xnT = P.alloc([128, 8, 4096], BF16)
    P.mark()
    gmix = P.alloc([128, 8], F32); load(gmix, gmix_d.ap(), "gmix")
    xs = [P.alloc([128, 4, 1024], F32) for _ in range(2)]
    xnb = [P.alloc([128, 4, 1024], BF16) for _ in range(2)]
    junk = P.alloc([128, 1024], BF16)
    ss = P.alloc([128, 32], F32); rs = P.alloc([128, 32], F32)
    for s in range(8):
        b = s % 2
        P.dma(xs[b], x[s * 512:(s + 1) * 512, :].rearrange("(j p) d -> p j d", p=128), writes=[("xs", b)])
        for j in range(4):
            t = s * 4 + j
            actf(junk, xs[b][:, j, :], AF.Square, [("xs", b)], ["junk", ("ss", t)], accum=ss[:, t:t + 1])
            actf(rs[:, t:t + 1], ss[:, t:t + 1], AF.Sqrt, [("ss", t)], [("rs", t)], bias=EPS, scale=1.0 / 1024)
            recip(rs[:, t:t + 1], rs[:, t:t + 1], [("rs", t)], [("rs", t)])
            ts("dve", xnb[b][:, j, :], xs[b][:, j, :], rs[:, t:t + 1], None, ALU.mult, None, [("xs", b), ("rs", t)], [("xnb", b, j)])
        for kc in range(8):
            bk = 6 + kc % 2
            for j in range(4):
                tr(Bb[bk][:, j * 128:(j + 1) * 128], xnb[b][:, j, kc * 128:(kc + 1) * 128], identb, [("xnb", b, j), "identb"], [BK[bk]])
            if kc % 2 == 0:
                ts("dve", xnT[:, kc, s * 512:(s + 1) * 512], Bb[bk][:, 0:512], gmix[:, kc:kc + 1], None, ALU.mult, None, [BK[bk], "gmix"], [("xnT", s)])
            else:
                actf(xnT[:, kc, s * 512:(s + 1) * 512], Bb[bk][:, 0:512], AF.Copy, [BK[bk], "gmix"], [("xnT", s)], scale=gmix[:, kc:kc + 1])
    XNT = [("xnT", s) for s in range(8)]
    tap("xnT", xnT, [128, 8, 4096], BF16, XNT)
    P.release()
    if stop <= 0:
        return finish(nc, P, out, tapd)

    P.phase = "p1a"
    P.mark()
    WS128 = P.alloc([128, 32, 36], F32)
    LB64 = P.alloc([64, 64, 36], F32)
    DEC = P.alloc([128, 8, 64], F32)
    P.mark()
    wgI = P.alloc([128, 8, 36], BF16); load(wgI, wgI_d.ap().rearrange("(k p) n -> p k n", p=128), "wgI", q="pool")
    wgF = P.alloc([128, 8, 36], BF16); load(wgF, wgF_d.ap().rearrange("(k p) n -> p k n", p=128), "wgF", q="pool")
    bI = P.alloc([36, 1], F32); load(bI, bI_d.ap(), "bI")
    nbF = P.alloc([36, 1], F32); load(nbF, bF_d.ap(), "nbF")
    ts("dve", nbF, nbF, -1.0, None, ALU.mult, None, ["nbF"], ["nbF"])
    reset = P.alloc([36, 4096], F32); load(reset, c_reset.ap(), "reset")
    sel36 = P.alloc([36, 8, 128], F32); load(sel36, c_sel36.ap(), "sel36")
    IT = P.alloc([36, 4096], F32); SP = P.alloc([36, 4096], F32); NB = P.alloc([36, 4096], F32)
    Gt = P.alloc([36, 64], F32); Gn = P.alloc([36, 64], F32); amax = P.alloc([36, 64], F32)
    Mm = P.alloc([36, 64], F32); Mend = P.alloc([36, 64], F32); MP = P.alloc([36, 64], F32); DECs = P.alloc([36, 64], F32)
    for s in range(8):
        sl = slice(s * 512, (s + 1) * 512)
        for kc in range(8):
            mm(B[0][0:36, :], wgI[:, kc, :], xnT[:, kc, sl], kc == 0, kc == 7, ["wgI"] + XNT, [BK[0]])
        actf(IT[:, sl], B[0][0:36, :], AF.Identity, [BK[0], "bI"], ["IT"], bias=bI[:, 0:1])
        for kc in range(8):
            mm(B[1][0:36, :], wgF[:, kc, :], xnT[:, kc, sl], kc == 0, kc == 7, ["wgF"] + XNT, [BK[1]])
        actf(SP[:, sl], B[1][0:36, :], AF.Exp, [BK[1], "nbF"], ["SP"], bias=nbF[:, 0:1], scale=-1.0)
    actf(SP, SP, AF.Ln, ["SP"], ["SP"], bias=1.0)
    P.dve(lambda e: e.tensor_tensor_scan(out=NB, data0=reset, data1=SP, initial=0.0, op0=ALU.mult, op1=ALU.add), ["reset", "SP"], ["NB"])
    NB3 = NB.rearrange("p (c t) -> p c t", t=64); IT3 = IT.rearrange("p (c t) -> p c t", t=64); SP3 = SP.rearrange("p (c t) -> p c t", t=64)
    cp("dve", Gt, NB3[:, :, 63], ["NB"], ["Gt"])
    Gt3 = Gt.rearrange("p (c o) -> p c o", o=1)
    tt("dve", NB3[32:36], Gt3[32:36].to_broadcast([4, 64, 64]), NB3[32:36], ALU.subtract, ["Gt", "NB"], ["NB"])
    tt("dve", NB3[32:36], NB3[32:36], SP3[32:36], ALU.add, ["NB", "SP"], ["NB"])
    tt("dve", IT, IT, NB, ALU.add, ["IT", "NB"], ["IT"])
    P.dve(lambda e: e.tensor_reduce(out=amax, in_=IT3, axis=AX.X, op=ALU.max), ["IT"], ["amax"])
    ts("dve", Gn, Gt, -1.0, None, ALU.mult, None, ["Gt"], ["Gn"])
    P.dve(lambda e: e.tensor_tensor_scan(out=Mm[0:4, :], data0=amax[0:4, :], data1=Gn[0:4, :], initial=0.0, op0=ALU.max, op1=ALU.add), ["amax", "Gn"], ["Mm"])
    P.dve(lambda e: e.tensor_tensor_scan(out=Mm[32:36, ::-1], data0=amax[32:36, ::-1], data1=Gn[32:36, ::-1], initial=0.0, op0=ALU.max, op1=ALU.add), ["amax", "Gn"], ["Mm"])
    tt("dve", Mend, Mm, Gt, ALU.add, ["Mm", "Gt"], ["Mend"])
    memset("dve", MP, 0.0, ["MP"])
    cp("dve", MP[0:4, 1:64], Mm[0:4, 0:63], ["Mm", "MP"], ["MP"])
    cp("dve", MP[32:36, 0:63], Mm[32:36, 1:64], ["Mm", "MP"], ["MP"])
    tt("dve", DECs, MP, Mend, ALU.subtract, ["MP", "Mend"], ["DECs"])
    actf(DECs, DECs, AF.Exp, ["DECs"], ["DECs"])
    Mend3 = Mend.rearrange("p (c o) -> p c o", o=1).to_broadcast([36, 64, 64])
    tt("dve", IT3, IT3, Mend3, ALU.subtract, ["IT", "Mend"], ["IT"])
    actf(IT, IT, AF.Exp, ["IT"], ["IT"])
    tt("dve", NB3, NB3, Mend3, ALU.subtract, ["NB", "Mend"], ["NB"])
    actf(NB, NB, AF.Exp, ["NB"], ["NB"])
    for g8 in range(4):
        bk = g8 % 2
        for jj in range(8):
            j = g8 * 8 + jj
            tr(B[bk][:, jj * 36:(jj + 1) * 36], IT[0:36, j * 128:(j + 1) * 128], identf[0:36, 0:36], ["IT", "identf"], [BK[bk]])
        cp("dve", WS128[:, g8 * 8:(g8 + 1) * 8, :], B[bk][:, 0:288].rearrange("p (a b) -> p a b", a=8), [BK[bk]], ["WS128"])
    for g8 in range(8):
        bk = 2 + g8 % 2
        for cc in range(8):
            c = g8 * 8 + cc
            tr(B[bk][0:64, cc * 36:(cc + 1) * 36], NB[0:36, c * 64:(c + 1) * 64], identf[0:36, 0:36], ["NB", "identf"], [BK[bk]])
        cp("dve", LB64[:, g8 * 8:(g8 + 1) * 8, :], B[bk][0:64, 0:288].rearrange("p (a b) -> p a b", a=8), [BK[bk]], ["LB64"])
    for l in range(8):
        mm(B[4][:, l * 64:(l + 1) * 64], sel36[:, l, :], DECs, True, True, ["sel36", "DECs"], [BK[4]])
    cp("dve", DEC, B[4][:, 0:512].rearrange("p (a b) -> p a b", a=8), [BK[4]], ["DEC"])
    tap("WS128", WS128, [128, 32, 36], F32, ["WS128"])
    tap("LB64", LB64, [64, 64, 36], F32, ["LB64"])
    tap("DEC", DEC, [128, 8, 64], F32, ["DEC"])
    P.release()
    if stop <= 1:
        return finish(nc, P, out, tapd)

    P.phase = "p1b"
    P.mark()
    wq = P.alloc([128, 8, 128], BF16); wk = P.alloc([128, 8, 128], BF16)
    wv = P.alloc([128, 8, 128], BF16); wo = P.alloc([128, 8, 128], BF16)
    convq = P.alloc([128, 4, 3], F32); load(convq, convq_d.ap(), "convq")
    convk = P.alloc([128, 4, 3], F32); load(convk, convk_d.ap(), "convk")
    ghm = P.alloc([128, 4], F32); load(ghm, ghm_d.ap(), "ghm")
    bmask = P.alloc([128, 2, 128], F32); load(bmask, c_bmask.ap(), "bmask")
    preq = P.alloc([128, 4098], BF16); prek = P.alloc([128, 4098], BF16)
    ctmp = [P.alloc([128, 512], F32) for _ in range(2)]
    qT = P.alloc([128, 4096], BF16); kT = P.alloc([128, 4096], BF16)
    k_tm = P.alloc([128, 32, 128], BF16); v_tm = P.alloc([128, 32, 129], BF16)
    sigT = P.alloc([128, 4096], BF16)
    PT = P.alloc([128, 2, 32, 128], BF16)
    hacc = P.alloc([64, 64, 128], F32)
    hn = [P.alloc([64, 8, 128], BF16) for _ in range(2)]
    ymT = qT
    C32 = [[P.alloc([128, 129], F32) for _ in range(2)] for _ in range(2)]
    Cbf = [[P.alloc([128, 129], BF16) for _ in range(2)] for _ in range(2)]
    vp = [P.alloc([128, 129], BF16) for _ in range(4)]
    dtmp = [P.alloc([64, 2], F32) for _ in range(4)]
    hsq = P.alloc([64, 128], BF16)
    hss = P.alloc([64, 64], F32)
    memset("dve", v_tm[:, :, 128:129], 1.0, ["v_tm"])
    for h in range(4):
        hs = slice(0, 128)
        P.phase = "p1b_proj"
        for wt, wd, wkey_ in ((wq, wq_d, "wq"), (wk, wk_d, "wk"), (wv, wv_d, "wv"), (wo, wo_d, "wo")):
            P.dma(wt, wd[:, h * 128:(h + 1) * 128].rearrange("(k p) n -> p k n", p=128), writes=[wkey_], q="pool")
        qk = (("convq", wq, "wq", convq, qT, "qT", preq, "preq"), ("convk", wk, "wk", convk, kT, "kT", prek, "prek"))
        for cwk, wmat, wkey, cw, dst, dkey, pre, pk in qk:
            if h == 0:
                memset("dve", pre[:, 0:1], 0.0, [pk]); memset("dve", pre[:, 4097:4098], 0.0, [pk])
            for s in range(8):
                sl = slice(s * 512, (s + 1) * 512)
                bk = s % 2
                for kc in range(8):
                    mm(B[bk][:, :], wmat[:, kc, hs], xnT[:, kc, sl], kc == 0, kc == 7, [wkey] + XNT, [BK[bk]])
                actf(pre[:, 1 + s * 512:1 + (s + 1) * 512], B[bk][:, :], AF.Copy, [BK[bk]], [pk])
        for s in range(8):
            sl = slice(s * 512, (s + 1) * 512)
            bk = s % 2
            for kc in range(8):
                mm(B[bk][:, :], wo[:, kc, hs], xnT[:, kc, sl], kc == 0, kc == 7, ["wo"] + XNT, [BK[bk]])
            actf(sigT[:, sl], B[bk][:, :], AF.Sigmoid, [BK[bk]], ["sigT"])
        for cwk, wmat, wkey, cw, dst, dkey, pre, pk in qk:
            for s in range(8):
                c0 = s * 512
                tb = ctmp[s % 2]
                tk = ("ctmp", s % 2)
                ts("dve", tb, pre[:, c0:c0 + 512], cw[:, h, 0:1], None, ALU.mult, None, [pk, cwk], [tk])
                stt("dve", tb, pre[:, c0 + 1:c0 + 513], cw[:, h, 1:2], tb, ALU.mult, ALU.add, [pk, tk], [tk])
                stt("dve", tb, pre[:, c0 + 2:c0 + 514], cw[:, h, 2:3], tb, ALU.mult, ALU.add, [pk, tk], [tk])
                actf(dst[:, c0:c0 + 512], tb, AF.Silu, [tk], [dkey])
        for j in range(32):
            bk = 2 + j % 2
            for kc in range(8):
                mm(B[bk][:, 0:128], xnT[:, kc, j * 128:(j + 1) * 128], wv[:, kc, hs], kc == 0, kc == 7, ["wv"] + XNT, [BK[bk]])
            cp("dve", v_tm[:, j, 0:128], B[bk][:, 0:128], [BK[bk]], ["v_tm"])
        for j in range(32):
            bk = 6 + j % 2
            tr(Bb[bk][:, 0:128], kT[:, j * 128:(j + 1) * 128], identb, ["kT", "identb"], [BK[bk]])
            ts("dve", k_tm[:, j, :], Bb[bk][:, 0:128], 128 ** -0.5, None, ALU.mult, None, [BK[bk]], ["k_tm"])
        P.phase = "p1b_scan"
        for j in range(32):
            bk = j % 2
            mm(B[bk][:, 0:128], kT[:, j * 128:(j + 1) * 128], qT[:, j * 128:(j + 1) * 128], True, True, ["kT", "qT"], [BK[bk]])
            for d in range(2):
                lc = h + 32 * d
                stt("dve", PT[:, d, j, :], B[bk][:, 0:128], WS128[:, j, lc:lc + 1], bmask[:, d, :], ALU.mult, ALU.mult,
                    [BK[bk], "WS128", "bmask"], [("PT", d, j)])
        for d in range(2):
            memset("dve", C32[d][0], 0.0, [("C32", d, 0)])
        units = []
        for i in range(64):
            for d in range(2):
                units.append((i, d, i if d == 0 else 63 - i))

        def emit_dc(n):
            i, d, c = units[n]
            j, r = c // 2, c % 2
            rs_ = slice(r * 64, (r + 1) * 64)
            lc = h + 32 * d
            vpb = vp[n % 4]; vk = ("vp", n % 4)
            actf(vpb[rs_, :], v_tm[rs_, j, :], AF.Copy, ["v_tm", "WS128"], [vk], scale=WS128[rs_, j, lc:lc + 1])
            db = 4 + n % 2
            mm(B[db][:, 0:129], k_tm[rs_, j, :], vpb[rs_, :], True, True, ["k_tm", vk], [BK[db]])

        def emit_norm(n):
            i, d, c = units[n]
            lc = h + 32 * d
            nb_ = n % 4
            dt_ = dtmp[n % 4]; dk = ("dtmp", n % 4)
            actf(dt_[:, 0:1], B[nb_][0:64, 128:129], AF.Abs, [BK[nb_]], [dk])
            ts("dve", dt_[:, 0:1], dt_[:, 0:1], LB64[:, c, lc:lc + 1], None, ALU.max, None, [dk, "LB64"], [dk])
            recip(dt_[:, 1:2], dt_[:, 0:1], [dk], [dk])
            if i < 32:
                ts("dve", hacc[:, c, :], B[nb_][0:64, 0:128], dt_[:, 1:2], None, ALU.mult, None, [BK[nb_], dk], [("hacc", c)])
            else:
                stt("dve", hacc[:, c, :], B[nb_][0:64, 0:128], dt_[:, 1:2], hacc[:, c, :], ALU.mult, ALU.add, [BK[nb_], dk, ("hacc", c)], [("hacc", c)])

        emit_dc(0)
        for n in range(128):
            i, d, c = units[n]
            j, r = c // 2, c % 2
            rs_ = slice(r * 64, (r + 1) * 64)
            l = h + 4 * d
            if n >= 4:
                emit_norm(n - 4)
            if n + 1 < 128:
                emit_dc(n + 1)
            cur, nxt = C32[d][i % 2], C32[d][(i + 1) % 2]
            ck, nk = ("C32", d, i % 2), ("C32", d, (i + 1) % 2)
            cb = Cbf[d][i % 2]; cbk = ("Cbf", d, i % 2)
            actf(cb, cur, AF.Copy, [ck, "DEC"], [cbk], scale=DEC[:, l, c:c + 1])
            db = 4 + n % 2
            stt("dve", nxt, cur, DEC[:, l, c:c + 1], B[db][:, 0:129], ALU.mult, ALU.add, [ck, "DEC", BK[db]], [nk])
            nb_ = n % 4
            mm(B[nb_][0:64, 0:129], qT[:, c * 64:(c + 1) * 64], cb, True, False, ["qT", cbk], [BK[nb_]])
            mm(B[nb_][0:64, 0:129], PT[rs_, d, j, rs_], v_tm[rs_, j, :], False, True, [("PT", d, j), "v_tm"], [BK[nb_]])
        for n in range(124, 128):
            emit_norm(n)
        P.phase = "p1b_post"
        HA = [("hacc", c) for c in range(64)]
        for c in range(64):
            actf(hsq, hacc[:, c, :], AF.Square, [("hacc", c)], ["hsq", "hssss"], accum=hss[:, c:c + 1])
        rms_rstd(hss, hss, 128, "hss")
        for g8 in range(8):
            hb = hn[g8 % 2]; hk = ("hn", g8 % 2)
            for cc in range(8):
                c = g8 * 8 + cc
                ts("dve", hb[:, cc, :], hacc[:, c, :], hss[:, c:c + 1], None, ALU.mult, None, [("hacc", c), "hssrs"], [hk])
            bk = 6 + g8 % 2
            for cc in range(8):
                tr(Bb[bk][:, cc * 64:(cc + 1) * 64], hb[:, cc, :], identb[0:64, 0:64], [hk, "identb"], [BK[bk]])
            sl = slice(g8 * 512, (g8 + 1) * 512)
            stt("dve", ymT[:, sl], Bb[bk][:, 0:512], ghm[:, h:h + 1], sigT[:, sl], ALU.mult, ALU.mult, [BK[bk], "ghm", "sigT"], ["qT"])
        P.dma(ymT_d[h * 128:(h + 1) * 128, :], ymT, reads=["qT"], writes=[("ymT_d", h)])
    P.release()
    P.release()
    if stop <= 2:
        return finish(nc, P, out, tapd, extra_out=[("ymT_d", ymT_d, [512, 4096], BF16)])
    P.phase = "p1c"
    P.mark()
    sinT = P.alloc([96, 4096], BF16); cosT = P.alloc([96, 4096], BF16)
    cqnT = P.alloc([128, 2, 4096], BF16); ckvnT = P.alloc([128, 4096], BF16); krT = P.alloc([96, 4096], BF16)
    R_ = slice(64, 96)
    P.mark()
    posi = P.alloc([96, 4096], I32); load(posi[R_, :], pos.ap(), "posi")
    invf = P.alloc([128, 1], F32); load(invf, c_invf.ap(), "invf")
    ang = P.alloc([96, 4096], F32); kf = P.alloc([96, 4096], F32); fw = P.alloc([96, 4096], F32)
    cp("dve", ang[R_], posi[R_], ["posi"], ["ang"])
    ts("dve", ang[R_], ang[R_], invf[R_, 0:1], 1.0 / (2 * np.pi), ALU.mult, ALU.mult, ["ang", "invf"], ["ang"])
    cp("dve", posi[R_], ang[R_], ["ang"], ["posi"])
    cp("dve", kf[R_], posi[R_], ["posi"], ["kf"])
    tt("dve", ang[R_], ang[R_], kf[R_], ALU.subtract, ["ang", "kf"], ["ang"])
    for dstT, shift, dk_ in ((sinT, 0.0, "sinT"), (cosT, 0.25, "cosT")):
        ts("dve", fw[R_], ang[R_], shift, None, ALU.add, None, ["ang"], ["fw"])
        ts("dve", kf[R_], fw[R_], 0.5, None, ALU.is_gt, None, ["fw"], ["kf"])
        tt("dve", fw[R_], fw[R_], kf[R_], ALU.subtract, ["fw", "kf"], ["fw"])
        ts("dve", kf[R_], fw[R_], -0.5, None, ALU.is_lt, None, ["fw"], ["kf"])
        tt("dve", fw[R_], fw[R_], kf[R_], ALU.add, ["fw", "kf"], ["fw"])
        actf(dstT[R_], fw[R_], AF.Sin, ["fw"], [dk_], scale=6.283185)
    P.release()
    if stop <= 2.3:
        tap("sinT", sinT[64:96], [32, 4096], BF16, ["sinT"]); tap("cosT", cosT[64:96], [32, 4096], BF16, ["cosT"])
        return finish(nc, P, out, tapd)
    def wload(shape, src, key):
        t_ = P.alloc(shape, BF16); load(t_, src, key, q="pool"); return t_
    wcq = wload([128, 8, 256], wcq_d.ap().rearrange("(k p) n -> p k n", p=128), "wcq")
    wckv = wload([128, 8, 128], wckv_d.ap().rearrange("(k p) n -> p k n", p=128), "wckv")
    wkr = wload([128, 8, 96], wkr_d.ap().rearrange("(k p) n -> p k n", p=128), "wkr")
    wkrr = wload([128, 8, 96], wkrr_d.ap().rearrange("(k p) n -> p k n", p=128), "wkrr")
    wqb = wload([128, 2, 768], wqb_d.ap().rearrange("(k p) n -> p k n", p=128), "wqb")
    wqbr = wload([128, 2, 768], wqbr_d.ap().rearrange("(k p) n -> p k n", p=128), "wqbr")
    wkn = wload([128, 512], wkn_d.ap(), "wkn"); wv2 = wload([128, 512], wv2_d.ap(), "wv2")
    ts("dve", wkrr[:, :, 64:80], wkrr[:, :, 64:80], -1.0, None, ALU.mult, None, ["wkrr"], ["wkrr"])
    wqbr4 = wqbr.rearrange("p k (h c) -> p k h c", c=96)
    for k2 in range(2):
        ts("dve", wqbr4[:, k2, :, 64:80], wqbr4[:, k2, :, 64:80], -1.0, None, ALU.mult, None, ["wqbr"], ["wqbr"])
    gqa = P.alloc([128, 2], F32); load(gqa, gqa_d.ap(), "gqa")
    gkva = P.alloc([128, 1], F32); load(gkva, gkva_d.ap(), "gkva")
    gha = P.alloc([64, 8], F32); load(gha, gha_d.ap(), "gha")
    nsel = P.alloc([128, 64], BF16); load(nsel, c_nsel.ap(), "nsel", q="pool")
    v_allf = P.alloc([128, 32, 592], BF16)
    v_all = v_allf[:, :, 0:528].rearrange("p j (h c) -> p j h c", c=66)
    memset("pool", v_allf, 0.0, ["v_all"]); memset("dve", v_all[:, :, :, 64:65], 1.0, ["v_all"])
    t1 = P.alloc([96, 512], F32); t2 = P.alloc([96, 512], F32)
    P.mark()
    sqb = P.alloc([128, 2, 512], BF16); cqf = P.alloc([128, 2, 512], F32); rstd = P.alloc([128, 512], F32)
    sq2 = P.alloc([128, 512], BF16); ckf = P.alloc([128, 512], F32); rstd2 = P.alloc([128, 512], F32)
    for s in range(8):
        sl = slice(s * 512, (s + 1) * 512)
        for blk in range(2):
            for kc in range(8):
                mm(B[blk][:, :], wcq[:, kc, blk * 128:(blk + 1) * 128], xnT[:, kc, sl], kc == 0, kc == 7, ["wcq"] + XNT, [BK[blk]])
            actf(sqb[:, blk, :], B[blk][:, :], AF.Square, [BK[blk]], [("sqb", blk)])
            actf(cqf[:, blk, :], B[blk][:, :], AF.Copy, [BK[blk]], [("cqf", blk)])
        mm(B[2][:, :], onesb, sqb[:, 0, :], True, False, ["onesb", ("sqb", 0)], [BK[2]])
        mm(B[2][:, :], onesb, sqb[:, 1, :], False, True, ["onesb", ("sqb", 1)], [BK[2]])
        actf(rstd, B[2][:, :], AF.Sqrt, [BK[2]], ["rstd"], bias=EPS, scale=1.0 / 256)
        recip(rstd, rstd, ["rstd"], ["rstd"])
        for blk in range(2):
            stt("dve", cqnT[:, blk, sl], cqf[:, blk, :], gqa[:, blk:blk + 1], rstd, ALU.mult, ALU.mult, [("cqf", blk), "gqa", "rstd"], ["cqnT"])
        for kc in range(8):
            mm(B[3][:, :], wckv[:, kc, :], xnT[:, kc, sl], kc == 0, kc == 7, ["wckv"] + XNT, [BK[3]])
        actf(sq2, B[3][:, :], AF.Square, [BK[3]], ["sq2"])
        actf(ckf, B[3][:, :], AF.Copy, [BK[3]], ["ckf"])
        mm(B[4][:, :], onesb, sq2, True, True, ["onesb", "sq2"], [BK[4]])
        actf(rstd2, B[4][:, :], AF.Sqrt, [BK[4]], ["rstd2"], bias=EPS, scale=1.0 / 128)
        recip(rstd2, rstd2, ["rstd2"], ["rstd2"])
        stt("dve", ckvnT[:, sl], ckf, gkva[:, 0:1], rstd2, ALU.mult, ALU.mult, ["ckf", "gkva", "rstd2"], ["ckvnT"])
        for kc in range(8):
            mm(B[5][0:96, :], wkr[:, kc, :], xnT[:, kc, sl], kc == 0, kc == 7, ["wkr"] + XNT, [BK[5]])
        for kc in range(8):
            mm(B[6][0:96, :], wkrr[:, kc, :], xnT[:, kc, sl], kc == 0, kc == 7, ["wkrr"] + XNT, [BK[6]])
        tt("dve", t1[R_], B[5][R_, :], cosT[R_, sl], ALU.mult, [BK[5], "cosT"], ["t1"])
        tt("dve", t2[R_], B[6][R_, :], sinT[R_, sl], ALU.mult, [BK[6], "sinT"], ["t2"])
        tt("dve", krT[R_, sl], t1[R_], t2[R_], ALU.add, ["t1", "t2"], ["krT"])
    for j in range(32):
        bk = j % 2
        mm(B[bk][:, :], ckvnT[:, j * 128:(j + 1) * 128], wv2, True, True, ["ckvnT", "wv2"], [BK[bk]])
        cp("dve", v_all[:, j, :, 0:64], B[bk][:, :].rearrange("p (h d) -> p h d", h=8), [BK[bk]], ["v_all"])
    P.release()
    if stop <= 2.6:
        tap("cqnT", cqnT, [128, 2, 4096], BF16, ["cqnT"]); tap("ckvnT", ckvnT, [128, 4096], BF16, ["ckvnT"]); tap("krT", krT[64:96], [32, 4096], BF16, ["krT"])
        return finish(nc, P, out, tapd, extra_out=[("ymT_d", ymT_d, [512, 4096], BF16)])
    P.barrier()
    qTh_ = [P.alloc([128, 4096], BF16), xnT[:, 0, :]]
    kTh_ = [P.alloc([128, 4096], BF16), xnT[:, 1, :]]
    vh_ = [P.alloc([128, 32, 128], BF16), xnT[:, 2, :].rearrange("p (j c) -> p j c", c=128)]
    for pb in range(2):
        memset("dve", qTh_[pb], 0.0, [("qTh", pb)]); memset("pool", kTh_[pb], 0.0, [("kTh", pb)])
        memset("pool", vh_[pb], 0.0, [("vh", pb)]); memset("dve", vh_[pb][:, :, 64:65], 1.0, [("vh", pb)])
    PT2 = [P.alloc([128, 1024], BF16) for _ in range(2)]
    osq = P.alloc([128, 512], BF16); memset("dve", osq, 0.0, ["osq"]); ocp = P.alloc([64, 512], F32); rden = P.alloc([64, 512], F32)
    yab = [P.alloc([64, 512], BF16) for _ in range(2)]

    def pro_groups(h, s):
        pb = h % 2
        qTh, kTh = qTh_[pb], kTh_[pb]
        hc = slice(h * 96, (h + 1) * 96)
        sl = slice(s * 512, (s + 1) * 512)

        def g0():
            ph = P.phase; P.phase = "p1c_pro"
            for k2 in range(2):
                mm(B[5][0:96, :], wqb[:, k2, hc], cqnT[:, k2, sl], k2 == 0, k2 == 1, ["wqb", "cqnT"], [BK[5]])
            cp("dve", qTh[0:64, sl], B[5][0:64, :], [BK[5]], [("qTh", pb)])
            tt("dve", t1[R_], B[5][R_, :], cosT[R_, sl], ALU.mult, [BK[5], "cosT"], ["t1"])
            P.phase = ph

        def g1():
            ph = P.phase; P.phase = "p1c_pro"
            for k2 in range(2):
                mm(B[5][0:96, :], wqbr[:, k2, hc], cqnT[:, k2, sl], k2 == 0, k2 == 1, ["wqbr", "cqnT"], [BK[5]])
            tt("dve", t2[R_], B[5][R_, :], sinT[R_, sl], ALU.mult, [BK[5], "sinT"], ["t2"])
            tt("dve", qTh[R_, sl], t1[R_], t2[R_], ALU.add, ["t1", "t2"], [("qTh", pb)])
            P.phase = ph

        def g2():
            ph = P.phase; P.phase = "p1c_pro"
            mm(B[5][0:64, :], wkn[:, h * 64:(h + 1) * 64], ckvnT[:, sl], True, True, ["wkn", "ckvnT"], [BK[5]])
            cp("dve", kTh[0:64, sl], B[5][0:64, :], [BK[5]], [("kTh", pb)])
            P.phase = ph
        return [g0, g1, g2]

    def pro_step(h, s):
        for g in pro_groups(h, s):
            g()

    def pro_fin(h):
        pb = h % 2
        cp("dve", kTh_[pb][R_, :], krT[R_, :], ["krT"], [("kTh", pb)])
        cp("dve", vh_[pb][:, :, 0:64], v_all[:, :, h, 0:64], ["v_all"], [("vh", pb)])

    def attn(h, Q, inserts=()):
        inserts = list(inserts)
        pb_ = h % 2
        qTh, kTh, vh = qTh_[pb_], kTh_[pb_], vh_[pb_]
        ql = slice(Q * 512, (Q + 1) * 512)
        ob = OB_BASE + Q % 2

        def st_(kp):
            pb = kp % 2
            for u in range(2):
                kt = 2 * kp + u
                mm(B[2 * pb + u][:, :], kTh[:, kt * 128:(kt + 1) * 128], qTh[:, ql], True, True, [("kTh", pb_), ("qTh", pb_)], [BK[2 * pb + u]])
            actf(PT2[pb], TP[pb][:, :], AF.Exp, [BK[2 * pb], BK[2 * pb + 1]], [("PT2", pb)], scale=96 ** -0.5)
        st_(0)
        for kp in range(16):
            if kp + 1 < 16:
                st_(kp + 1)
            for u in range(2):
                kt = 2 * kp + u
                mm(B[ob][:, :], vh[:, kt, :], PT2[kp % 2][:, u * 512:(u + 1) * 512], kt == 0, kt == 31, [("vh", pb_), ("PT2", kp % 2)], [BK[ob]])
            if kp in (3, 8, 13) and inserts:
                inserts.pop(0)()
        actf(osq, B[ob][:, :], AF.Square, [BK[ob]], ["osq"])
        cp("dve", ocp, B[ob][0:64, :], [BK[ob]], ["ocp"])
        mm(B[4][0:64, :], nsel[:, :], osq[:, :], True, True, ["nsel", "osq"], [BK[4]])
        actf(rden, B[4][0:64, :], AF.Sqrt, [BK[4]], ["rden"])
        recip(rden, rden, ["rden"], ["rden"])
        yb = yab[Q % 2]; yk = ("yab", Q % 2)
        stt("dve", yb, ocp, gha[:, h:h + 1], rden, ALU.mult, ALU.mult, ["ocp", "gha", "rden"], [yk])
        P.dma(yaT_d[h, :, ql], yb, reads=[yk], writes=[("yaT_d", h, Q)])

    for s_ in range(8):
        pro_step(0, s_)
    pro_fin(0)
    P.phase = "p1c_attn"
    for h in range(NHEADS_MLA):
        for Q in range(8):
            attn(h, Q, pro_groups(h + 1, Q) if h + 1 < NHEADS_MLA else ())
        if h + 1 < NHEADS_MLA:
            pro_fin(h + 1)
    P.release()
    P.barrier()
    P.top = 0
    identb = P.alloc([128, 128], BF16); load(identb, c_identb.ap(), "identb")
    identf = P.alloc([128, 128], F32); load(identf, c_identf.ap(), "identf")
    if stop <= 3:
        return finish(nc, P, out, tapd, extra_out=[("ymT_d", ymT_d, [512, 4096], BF16), ("yaT_d", yaT_d, [8, 64, 4096], BF16)])

    P.phase = "p2"
    AFF = P.alloc([128, 32, 16], F32)
    P.mark()
    woutm = wload([128, 4, 1024], woutm_d.ap().rearrange("(k p) n -> p k n", p=128), "woutm")
    wouta = wload([128, 4, 1024], wouta_d.ap().rearrange("h p n -> (h p) n").rearrange("(k p) n -> p k n", p=128), "wouta")
    wmq = wload([128, 8, 1024], wmq_d.ap().rearrange("(k p) n -> p k n", p=128), "wmq")
    wmo = wload([128, 8, 1024], wmo_d.ap().rearrange("(k p) n -> p k n", p=128), "wmo")
    gmx = P.alloc([128, 8], F32); load(gmx, gmx_d.ap(), "gmx")
    gffn_b = P.alloc([128, 1024], F32); load(gffn_b, gffn_d.ap().to_broadcast([128, 1024]), "gffn_b")
    gffn_p = P.alloc([128, 8], F32); load(gffn_p, gffnp_d.ap(), "gffn_p")
    wr = P.alloc([128, 8, 16], F32); load(wr, wr_d.ap().rearrange("(k p) n -> p k n", p=128), "wr")
    memKT = P.alloc([128, 8, 256], BF16); memV = P.alloc([128, 2, 4, 257], BF16)
    memset("dve", memV[:, :, :, 256:257], 1.0, ["memV"])
    P.mark()
    wmk = wload([128, 8, 1024], wmk_d.ap().rearrange("(k p) n -> p k n", p=128), "wmk")
    wmv = wload([128, 8, 1024], wmv_d.ap().rearrange("(k p) n -> p k n", p=128), "wmv")
    gmkv = P.alloc([128, 8], F32); load(gmkv, gmkv_d.ap(), "gmkv")
    mx_ = P.alloc([128, 2, 1024], F32); load(mx_, mem.ap().rearrange("(j p) d -> p j d", p=128), "mx")
    mnb = P.alloc([128, 2, 1024], BF16); memnT = P.alloc([128, 8, 256], BF16)
    mss = P.alloc([128, 2], F32); mjunk = P.alloc([128, 1024], BF16)
    for j in range(2):
        actf(mjunk, mx_[:, j, :], AF.Square, ["mx"], ["mjunk", ("mss", j)], accum=mss[:, j:j + 1])
        actf(mss[:, j:j + 1], mss[:, j:j + 1], AF.Sqrt, [("mss", j)], [("mss", j)], bias=EPS, scale=1.0 / 1024)
        recip(mss[:, j:j + 1], mss[:, j:j + 1], [("mss", j)], [("mss", j)])
        ts("dve", mnb[:, j, :], mx_[:, j, :], mss[:, j:j + 1], None, ALU.mult, None, ["mx", ("mss", j)], [("mnb", j)])
    for kc in range(8):
        bk = 6 + kc % 2
        for j in range(2):
            tr(Bb[bk][:, j * 128:(j + 1) * 128], mnb[:, j, kc * 128:(kc + 1) * 128], identb, [("mnb", j), "identb"], [BK[bk]])
        ts("dve", memnT[:, kc, :], Bb[bk][:, 0:256], gmkv[:, kc:kc + 1], None, ALU.mult, None, [BK[bk], "gmkv"], ["memnT"])
    for blk in range(8):
        bk = blk % 2
        for kc in range(8):
            mm(B[bk][:, 0:256], wmk[:, kc, blk * 128:(blk + 1) * 128], memnT[:, kc, :], kc == 0, kc == 7, ["wmk", "memnT"], [BK[bk]])
        cp("dve", memKT[:, blk, :], B[bk][:, 0:256], [BK[bk]], ["memKT"])
    for mt in range(2):
        for hf in range(2):
            bk = 2 + hf
            for kc in range(8):
                mm(B[bk][:, :], memnT[:, kc, mt * 128:(mt + 1) * 128], wmv[:, kc, hf * 512:(hf + 1) * 512], kc == 0, kc == 7, ["wmv", "memnT"], [BK[bk]])
            cp("dve", memV[:, mt, 2 * hf:2 * hf + 2, 0:256], B[bk][:, :].rearrange("p (h d) -> p h d", h=2), [BK[bk]], ["memV"])
    P.release()
    xs2_ = [P.alloc([128, 4, 1024], F32) for _ in range(2)]; ymTs_ = [P.alloc([128, 4, 512], BF16) for _ in range(2)]; yaTs_ = [P.alloc([128, 4, 512], BF16) for _ in range(2)]
    xn2 = P.alloc([128, 4, 1024], BF16); xn2T = P.alloc([128, 8, 512], BF16); qmT = P.alloc([128, 8, 512], BF16)
    PmT = P.alloc([128, 4, 2, 512], BF16); om = P.alloc([128, 4, 1024], BF16); omT = P.alloc([128, 8, 512], BF16)
    x2T = P.alloc([128, 8, 128], F32); xn3 = P.alloc([128, 4, 1024], BF16)
    st2_ = [P.alloc([128, 16], F32) for _ in range(2)]; rtmp = P.alloc([128, 16], F32); lg = P.alloc([128, 16], F32); ex = P.alloc([128, 16], F32)
    junk2 = P.alloc([128, 1024], BF16)
    ymT_v = ymT_d.ap().rearrange("(k p) t -> p k t", p=128)
    yaT_v = yaT_d.ap().rearrange("h p t -> (h p) t").rearrange("(k p) t -> p k t", p=128)
    YD = [("ymT_d", h_) for h_ in range(4)]
    def stageA(s):
        pb = s % 2
        xs2, ymTs, yaTs, st2 = xs2_[pb], ymTs_[pb], yaTs_[pb], st2_[pb]
        sl = slice(s * 512, (s + 1) * 512)
        P.dma(xs2, x[s * 512:(s + 1) * 512, :].rearrange("(j p) d -> p j d", p=128), writes=[("xs2", pb)])
        P.dma(ymTs, ymT_v[:, :, sl], reads=YD, writes=[("ymTs", pb)])
        P.dma(yaTs, yaT_v[:, :, sl], reads=[("yaT_d", h_, s) for h_ in range(8)], writes=[("yaTs", pb)])
        for j in range(4):
            tl = slice(j * 128, (j + 1) * 128)
            for hf in range(2):
                fl = slice(hf * 512, (hf + 1) * 512)
                bk = (2 * j + hf) % 4
                for blk in range(4):
                    mm(B[bk][:, :], ymTs[:, blk, tl], woutm[:, blk, fl], blk == 0, False, [("ymTs", pb), "woutm"], [BK[bk]])
                for blk in range(4):
                    mm(B[bk][:, :], yaTs[:, blk, tl], wouta[:, blk, fl], False, blk == 3, [("yaTs", pb), "wouta"], [BK[bk]])
                tt("dve", xs2[:, j, fl], xs2[:, j, fl], B[bk][:, :], ALU.add, [("xs2", pb), BK[bk]], [("xs2", pb)])
        for j in range(4):
            actf(junk2, xs2[:, j, :], AF.Square, [("xs2", pb)], ["junk2", ("st2", pb, j)], accum=st2[:, j:j + 1])
            actf(st2[:, j:j + 1], st2[:, j:j + 1], AF.Sqrt, [("st2", pb, j)], [("st2", pb, j)], bias=EPS, scale=1.0 / 1024)
            recip(st2[:, j:j + 1], st2[:, j:j + 1], [("st2", pb, j)], [("st2", pb, j)])
            ts("dve", xn2[:, j, :], xs2[:, j, :], st2[:, j:j + 1], None, ALU.mult, None, [("xs2", pb), ("st2", pb, j)], ["xn2"])
        for kc in range(8):
            bk = 6 + kc % 2
            for j in range(4):
                tr(Bb[bk][:, j * 128:(j + 1) * 128], xn2[:, j, kc * 128:(kc + 1) * 128], identb, ["xn2", "identb"], [BK[bk]])
            if kc % 2:
                ts("dve", xn2T[:, kc, :], Bb[bk][:, 0:512], gmx[:, kc:kc + 1], None, ALU.mult, None, [BK[bk], "gmx"], ["xn2T"])
            else:
                actf(xn2T[:, kc, :], Bb[bk][:, 0:512], AF.Copy, [BK[bk], "gmx"], ["xn2T"], scale=gmx[:, kc:kc + 1])
        for blk in range(8):
            bk = blk % 2
            for kc in range(8):
                mm(B[bk][:, :], wmq[:, kc, blk * 128:(blk + 1) * 128], xn2T[:, kc, :], kc == 0, kc == 7, ["wmq", "xn2T"], [BK[bk]])
            if blk % 2:
                cp("dve", qmT[:, blk, :], B[bk][:, :], [BK[bk]], ["qmT"])
            else:
                actf(qmT[:, blk, :], B[bk][:, :], AF.Copy, [BK[bk]], ["qmT"])
    def stageB(s):
        pb = s % 2
        xs2, ymTs, yaTs, st2 = xs2_[pb], ymTs_[pb], yaTs_[pb], st2_[pb]
        for h_ in range(4):
            for mc in range(2):
                bk = 2 + (2 * h_ + mc) % 2
                ml = slice(mc * 128, (mc + 1) * 128)
                mm(B[bk][:, :], memKT[:, 2 * h_, ml], qmT[:, 2 * h_, :], True, False, ["memKT", "qmT"], [BK[bk]])
                mm(B[bk][:, :], memKT[:, 2 * h_ + 1, ml], qmT[:, 2 * h_ + 1, :], False, True, ["memKT", "qmT"], [BK[bk]])
                actf(PmT[:, h_, mc, :], B[bk][:, :], AF.Exp, [BK[bk]], ["PmT"], scale=1.0 / 16)
        for j in range(4):
            tl = slice(j * 128, (j + 1) * 128)
            for h_ in range(4):
                i_ = 4 * j + h_
                bk = 4 + i_ % 2
                mm(B[bk][:, 0:257], PmT[:, h_, 0, tl], memV[:, 0, h_, :], True, False, ["PmT", "memV"], [BK[bk]])
                mm(B[bk][:, 0:257], PmT[:, h_, 1, tl], memV[:, 1, h_, :], False, True, ["PmT", "memV"], [BK[bk]])
                recip(rtmp[:, i_:i_ + 1], B[bk][:, 256:257], [BK[bk]], [("rtmp", i_)])
                ts("dve", om[:, j, h_ * 256:(h_ + 1) * 256], B[bk][:, 0:256], rtmp[:, i_:i_ + 1], None, ALU.mult, None, [BK[bk], ("rtmp", i_)], ["om"])
        for kc in range(8):
            bk = 6 + kc % 2
            for j in range(4):
                tr(Bb[bk][:, j * 128:(j + 1) * 128], om[:, j, kc * 128:(kc + 1) * 128], identb, ["om", "identb"], [BK[bk]])
            if kc % 2:
                cp("dve", omT[:, kc, :], Bb[bk][:, 0:512], [BK[bk]], ["omT"])
            else:
                actf(omT[:, kc, :], Bb[bk][:, 0:512], AF.Copy, [BK[bk]], ["omT"])
        for j in range(4):
            tl = slice(j * 128, (j + 1) * 128)
            for hf in range(2):
                fl = slice(hf * 512, (hf + 1) * 512)
                bk = (2 * j + hf) % 4
                for kc in range(8):
                    mm(B[bk][:, :], omT[:, kc, tl], wmo[:, kc, fl], kc == 0, kc == 7, ["omT", "wmo"], [BK[bk]])
                tt("dve", xs2[:, j, fl], xs2[:, j, fl], B[bk][:, :], ALU.add, [("xs2", pb), BK[bk]], [("xs2", pb)])
    def stageC(s):
        pb = s % 2
        xs2, ymTs, yaTs, st2 = xs2_[pb], ymTs_[pb], yaTs_[pb], st2_[pb]
        P.dma(acc_d[s * 512:(s + 1) * 512, :].rearrange("(j p) d -> p j d", p=128), xs2, reads=[("xs2", pb)], writes=["acc_d"])
        for j in range(4):
            t = s * 4 + j
            q_ = 4 + j
            actf(junk2, xs2[:, j, :], AF.Square, [("xs2", pb)], ["junk2", ("st2", pb, q_)], accum=st2[:, q_:q_ + 1])
            actf(st2[:, q_:q_ + 1], st2[:, q_:q_ + 1], AF.Sqrt, [("st2", pb, q_)], [("st2", pb, q_)], bias=EPS, scale=1.0 / 1024)
            recip(st2[:, q_:q_ + 1], st2[:, q_:q_ + 1], [("st2", pb, q_)], [("st2", pb, q_)])
            stt("dve", xn3[:, j, :], xs2[:, j, :], st2[:, q_:q_ + 1], gffn_b, ALU.mult, ALU.mult, [("xs2", pb), ("st2", pb, q_), "gffn_b"], ["xn3"])
            for kc in range(8):
                bk = kc % 2
                tr(B[bk][:, 0:128], xs2[:, j, kc * 128:(kc + 1) * 128], identf, [("xs2", pb), "identf"], [BK[bk]])
                if kc % 2:
                    ts("dve", x2T[:, kc, :], B[bk][:, 0:128], gffn_p[:, kc:kc + 1], None, ALU.mult, None, [BK[bk], "gffn_p"], [("x2T", kc)])
                else:
                    actf(x2T[:, kc, :], B[bk][:, 0:128], AF.Copy, [BK[bk], "gffn_p"], [("x2T", kc)], scale=gffn_p[:, kc:kc + 1])
            for kc in range(8):
                mm(B[2][:, 0:16], x2T[:, kc, :], wr[:, kc, :], kc == 0, kc == 7, [("x2T", kc), "wr"], [BK[2]])
            ts("dve", lg, B[2][:, 0:16], st2[:, q_:q_ + 1], None, ALU.mult, None, [BK[2], ("st2", pb, q_)], ["lg"])
            P.dve(lambda e: e.tensor_reduce(out=ex[:, 0:1], in_=lg, axis=AX.X, op=ALU.max), ["lg"], ["exm"])
            ts("dve", ex[:, 0:1], ex[:, 0:1], -1.0, None, ALU.mult, None, ["exm"], ["exm"])
            actf(lg, lg, AF.Exp, ["lg", "exm"], ["lg", "exs"], bias=ex[:, 0:1], accum=ex[:, 1:2])
            recip(ex[:, 1:2], ex[:, 1:2], ["exs"], ["exs"])
            ts("dve", AFF[:, t, :], lg, ex[:, 1:2], None, ALU.mult, None, ["lg", "exs"], ["AFF"])
        P.dma(h_d[s * 512:(s + 1) * 512, :].rearrange("(j p) d -> p j d", p=128), xn3, reads=["xn3"], writes=["h_d"])
    stageA(0)
    for s in range(8):
        stageB(s)
        if s + 1 < 8:
            stageA(s + 1)
        stageC(s)
    tap("AFF", AFF, [128, 32, 16], F32, ["AFF"])
    P.release()
    if stop <= 4:
        return finish(nc, P, out, tapd, extra_out=[("acc_d", acc_d, [4096, 1024], F32)])

    P.phase = "p3"
    P.mark()
    iota = P.alloc([128, 512], F32); load(iota, c_iota.ap(), "iota")
    posm = P.alloc([128, 32, 16], F32)
    RH = P.alloc([128, 32, 16, 4], BF16)
    P.mark()
    onesf = P.alloc([128, 128], F32); load(onesf, c_onesf.ap(), "onesf")
    Uf = P.alloc([128, 128], F32); load(Uf, c_U.ap(), "Uf")
    jp = P.alloc([128, 32, 2], F32); load(jp, c_jp.ap(), "jp")
    lo = P.alloc([128, 16], F32); mid = P.alloc([128, 16], F32); tot = P.alloc([128, 16], F32); tq = P.alloc([128, 16], F32)
    sel = P.alloc([128, 32, 16], F32); cum = P.alloc([128, 32, 16], F32)
    ones32 = P.alloc([128, 32], F32); memset("dve", ones32, 1.0, ["ones32"])
    afh = P.alloc([128, 32, 16], BF16); afl = P.alloc([128, 32, 16], F32)
    memset("dve", lo, 0.0, ["lo"])
    mid3 = mid.rearrange("p (o e) -> p o e", o=1).to_broadcast([128, 32, 16])
    lo3 = lo.rearrange("p (o e) -> p o e", o=1).to_broadcast([128, 32, 16])
    self2 = sel.rearrange("p j e -> p (j e)")
    for it in range(24):
        step = 0.5 ** (it + 1)
        ts("dve", mid, lo, step, None, ALU.add, None, ["lo"], ["mid"])
        tt("dve", sel, AFF, mid3, ALU.is_ge, ["AFF", "mid"], ["sel"])
        mm(B[0][:, :], onesf, self2, True, True, ["onesf", "sel"], [BK[0]])
        P.dve(lambda e: e.tensor_reduce(out=tot, in_=B[0][:, :].rearrange("p (j e) -> p e j", e=16), axis=AX.X, op=ALU.add), [BK[0]], ["tot"])
        stt("dve", tq, tot, 512.0, mid, ALU.is_ge, ALU.mult, ["tot", "mid"], ["tq"])
        tt("dve", lo, lo, tq, ALU.max, ["lo", "tq"], ["lo"])
    tt("dve", sel, AFF, lo3, ALU.is_ge, ["AFF", "lo"], ["sel"])
    for e_ in range(16):
        P.dve(lambda e, e_=e_: e.tensor_tensor_scan(out=cum[:, :, e_], data0=ones32, data1=sel[:, :, e_], initial=0.0, op0=ALU.mult, op1=ALU.add), ["ones32", "sel"], ["cum"])
    tt("dve", cum, cum, sel, ALU.subtract, ["cum", "sel"], ["cum"])
    mm(B[1][:, :], onesf, cum.rearrange("p j e -> p (j e)"), True, False, ["onesf", "cum"], [BK[1]])
    mm(B[1][:, :], Uf, self2, False, True, ["Uf", "sel"], [BK[1]])
    stt("dve", posm.rearrange("p j e -> p (j e)"), B[1][:, :], 1.0, self2, ALU.add, ALU.mult, [BK[1], "sel"], ["posm"])
    ts("dve", posm, posm, -1.0, None, ALU.add, None, ["posm"], ["posm"])
    cp("dve", afh, AFF, ["AFF"], ["afh"])
    tt("dve", afl, AFF, afh, ALU.subtract, ["AFF", "afh"], ["afl"])
    cp("dve", RH[:, :, :, 2], afh, ["afh"], ["RH"])
    cp("dve", RH[:, :, :, 3], afl, ["afl", "RH"], ["RH"])
    for e_ in range(16):
        cp("dve", RH[:, :, e_, 0:2], jp, ["jp", "RH"], ["RH"])
    P.release()
    P.phase = "p3_alloc"
    OH = P.alloc([128, 32, 512], BF16)
    res = P.alloc([128, 16], F32); idxf = [P.alloc([128, 4], F32) for _ in range(2)]; idxi = [P.alloc([128, 4], I32) for _ in range(2)]
    gate_ = [P.alloc([128, 4], F32) for _ in range(2)]
    xe = [P.alloc([128, 4, 1024], BF16) for _ in range(2)]
    xeT = P.alloc([128, 8, 512], BF16); hidT = P.alloc([128, 16, 512], BF16)
    sg = [P.alloc([128, 512], F32) for _ in range(2)]
    yacc = [P.alloc([128, 1024], F32) for _ in range(4)]
    NGU, ND = 4, 4
    gub = [P.alloc([128, 2, 8, 512], BF16) for _ in range(NGU)]
    dbf = [P.alloc([128, 4, 1024], BF16) for _ in range(ND)]

    def route_oh(e_, j0, j1):
        ph = P.phase; P.phase = "p3_route"
        for j in range(j0, j1):
            ts("dve", OH[:, j, :], iota, posm[:, j, e_:e_ + 1], None, ALU.is_equal, None, ["iota", "posm"], [("OH", j)])
        P.phase = ph

    def route_rest(e_):
        ph = P.phase; P.phase = "p3_route"
        p_ = e_ % 2
        for sc in range(4):
            for j in range(32):
                mm(B[0][:, sc * 4:(sc + 1) * 4], OH[:, j, sc * 128:(sc + 1) * 128], RH[:, j, e_, :], j == 0, j == 31, [("OH", j), "RH"], [BK[0]])
        cp("dve", res, B[0][:, 0:16], [BK[0]], ["res"])
        r3 = res.rearrange("p (s c) -> p s c", c=4)
        stt("dve", idxf[p_], r3[:, :, 0], 128.0, r3[:, :, 1], ALU.mult, ALU.add, ["res"], [("idxf", p_)])
        tt("dve", gate_[p_], r3[:, :, 2], r3[:, :, 3], ALU.add, ["res"], [("gate", p_)])
        cp("dve", idxi[p_], idxf[p_], [("idxf", p_)], [("idxi", p_)])
        for sc in range(4):
            P.op("pool", lambda e, sc=sc, p_=p_: e.indirect_dma_start(out=xe[p_][:, sc, :], out_offset=None, in_=h_d[:, :],
                                                                in_offset=bass.IndirectOffsetOnAxis(ap=idxi[p_][:, sc:sc + 1], axis=0)),
                 reads=[("idxi", p_), "h_d"], writes=[("xe", p_, sc)], dma=True)
        P.phase = ph

    def wload_gu(e_, q4):
        ph = P.phase; P.phase = "p3_wload"
        g_ = gub[q4 % NGU]
        P.dma(g_[:, 0, :, :], weg_d[e_, :, q4 * 512:(q4 + 1) * 512].rearrange("(k p) n -> p k n", p=128), writes=[("gub", q4 % NGU, 0)], q="pool")
        P.dma(g_[:, 1, :, :], weu_d[e_, :, q4 * 512:(q4 + 1) * 512].rearrange("(k p) n -> p k n", p=128), writes=[("gub", q4 % NGU, 1)], q="pool")
        P.phase = ph

    def wload_d(e_, q4):
        ph = P.phase; P.phase = "p3_wload"
        P.dma(dbf[q4 % ND], wed_d[e_, q4 * 512:(q4 + 1) * 512, :].rearrange("(k p) n -> p k n", p=128), writes=[("dbf", q4 % ND)], q="pool")
        P.phase = ph

    def wloads(e_):
        for q4 in range(4):
            wload_gu(e_, q4)
        for q4 in range(4):
            wload_d(e_, q4)

    def compute(e_):
        P.phase = "p3_xT"
        p_ = e_ % 2
        for kc in range(8):
            for sc in range(4):
                tr(Bb[7][:, sc * 128:(sc + 1) * 128], xe[p_][:, sc, kc * 128:(kc + 1) * 128], identb, [("xe", p_, sc), "identb"], [BK[7]])
            if kc % 2:
                cp("dve", xeT[:, kc, :], Bb[7][:, 0:512], [BK[7]], ["xeT"])
            else:
                actf(xeT[:, kc, :], Bb[7][:, 0:512], AF.Copy, [BK[7]], ["xeT"])
        P.phase = "p3_gu"
        for fc in range(16):
            q4, f4 = fc // 4, fc % 4
            g_ = gub[q4 % NGU]
            bg, bu = 1 + (fc % 2) * 2, 2 + (fc % 2) * 2
            for kc in range(8):
                mm(B[bg][:, :], g_[:, 0, kc, f4 * 128:(f4 + 1) * 128], xeT[:, kc, :], kc == 0, kc == 7, [("gub", q4 % NGU, 0), "xeT"], [BK[bg]])
            for kc in range(8):
                mm(B[bu][:, :], g_[:, 1, kc, f4 * 128:(f4 + 1) * 128], xeT[:, kc, :], kc == 0, kc == 7, [("gub", q4 % NGU, 1), "xeT"], [BK[bu]])
            actf(sg[fc % 2], B[bg][:, :], AF.Silu, [BK[bg]], [("sg", fc % 2)])
            tt("dve", hidT[:, fc, :], sg[fc % 2], B[bu][:, :], ALU.mult, [("sg", fc % 2), BK[bu]], [("hidT", fc)])
            if e_ + 1 < 16:
                route_oh(e_ + 1, 2 * fc, 2 * fc + 2)
            if f4 == 3 and e_ + 1 < 16:
                wload_gu(e_ + 1, q4)
        if e_ + 1 < 16:
            route_rest(e_ + 1)
        P.phase = "p3_down"
        gi = 0
        for q4 in range(4):
            for st_ in range(4):
                for hf in range(2):
                    bk = 5 + gi % 2
                    gi += 1
                    fl = slice(hf * 512, (hf + 1) * 512)
                    for f4 in range(4):
                        fc = q4 * 4 + f4
                        mm(B[bk][:, :], hidT[:, fc, st_ * 128:(st_ + 1) * 128], dbf[q4 % ND][:, f4, fl], f4 == 0, f4 == 3,
                           [("hidT", fc), ("dbf", q4 % ND)], [BK[bk]])
                    yk = ("ya", st_, hf)
                    if q4 == 0:
                        actf(yacc[st_][:, fl], B[bk][:, :], AF.Copy, [BK[bk], ("gate", p_)], [yk], scale=gate_[p_][:, st_:st_ + 1])
                    else:
                        stt("dve", yacc[st_][:, fl], B[bk][:, :], gate_[p_][:, st_:st_ + 1], yacc[st_][:, fl], ALU.mult, ALU.add,
                            [BK[bk], ("gate", p_), yk], [yk])
            if e_ + 1 < 16:
                wload_d(e_ + 1, q4)
        for st_ in range(4):
            P.op("pool", lambda e, st_=st_, p_=p_: e.indirect_dma_start(out=acc_d[:, :], out_offset=bass.IndirectOffsetOnAxis(ap=idxi[p_][:, st_:st_ + 1], axis=0),
                                                                in_=yacc[st_], in_offset=None, compute_op=ALU.add),
                 reads=[("idxi", p_), ("ya", st_, 0), ("ya", st_, 1), "acc_d"], writes=["acc_d"], dma=True)

    route_oh(0, 0, 32)
    route_rest(0)
    wloads(0)
    for e_ in range(16):
        compute(e_)
    P.release()
    if stop <= 5:
        return finish(nc, P, out, tapd, extra_out=[("acc_d", acc_d, [4096, 1024], F32)])

    P.phase = "p4"
    gfin_b = P.alloc([128, 1024], F32); load(gfin_b, gfin_d.ap().to_broadcast([128, 1024]), "gfin_b")
    xf = [P.alloc([128, 4, 1024], F32) for _ in range(2)]
    of = [P.alloc([128, 4, 1024], F32) for _ in range(2)]
    fs = P.alloc([128, 32], F32); junk3 = P.alloc([128, 1024], BF16)
    for s in range(8):
        b = s % 2
        P.dma(xf[b], acc_d[s * 512:(s + 1) * 512, :].rearrange("(j p) d -> p j d", p=128), reads=["acc_d"], writes=[("xf", b)])
        for j in range(4):
            t = s * 4 + j
            actf(junk3, xf[b][:, j, :], AF.Square, [("xf", b)], ["junk3", ("fs", t)], accum=fs[:, t:t + 1])
            actf(fs[:, t:t + 1], fs[:, t:t + 1], AF.Sqrt, [("fs", t)], [("fs", t)], bias=EPS, scale=1.0 / 1024)
            recip(fs[:, t:t + 1], fs[:, t:t + 1], [("fs", t)], [("fs", t)])
            stt("dve", of[b][:, j, :], xf[b][:, j, :], fs[:, t:t + 1], gfin_b, ALU.mult, ALU.mult, [("xf", b), ("fs", t), "gfin_b"], [("of", b)])
        P.dma(out[s * 512:(s + 1) * 512, :].rearrange("(j p) d -> p j d", p=128), of[b], reads=[("of", b)], writes=[("out", s)])
    return finish(nc, P, out, tapd)


def finish(nc, P, out, tapd, extra_out=()):
    for name, src, shp, dt in extra_out:
        t = nc.dram_tensor("tap_" + name, list(shp), dt, kind="ExternalOutput")
        tapd[name] = t
        P.barrier()
        P.dma(t.ap(), src.ap(), writes=[("tap", name)])
    P.barrier()
    P.emit()
    return nc, P, tapd


def _bf(a):
    return np.ascontiguousarray(a).astype(ml_dtypes.bfloat16)


def _pk(v, k):
    return np.ascontiguousarray(np.asarray(v, np.float32).reshape(k, 128).T)


def const_inputs():
    c = {}
    c["c_identb"] = _bf(np.eye(128, dtype=np.float32))
    c["c_identf"] = np.eye(128, dtype=np.float32)
    c["c_onesb"] = _bf(np.ones((128, 128), np.float32))
    c["c_onesf"] = np.ones((128, 128), np.float32)
    s = np.arange(128)[:, None]; t = np.arange(128)[None, :]
    same = (s // 64) == (t // 64)
    bm = np.zeros((128, 2, 128), np.float32)
    bm[:, 0, :] = (same & (s <= t)); bm[:, 1, :] = (same & (s >= t))
    bm *= np.float32(128 ** -0.5)
    c["c_bmask"] = bm
    sel = np.zeros((36, 8, 128), np.float32)
    for l, p in enumerate(LANE_PART):
        sel[p, l, :] = 1.0
    c["c_sel36"] = sel
    ns = np.zeros((128, 64), np.float32); ns[0:64, :] = 1.0 / 64; ns[64, :] = EPS
    c["c_nsel"] = ns
    invf = np.zeros((128, 1), np.float32)
    f = (10000.0 ** (-np.arange(0, 32, 2, dtype=np.float32) / 32)).astype(np.float32)
    invf[64:80, 0] = f; invf[80:96, 0] = f
    c["c_invf"] = invf
    r = np.ones((36, 4096), np.float32); r[:, ::64] = 0.0
    c["c_reset"] = r
    c["c_iota"] = np.tile(np.arange(512, dtype=np.float32)[None, :], (128, 1))
    c["c_tokid"] = (np.arange(32)[None, :] * 128 + np.arange(128)[:, None]).astype(np.float32)
    c["c_U"] = (s < t).astype(np.float32)
    jp = np.zeros((128, 32, 2), np.float32); jp[:, :, 0] = np.arange(32)[None, :]; jp[:, :, 1] = np.arange(128)[:, None]
    c["c_jp"] = jp
    return c


def weight_inputs(I):
    g = lambda k: np.asarray(I[k], np.float32)
    w = {}
    w_in = g("w_in")[0]
    w["gmix"] = _pk(g("g_mix")[0], 8)
    w["w_q"] = np.ascontiguousarray(w_in[:, 0:512]); w["w_k"] = np.ascontiguousarray(w_in[:, 512:1024])
    w["w_v"] = np.ascontiguousarray(w_in[:, 1024:1536]); w["w_o"] = np.ascontiguousarray(w_in[:, 1536:2048])
    gt = w_in[:, 2048:2064]; bg = g("b_gates")[0]
    wI = np.zeros((1024, 36), np.float32); wF = np.zeros((1024, 36), np.float32)
    bI = np.zeros((36, 1), np.float32); bF = np.zeros((36, 1), np.float32)
    wI[:, 0:4] = gt[:, 0:4]; wI[:, 32:36] = gt[:, 8:12]; wF[:, 0:4] = gt[:, 4:8]; wF[:, 32:36] = gt[:, 12:16]
    bI[0:4, 0] = bg[0:4]; bI[32:36, 0] = bg[8:12]; bF[0:4, 0] = bg[4:8]; bF[32:36, 0] = bg[12:16]
    w["w_gI"], w["w_gF"], w["b_I"], w["b_F"] = wI, wF, bI, bF
    w["w_cq"] = np.ascontiguousarray(w_in[:, 2064:2320]); w["w_ckv"] = np.ascontiguousarray(w_in[:, 2320:2448])
    kr = w_in[:, 2448:2480]
    wkr = np.zeros((1024, 96), np.float32); wkr[:, 64:96] = kr
    wkrr = np.zeros((1024, 96), np.float32); wkrr[:, 64:80] = kr[:, 16:32]; wkrr[:, 80:96] = kr[:, 0:16]
    w["w_kr"], w["w_krr"] = wkr, wkrr
    cv = g("conv_qk")[0]
    w["convq"] = np.ascontiguousarray(cv[:, 0:512].reshape(3, 4, 128).transpose(2, 1, 0))
    w["convk"] = np.ascontiguousarray(cv[:, 512:1024].reshape(3, 4, 128).transpose(2, 1, 0))
    w["g_qa"] = _pk(g("g_q_a")[0], 2); w["g_kva"] = _pk(g("g_kv_a")[0], 1)
    wqb = g("w_q_b")[0]
    w["w_qb"] = wqb
    wqbr = np.zeros_like(wqb).reshape(256, 8, 96); q3 = wqb.reshape(256, 8, 96)
    wqbr[:, :, 64:80] = q3[:, :, 80:96]; wqbr[:, :, 80:96] = q3[:, :, 64:80]
    w["w_qbr"] = np.ascontiguousarray(wqbr.reshape(256, 768))
    kv3 = g("w_kv_b")[0].reshape(128, 8, 128)
    w["w_kn"] = np.ascontiguousarray(kv3[:, :, 0:64].reshape(128, 512)); w["w_v2"] = np.ascontiguousarray(kv3[:, :, 64:128].reshape(128, 512))
    w["g_hm"] = _pk(g("g_head_mlstm")[0], 4)
    w["g_ha"] = np.ascontiguousarray(g("g_head_mla")[0].reshape(8, 64).T)
    wout = g("w_out")[0]
    w["w_outm"] = np.ascontiguousarray(wout[0:512]); w["w_outa"] = np.ascontiguousarray(wout[512:1024].reshape(8, 64, 1024))
    w["g_mx"] = _pk(g("g_mem_x")[0], 8); w["g_mkv"] = _pk(g("g_mem_kv")[0], 8)
    w["w_mq"], w["w_mk"], w["w_mv"], w["w_mo"] = g("w_mem_q")[0], g("w_mem_k")[0], g("w_mem_v")[0], g("w_mem_o")[0]
    w["g_ffnp"] = _pk(g("g_ffn")[0], 8); w["g_ffn"] = g("g_ffn")[0].reshape(1, 1024); w["g_fin"] = g("g_final").reshape(1, 1024)
    w["w_r"] = g("w_router")[0]
    w["w_eg"], w["w_eu"], w["w_ed"] = g("w_exp_gate")[0], g("w_exp_up")[0], g("w_exp_down")[0]
    return w


def core_inputs(I, b, shared):
    m = dict(shared)
    m["x"] = np.ascontiguousarray(np.asarray(I["x"], np.float32)[b])
    m["mem"] = np.ascontiguousarray(np.asarray(I["mem"], np.float32)[b])
    m["pos"] = np.ascontiguousarray(np.tile(np.asarray(I["positions"], np.int32)[b][None, :], (32, 1)))
    return m


_CACHE = {}


def kernel(**inputs):
    if "nc" not in _CACHE:
        _CACHE["nc"] = build()[0]
    nc = _CACHE["nc"]
    shared = const_inputs()
    shared.update(weight_inputs(inputs))
    in_maps = [core_inputs(inputs, c % 4, shared) for c in range(8)]
    res = run_bass_kernel_spmd(nc, in_maps, core_ids=list(range(8)))
    return np.stack([np.asarray(res.results[b]["out"], np.float32) for b in range(4)], axis=0)
```

```python
from contextlib import ExitStack
import numpy as np
import ml_dtypes
import concourse.bass as bass
import concourse.mybir as mybir
from concourse.bass_utils import run_bass_kernel_spmd

F32 = mybir.dt.float32
BF16 = mybir.dt.bfloat16
I32 = mybir.dt.int32
AF = mybir.ActivationFunctionType
ALU = mybir.AluOpType
AX = mybir.AxisListType

SEM_CH = 1000
NDMA_SEM = 12
ARENA_WORDS = 53000
EPS = 1e-6


class Op:
    __slots__ = ("eng", "fn", "deps", "signal", "semval", "idx", "dma", "dsem", "dval", "phase")


class Prog:
    ENGS = ("pe", "act", "dve", "pool", "sp")

    def __init__(self, nc):
        self.nc = nc
        self.streams = {e: [] for e in self.ENGS}
        self.last_w = {}
        self.readers = {}
        self.ndma = {"sp": 0, "pool": 0}
        self.dma_ops = {"sp": [], "pool": []}
        self.seen = {e: {p: -1 for p in self.ENGS} for e in self.ENGS}
        self.seen_dma = {e: {} for e in self.ENGS}
        self.stack = ExitStack()
        self.n_ops = 0
        self.arena = self.stack.enter_context(nc.sbuf_tensor("arena", [128, ARENA_WORDS], F32))
        self.top = 0
        self.marks = []
        self.peak = 0
        self.phase = "p0"
        self.scopes = False

    def alloc(self, shape, dtype):
        esz = 2 if dtype == BF16 else 4
        n = 1
        for s in shape[1:]:
            n *= s
        words = (n * esz + 3) // 4
        off = self.top
        self.top += (words + 7) // 8 * 8
        self.peak = max(self.peak, self.top)
        assert self.top <= ARENA_WORDS, f"SBUF arena overflow {self.top}"
        ap = self.arena[0:shape[0], off:off + words]
        if dtype != F32:
            ap = ap.bitcast(dtype)
        if ap.shape[1] != n:
            ap = ap[:, 0:n]
        if len(shape) == 3:
            ap = ap.rearrange("p (a b) -> p a b", a=shape[1])
        elif len(shape) == 4:
            ap = ap.rearrange("p (a b c) -> p a b c", a=shape[1], b=shape[2])
        return ap

    def mark(self):
        self.marks.append(self.top)

    def release(self):
        self.barrier()
        self.top = self.marks.pop()

    def psum(self, name, shape, dtype=F32):
        return self.stack.enter_context(self.nc.psum_tensor(name, list(shape), dtype))

    def op(self, eng, fn, reads=(), writes=(), dma=False, extra=()):
        xr = [k for k in reads if isinstance(k, tuple) and k[0] == "B"]
        if xr:
            writes = list(writes) + [k for k in xr if k not in writes]
        o = Op()
        o.eng, o.fn, o.dma, o.signal, o.semval = eng, fn, dma, False, 0
        o.dsem = o.dval = None
        o.phase = self.phase
        stream = self.streams[eng]
        o.idx = len(stream)
        deps = {}
        cand = list(extra)
        for k in reads:
            w = self.last_w.get(k)
            if w is not None:
                cand.append(w)
        for k in writes:
            w = self.last_w.get(k)
            if w is not None:
                cand.append(w)
            cand.extend(self.readers.get(k, ()))
        if dma:
            q = eng
            n = self.ndma[q]
            self.ndma[q] = n + 1
            o.dsem = n % NDMA_SEM
            o.dval = 16 * (n // NDMA_SEM + 1)
            if n >= NDMA_SEM:
                cand.append(self.dma_ops[q][n - NDMA_SEM])
            self.dma_ops[q].append(o)
        for d in cand:
            if d is o or d.fn is None:
                continue
            if d.dma:
                key = (d.eng, d.dsem)
                if self.seen_dma[eng].get(key, 0) >= d.dval:
                    continue
                self.seen_dma[eng][key] = d.dval
                deps[("dma",) + key] = d
            else:
                if d.eng == "pe" and eng == "pe" and not dma:
                    continue
                if self.seen[eng][d.eng] >= d.idx:
                    continue
                cur = deps.get(("c", d.eng))
                if cur is None or cur.idx < d.idx:
                    deps[("c", d.eng)] = d
        for k, d in deps.items():
            if k[0] == "c":
                self.seen[eng][d.eng] = d.idx
                d.signal = True
        o.deps = list(deps.values())
        stream.append(o)
        self.n_ops += 1
        for k in reads:
            lst = self.readers.setdefault(k, [])
            if not dma:
                lst[:] = [r for r in lst if r.dma or r.eng != eng]
            lst.append(o)
        for k in writes:
            self.last_w[k] = o
            self.readers[k] = []
        return o

    def pe(self, fn, reads=(), writes=()):
        return self.op("pe", fn, reads, writes)

    def act(self, fn, reads=(), writes=()):
        return self.op("act", fn, reads, writes)

    def dve(self, fn, reads=(), writes=()):
        return self.op("dve", fn, reads, writes)

    def pool(self, fn, reads=(), writes=()):
        return self.op("pool", fn, reads, writes)

    def dma(self, out, in_, reads=(), writes=(), q="sp", **kw):
        return self.op(q, lambda e: e.dma_start(out=out, in_=in_, **kw), reads, writes, dma=True)

    def barrier(self):
        allops = set()
        for w in self.last_w.values():
            allops.add(w)
        for lst in self.readers.values():
            allops.update(lst)
        for q in self.dma_ops:
            allops.update(self.dma_ops[q][-NDMA_SEM:])
        allops = [o for o in allops if o.fn is not None]
        for e in self.ENGS:
            self.op(e, None, extra=allops)
        self.last_w.clear()
        self.readers.clear()

    def emit(self):
        nc = self.nc
        st = self.stack
        csem = {}
        for e in ("pe", "act", "dve", "pool"):
            cnt = 0
            for o in self.streams[e]:
                if o.signal and not o.dma:
                    cnt += 1
                    o.semval = cnt
            nsem = max(1, (cnt + SEM_CH - 1) // SEM_CH)
            csem[e] = [st.enter_context(nc.semaphore(f"s_{e}{i}")) for i in range(nsem)]
        dsem = {}
        for q in ("sp", "pool"):
            dsem[q] = [st.enter_context(nc.semaphore(f"d_{q}{i}")) for i in range(NDMA_SEM)]

        def emit_stream(ename, eng):
            if self.scopes:
                cur = None
                cm = None
                for o in self.streams[ename]:
                    if o.phase != cur:
                        if cm is not None:
                            cm.__exit__(None, None, None)
                        cur = o.phase
                        cm = nc.named_scope(cur)
                        cm.__enter__()
                    emit_one(ename, eng, o)
                if cm is not None:
                    cm.__exit__(None, None, None)
                return
            for o in self.streams[ename]:
                emit_one(ename, eng, o)

        def emit_one(ename, eng, o):
            if True:
                for d in o.deps:
                    if d.dma:
                        eng.wait_ge(dsem[d.eng][d.dsem], d.dval)
                    else:
                        v = d.semval - 1
                        eng.wait_ge(csem[d.eng][v // SEM_CH], v % SEM_CH + 1)
                if o.fn is None:
                    return
                ins = o.fn(eng)
                if o.dma:
                    ins.then_inc(dsem[ename][o.dsem], 16)
                elif o.signal:
                    v = o.semval - 1
                    ins.then_inc(csem[ename][v // SEM_CH], 1)

        with nc.Block() as block:
            @block.tensor
            def _(eng):
                emit_stream("pe", eng)

            @block.scalar
            def _(eng):
                emit_stream("act", eng)

            @block.vector
            def _(eng):
                emit_stream("dve", eng)

            @block.gpsimd
            def _(eng):
                emit_stream("pool", eng)

            @block.sync
            def _(eng):
                emit_stream("sp", eng)


LANE_PART = [0, 1, 2, 3, 32, 33, 34, 35]
NHEADS_MLA = 8
PV_NOACC = False
OB_BASE = 6
SCOPES = False


def build(stop=99, taps=False):
    nc = bass.Bass("TRN2", target_bir_lowering=False)
    P = Prog(nc)
    P.scopes = SCOPES

    def din(n, shp, dt=F32):
        return nc.dram_tensor(n, list(shp), dt, kind="ExternalInput")

    def dscr(n, shp, dt):
        return nc.dram_tensor(n, list(shp), dt, kind="Internal")

    x = din("x", [4096, 1024]); mem = din("mem", [256, 1024]); pos = din("pos", [32, 4096], I32)
    c_identb = din("c_identb", [128, 128], BF16); c_identf = din("c_identf", [128, 128])
    c_onesb = din("c_onesb", [128, 128], BF16)
    c_bmask = din("c_bmask", [128, 2, 128]); c_sel36 = din("c_sel36", [36, 8, 128])
    c_nsel = din("c_nsel", [128, 64]); c_invf = din("c_invf", [128, 1])
    c_reset = din("c_reset", [36, 4096])
    c_iota = din("c_iota", [128, 512]); c_tokid = din("c_tokid", [128, 32]); c_U = din("c_U", [128, 128])
    c_onesf = din("c_onesf", [128, 128])
    gmix_d = din("gmix", [128, 8])
    wq_d = din("w_q", [1024, 512]); wk_d = din("w_k", [1024, 512]); wv_d = din("w_v", [1024, 512]); wo_d = din("w_o", [1024, 512])
    wgI_d = din("w_gI", [1024, 36]); wgF_d = din("w_gF", [1024, 36]); bI_d = din("b_I", [36, 1]); bF_d = din("b_F", [36, 1])
    wcq_d = din("w_cq", [1024, 256]); wckv_d = din("w_ckv", [1024, 128])
    wkr_d = din("w_kr", [1024, 96]); wkrr_d = din("w_krr", [1024, 96])
    convq_d = din("convq", [128, 4, 3]); convk_d = din("convk", [128, 4, 3])
    gqa_d = din("g_qa", [128, 2]); gkva_d = din("g_kva", [128, 1])
    wqb_d = din("w_qb", [256, 768]); wqbr_d = din("w_qbr", [256, 768])
    wkn_d = din("w_kn", [128, 512]); wv2_d = din("w_v2", [128, 512])
    ghm_d = din("g_hm", [128, 4]); gha_d = din("g_ha", [64, 8])
    woutm_d = din("w_outm", [512, 1024]); wouta_d = din("w_outa", [8, 64, 1024])
    gmx_d = din("g_mx", [128, 8]); gmkv_d = din("g_mkv", [128, 8])
    wmq_d = din("w_mq", [1024, 1024]); wmk_d = din("w_mk", [1024, 1024]); wmv_d = din("w_mv", [1024, 1024]); wmo_d = din("w_mo", [1024, 1024])
    gffn_d = din("g_ffn", [1, 1024]); gfin_d = din("g_fin", [1, 1024]); gffnp_d = din("g_ffnp", [128, 8]); c_jp = din("c_jp", [128, 32, 2])
    wr_d = din("w_r", [1024, 16])
    if stop > 4:
        weg_d = din("w_eg", [16, 1024, 2048]); weu_d = din("w_eu", [16, 1024, 2048]); wed_d = din("w_ed", [16, 2048, 1024])
    else:
        weg_d = din("w_eg", [1, 1, 1]); weu_d = din("w_eu", [1, 1, 1]); wed_d = din("w_ed", [1, 1, 1])
    out = nc.dram_tensor("out", [4096, 1024], F32, kind="ExternalOutput")
    ymT_d = dscr("ymT_d", [512, 4096], BF16)
    yaT_d = dscr("yaT_d", [8, 64, 4096], BF16)
    acc_d = dscr("acc_d", [4096, 1024], F32)
    h_d = dscr("h_d", [4096, 1024], BF16)
    tapd = {}

    def tap(name, ap_sb, shape, dt, reads):
        if not taps:
            return
        t = nc.dram_tensor("tap_" + name, list(shape), dt, kind="ExternalOutput")
        tapd[name] = t
        P.dma(t.ap(), ap_sb, reads=reads, writes=[("tap", name)])

    def mm(o, lhsT, rhs, st, sp, R, W):
        P.pe(lambda e: e.matmul(o, lhsT=lhsT, rhs=rhs, start=st, stop=sp), R, W)

    def tr(o, i, idn, R, W):
        P.pe(lambda e: e.transpose(out=o, in_=i, identity=idn), R, W)

    def actf(o, i, func, R, W, bias=None, scale=None, accum=None):
        kw = {}
        if bias is not None:
            kw["bias"] = bias
        if scale is not None:
            kw["scale"] = scale
        if accum is not None:
            kw["accum_out"] = accum
        P.act(lambda e: e.activation(out=o, in_=i, func=func, **kw), R, W)

    def ts(eng, o, i0, s1, s2, op0, op1, R, W):
        if op1 is None:
            P.op(eng, lambda e: e.tensor_scalar(out=o, in0=i0, scalar1=s1, scalar2=None, op0=op0), R, W)
        else:
            P.op(eng, lambda e: e.tensor_scalar(out=o, in0=i0, scalar1=s1, scalar2=s2, op0=op0, op1=op1), R, W)

    def tt(eng, o, i0, i1, op, R, W):
        P.op(eng, lambda e: e.tensor_tensor(out=o, in0=i0, in1=i1, op=op), R, W)

    def stt(eng, o, i0, sc, i1, op0, op1, R, W):
        P.op(eng, lambda e: e.scalar_tensor_tensor(out=o, in0=i0, scalar=sc, in1=i1, op0=op0, op1=op1), R, W)

    def cp(eng, o, i, R, W):
        P.op(eng, lambda e: e.tensor_copy(out=o, in_=i), R, W)

    def recip(o, i, R, W):
        P.dve(lambda e: e.reciprocal(out=o, in_=i), R, W)

    def memset(eng, o, v, W):
        P.op(eng, lambda e: e.memset(o, v), (), W)

    def load(dst, src, key, q="sp"):
        P.dma(dst, src, writes=[key], q=q)

    TP = [P.psum(f"pp{i}", [128, 1024], F32) for i in range(4)]
    B = []
    for t_ in TP:
        B += [t_[:, 0:512], t_[:, 512:1024]]
    Bb = [b.bitcast(BF16) for b in B]
    BK = [("B", i) for i in range(8)]

    identb = P.alloc([128, 128], BF16); load(identb, c_identb.ap(), "identb")
    identf = P.alloc([128, 128], F32); load(identf, c_identf.ap(), "identf")
    onesb = P.alloc([128, 128], BF16); load(onesb, c_onesb.ap(), "onesb")

    def rms_rstd(ssum, rs, n, key):
        actf(rs, ssum, AF.Sqrt, [key + "ss"], [key + "rs"], bias=EPS, scale=1.0 / n)
        recip(rs, rs, [key + "rs"], [key + "rs"])

    xnT = P.alloc([128, 8, 4096], BF16)
    P.mark()
    gmix = P.alloc([128, 8], F32); load(gmix, gmix_d.ap(), "gmix")
    xs = [P.alloc([128, 4, 1024], F32) for _ in range(2)]
    xnb = [P.alloc([128, 4, 1024], BF16) for _ in range(2)]
    junk = P.alloc([128, 1024], BF16)
    ss = P.alloc([128, 32], F32); rs = P.alloc([128, 32], F32)
    for s in range(8):
        b = s % 2
        P.dma(xs[b], x[s * 512:(s + 1) * 512, :].rearrange("(j p) d -> p j d", p=128), writes=[("xs", b)])
        for j in range(4):
            t = s * 4 + j
            actf(junk, xs[b][:, j, :], AF.Square, [("xs", b)], ["junk", ("ss", t)], accum=ss[:, t:t + 1])
            actf(rs[:, t:t + 1], ss[:, t:t + 1], AF.Sqrt, [("ss", t)], [("rs", t)], bias=EPS, scale=1.0 / 1024)
            recip(rs[:, t:t + 1], rs[:, t:t + 1], [("rs", t)], [("rs", t)])
            ts("dve", xnb[b][:, j, :], xs[b][:, j, :], rs[:, t:t + 1], None, ALU.mult, None, [("xs", b), ("rs", t)], [("xnb", b, j)])
        for kc in range(8):
            bk = 6 + kc % 2
            for j in range(4):
                tr(Bb[bk][:, j * 128:(j + 1) * 128], xnb[b][:, j, kc * 128:(kc + 1) * 128], identb, [("xnb", b, j), "identb"], [BK[bk]])
            if kc % 2 == 0:
                ts("dve", xnT[:, kc, s * 512:(s + 1) * 512], Bb[bk][:, 0:512], gmix[:, kc:kc + 1], None, ALU.mult, None, [BK[bk], "gmix"], [("xnT", s)])
            else:
                actf(xnT[:, kc, s * 512:(s + 1) * 512], Bb[bk][:, 0:512], AF.Copy, [BK[bk], "gmix"], [("xnT", s)], scale=gmix[:, kc:kc + 1])
    XNT = [("xnT", s) for s in range(8)]
    tap("xnT", xnT, [128, 8, 4096], BF16, XNT)
    P.release()
    if stop <= 0:
        return finish(nc, P, out, tapd)

    P.phase = "p1a"
    P.mark()
    WS128 = P.alloc([128, 32, 36], F32)
    LB64 = P.alloc([64, 64, 36], F32)
    DEC = P.alloc([128, 8, 64], F32)
    P.mark()
    wgI = P.alloc([128, 8, 36], BF16); load(wgI, wgI_d.ap().rearrange("(k p) n -> p k n", p=128), "wgI", q="pool")
    wgF = P.alloc([128, 8, 36], BF16); load(wgF, wgF_d.ap().rearrange("(k p) n -> p k n", p=128), "wgF", q="pool")
    bI = P.alloc([36, 1], F32); load(bI, bI_d.ap(), "bI")
    nbF = P.alloc([36, 1], F32); load(nbF, bF_d.ap(), "nbF")
    ts("dve", nbF, nbF, -1.0, None, ALU.mult, None, ["nbF"], ["nbF"])
    reset = P.alloc([36, 4096], F32); load(reset, c_reset.ap(), "reset")
    sel36 = P.alloc([36, 8, 128], F32); load(sel36, c_sel36.ap(), "sel36")
    IT = P.alloc([36, 4096], F32); SP = P.alloc([36, 4096], F32); NB = P.alloc([36, 4096], F32)
    Gt = P.alloc([36, 64], F32); Gn = P.alloc([36, 64], F32); amax = P.alloc([36, 64], F32)
    Mm = P.alloc([36, 64], F32); Mend = P.alloc([36, 64], F32); MP = P.alloc([36, 64], F32); DECs = P.alloc([36, 64], F32)
    for s in range(8):
        sl = slice(s * 512, (s + 1) * 512)
        for kc in range(8):
            mm(B[0][0:36, :], wgI[:, kc, :], xnT[:, kc, sl], kc == 0, kc == 7, ["wgI"] + XNT, [BK[0]])
        actf(IT[:, sl], B[0][0:36, :], AF.Identity, [BK[0], "bI"], ["IT"], bias=bI[:, 0:1])
        for kc in range(8):
            mm(B[1][0:36, :], wgF[:, kc, :], xnT[:, kc, sl], kc == 0, kc == 7, ["wgF"] + XNT, [BK[1]])
        actf(SP[:, sl], B[1][0:36, :], AF.Exp, [BK[1], "nbF"], ["SP"], bias=nbF[:, 0:1], scale=-1.0)
    actf(SP, SP, AF.Ln, ["SP"], ["SP"], bias=1.0)
    P.dve(lambda e: e.tensor_tensor_scan(out=NB, data0=reset, data1=SP, initial=0.0, op0=ALU.mult, op1=ALU.add), ["reset", "SP"], ["NB"])
    NB3 = NB.rearrange("p (c t) -> p c t", t=64); IT3 = IT.rearrange("p (c t) -> p c t", t=64); SP3 = SP.rearrange("p (c t) -> p c t", t=64)
    cp("dve", Gt, NB3[:, :, 63], ["NB"], ["Gt"])
    Gt3 = Gt.rearrange("p (c o) -> p c o", o=1)
    tt("dve", NB3[32:36], Gt3[32:36].to_broadcast([4, 64, 64]), NB3[32:36], ALU.subtract, ["Gt", "NB"], ["NB"])
    tt("dve", NB3[32:36], NB3[32:36], SP3[32:36], ALU.add, ["NB", "SP"], ["NB"])
    tt("dve", IT, IT, NB, ALU.add, ["IT", "NB"], ["IT"])
    P.dve(lambda e: e.tensor_reduce(out=amax, in_=IT3, axis=AX.X, op=ALU.max), ["IT"], ["amax"])
    ts("dve", Gn, Gt, -1.0, None, ALU.mult, None, ["Gt"], ["Gn"])
    memset("dve", Mm, 0.0, ["Mm"])
    P.dve(lambda e: e.tensor_tensor_scan(out=Mm[0:4, :], data0=amax[0:4, :], data1=Gn[0:4, :], initial=0.0, op0=ALU.max, op1=ALU.add), ["amax", "Gn"], ["Mm"])
    P.dve(lambda e: e.tensor_tensor_scan(out=Mm[32:36, ::-1], data0=amax[32:36, ::-1], data1=Gn[32:36, ::-1], initial=0.0, op0=ALU.max, op1=ALU.add), ["amax", "Gn"], ["Mm"])
    tt("dve", Mend, Mm, Gt, ALU.add, ["Mm", "Gt"], ["Mend"])
    memset("dve", MP, 0.0, ["MP"])
    cp("dve", MP[0:4, 1:64], Mm[0:4, 0:63], ["Mm", "MP"], ["MP"])
    cp("dve", MP[32:36, 0:63], Mm[32:36, 1:64], ["Mm", "MP"], ["MP"])
    tt("dve", DECs, MP, Mend, ALU.subtract, ["MP", "Mend"], ["DECs"])
    actf(DECs, DECs, AF.Exp, ["DECs"], ["DECs"])
    Mend3 = Mend.rearrange("p (c o) -> p c o", o=1).to_broadcast([36, 64, 64])
    tt("dve", IT3, IT3, Mend3, ALU.subtract, ["IT", "Mend"], ["IT"])
    actf(IT, IT, AF.Exp, ["IT"], ["IT"])
    tt("dve", NB3, NB3, Mend3, ALU.subtract, ["NB", "Mend"], ["NB"])
    actf(NB, NB, AF.Exp, ["NB"], ["NB"])
    for g8 in range(4):
        bk = g8 % 2
        for jj in range(8):
            j = g8 * 8 + jj
            tr(B[bk][:, jj * 36:(jj + 1) * 36], IT[0:36, j * 128:(j + 1) * 128], identf[0:36, 0:36], ["IT", "identf"], [BK[bk]])
        cp("dve", WS128[:, g8 * 8:(g8 + 1) * 8, :], B[bk][:, 0:288].rearrange("p (a b) -> p a b", a=8), [BK[bk]], ["WS128"])
    for g8 in range(8):
        bk = 2 + g8 % 2
        for cc in range(8):
            c = g8 * 8 + cc
            tr(B[bk][0:64, cc * 36:(cc + 1) * 36], NB[0:36, c * 64:(c + 1) * 64], identf[0:36, 0:36], ["NB", "identf"], [BK[bk]])
        cp("dve", LB64[:, g8 * 8:(g8 + 1) * 8, :], B[bk][0:64, 0:288].rearrange("p (a b) -> p a b", a=8), [BK[bk]], ["LB64"])
    for l in range(8):
        mm(B[4][:, l * 64:(l + 1) * 64], sel36[:, l, :], DECs, True, True, ["sel36", "DECs"], [BK[4]])
    cp("dve", DEC, B[4][:, 0:512].rearrange("p (a b) -> p a b", a=8), [BK[4]], ["DEC"])
    tap("WS128", WS128, [128, 32, 36], F32, ["WS128"])
    tap("LB64", LB64, [64, 64, 36], F32, ["LB64"])
    tap("DEC", DEC, [128, 8, 64], F32, ["DEC"])
    P.release()
    if stop <= 1:
        return finish(nc, P, out, tapd)

    P.phase = "p1b"
    P.mark()
    wq = P.alloc([128, 8, 128], BF16); wk = P.alloc([128, 8, 128], BF16)
    wv = P.alloc([128, 8, 128], BF16); wo = P.alloc([128, 8, 128], BF16)
    convq = P.alloc([128, 4, 3], F32); load(convq, convq_d.ap(), "convq")
    convk = P.alloc([128, 4, 3], F32); load(convk, convk_d.ap(), "convk")
    ghm = P.alloc([128, 4], F32); load(ghm, ghm_d.ap(), "ghm")
    bmask = P.alloc([128, 2, 128], F32); load(bmask, c_bmask.ap(), "bmask")
    preq = P.alloc([128, 4098], BF16); prek = P.alloc([128, 4098], BF16)
    ctmp = [P.alloc([128, 512], F32) for _ in range(2)]
    qT = P.alloc([128, 4096], BF16); kT = P.alloc([128, 4096], BF16)
    k_tm = P.alloc([128, 32, 128], BF16); v_tm = P.alloc([128, 32, 129], BF16)
    sigT = P.alloc([128, 4096], BF16)
    PT = P.alloc([128, 2, 32, 128], BF16)
    hacc = P.alloc([64, 64, 128], F32)
    hn = [P.alloc([64, 8, 128], BF16) for _ in range(2)]
    ymT = qT
    C32 = [[P.alloc([128, 129], F32) for _ in range(2)] for _ in range(2)]
    Cbf = [[P.alloc([128, 129], BF16) for _ in range(2)] for _ in range(2)]
    vp = [P.alloc([128, 129], BF16) for _ in range(4)]
    dtmp = [P.alloc([64, 2], F32) for _ in range(4)]
    hsq = P.alloc([64, 128], BF16)
    hss = P.alloc([64, 64], F32)
    memset("dve", v_tm[:, :, 128:129], 1.0, ["v_tm"])
    for h in range(4):
        hs = slice(0, 128)
        P.phase = "p1b_proj"
        for wt, wd, wkey_ in ((wq, wq_d, "wq"), (wk, wk_d, "wk"), (wv, wv_d, "wv"), (wo, wo_d, "wo")):
            P.dma(wt, wd[:, h * 128:(h + 1) * 128].rearrange("(k p) n -> p k n", p=128), writes=[wkey_], q="pool")
        qk = (("convq", wq, "wq", convq, qT, "qT", preq, "preq"), ("convk", wk, "wk", convk, kT, "kT", prek, "prek"))
        for cwk, wmat, wkey, cw, dst, dkey, pre, pk in qk:
            if h == 0:
                memset("dve", pre[:, 0:1], 0.0, [pk]); memset("dve", pre[:, 4097:4098], 0.0, [pk])
            for s in range(8):
                sl = slice(s * 512, (s + 1) * 512)
                bk = s % 2
                for kc in range(8):
                    mm(B[bk][:, :], wmat[:, kc, hs], xnT[:, kc, sl], kc == 0, kc == 7, [wkey] + XNT, [BK[bk]])
                actf(pre[:, 1 + s * 512:1 + (s + 1) * 512], B[bk][:, :], AF.Copy, [BK[bk]], [pk])
        for s in range(8):
            sl = slice(s * 512, (s + 1) * 512)
            bk = s % 2
            for kc in range(8):
                mm(B[bk][:, :], wo[:, kc, hs], xnT[:, kc, sl], kc == 0, kc == 7, ["wo"] + XNT, [BK[bk]])
            actf(sigT[:, sl], B[bk][:, :], AF.Sigmoid, [BK[bk]], ["sigT"])
        for cwk, wmat, wkey, cw, dst, dkey, pre, pk in qk:
            for s in range(8):
                c0 = s * 512
                tb = ctmp[s % 2]
                tk = ("ctmp", s % 2)
                ts("dve", tb, pre[:, c0:c0 + 512], cw[:, h, 0:1], None, ALU.mult, None, [pk, cwk], [tk])
                stt("dve", tb, pre[:, c0 + 1:c0 + 513], cw[:, h, 1:2], tb, ALU.mult, ALU.add, [pk, tk], [tk])
                stt("dve", tb, pre[:, c0 + 2:c0 + 514], cw[:, h, 2:3], tb, ALU.mult, ALU.add, [pk, tk], [tk])
                actf(dst[:, c0:c0 + 512], tb, AF.Silu, [tk], [dkey])
        for j in range(32):
            bk = 2 + j % 2
            for kc in range(8):
                mm(B[bk][:, 0:128], xnT[:, kc, j * 128:(j + 1) * 128], wv[:, kc, hs], kc == 0, kc == 7, ["wv"] + XNT, [BK[bk]])
            cp("dve", v_tm[:, j, 0:128], B[bk][:, 0:128], [BK[bk]], ["v_tm"])
        for j in range(32):
            bk = 6 + j % 2
            tr(Bb[bk][:, 0:128], kT[:, j * 128:(j + 1) * 128], identb, ["kT", "identb"], [BK[bk]])
            ts("dve", k_tm[:, j, :], Bb[bk][:, 0:128], 128 ** -0.5, None, ALU.mult, None, [BK[bk]], ["k_tm"])
        P.phase = "p1b_scan"
        for j in range(32):
            bk = j % 2
            mm(B[bk][:, 0:128], kT[:, j * 128:(j + 1) * 128], qT[:, j * 128:(j + 1) * 128], True, True, ["kT", "qT"], [BK[bk]])
            for d in range(2):
                lc = h + 32 * d
                stt("dve", PT[:, d, j, :], B[bk][:, 0:128], WS128[:, j, lc:lc + 1], bmask[:, d, :], ALU.mult, ALU.mult,
                    [BK[bk], "WS128", "bmask"], [("PT", d, j)])
        for d in range(2):
            memset("dve", C32[d][0], 0.0, [("C32", d, 0)])
        units = []
        for i in range(64):
            for d in range(2):
                units.append((i, d, i if d == 0 else 63 - i))

        def emit_dc(n):
            i, d, c = units[n]
            j, r = c // 2, c % 2
            rs_ = slice(r * 64, (r + 1) * 64)
            lc = h + 32 * d
            vpb = vp[n % 4]; vk = ("vp", n % 4)
            actf(vpb[rs_, :], v_tm[rs_, j, :], AF.Copy, ["v_tm", "WS128"], [vk], scale=WS128[rs_, j, lc:lc + 1])
            db = 4 + n % 2
            mm(B[db][:, 0:129], k_tm[rs_, j, :], vpb[rs_, :], True, True, ["k_tm", vk], [BK[db]])

        def emit_norm(n):
            i, d, c = units[n]
            lc = h + 32 * d
            nb_ = n % 4
            dt_ = dtmp[n % 4]; dk = ("dtmp", n % 4)
            actf(dt_[:, 0:1], B[nb_][0:64, 128:129], AF.Abs, [BK[nb_]], [dk])
            ts("dve", dt_[:, 0:1], dt_[:, 0:1], LB64[:, c, lc:lc + 1], None, ALU.max, None, [dk, "LB64"], [dk])
            recip(dt_[:, 1:2], dt_[:, 0:1], [dk], [dk])
            if i < 32:
                ts("dve", hacc[:, c, :], B[nb_][0:64, 0:128], dt_[:, 1:2], None, ALU.mult, None, [BK[nb_], dk], [("hacc", c)])
            else:
                stt("dve", hacc[:, c, :], B[nb_][0:64, 0:128], dt_[:, 1:2], hacc[:, c, :], ALU.mult, ALU.add, [BK[nb_], dk, ("hacc", c)], [("hacc", c)])

        emit_dc(0)
        for n in range(128):
            i, d, c = units[n]
            j, r = c // 2, c % 2
            rs_ = slice(r * 64, (r + 1) * 64)
            l = h + 4 * d
            if n >= 4:
                emit_norm(n - 4)
            if n + 1 < 128:
                emit_dc(n + 1)
            cur, nxt = C32[d][i % 2], C32[d][(i + 1) % 2]
            ck, nk = ("C32", d, i % 2), ("C32", d, (i + 1) % 2)
            cb = Cbf[d][i % 2]; cbk = ("Cbf", d, i % 2)
            actf(cb, cur, AF.Copy, [ck, "DEC"], [cbk], scale=DEC[:, l, c:c + 1])
            db = 4 + n % 2
            stt("dve", nxt, cur, DEC[:, l, c:c + 1], B[db][:, 0:129], ALU.mult, ALU.add, [ck, "DEC", BK[db]], [nk])
            nb_ = n % 4
            mm(B[nb_][0:64, 0:129], qT[:, c * 64:(c + 1) * 64], cb, True, False, ["qT", cbk], [BK[nb_]])
            mm(B[nb_][0:64, 0:129], PT[rs_, d, j, rs_], v_tm[rs_, j, :], False, True, [("PT", d, j), "v_tm"], [BK[nb_]])
        for n in range(124, 128):
            emit_norm(n)
        P.phase = "p1b_post"
        HA = [("hacc", c) for c in range(64)]
        for c in range(64):
            actf(hsq, hacc[:, c, :], AF.Square, [("hacc", c)], ["hsq", "hssss"], accum=hss[:, c:c + 1])
        rms_rstd(hss, hss, 128, "hss")
        for g8 in range(8):
            hb = hn[g8 % 2]; hk = ("hn", g8 % 2)
            for cc in range(8):
                c = g8 * 8 + cc
                ts("dve", hb[:, cc, :], hacc[:, c, :], hss[:, c:c + 1], None, ALU.mult, None, [("hacc", c), "hssrs"], [hk])
            bk = 6 + g8 % 2
            for cc in range(8):
                tr(Bb[bk][:, cc * 64:(cc + 1) * 64], hb[:, cc, :], identb[0:64, 0:64], [hk, "identb"], [BK[bk]])
            sl = slice(g8 * 512, (g8 + 1) * 512)
            stt("dve", ymT[:, sl], Bb[bk][:, 0:512], ghm[:, h:h + 1], sigT[:, sl], ALU.mult, ALU.mult, [BK[bk], "ghm", "sigT"], ["qT"])
        P.dma(ymT_d[h * 128:(h + 1) * 128, :], ymT, reads=["qT"], writes=[("ymT_d", h)])
    P.release()
    P.release()
    if stop <= 2:
        return finish(nc, P, out, tapd, extra_out=[("ymT_d", ymT_d, [512, 4096], BF16)])
    P.phase = "p1c"
    P.mark()
    sinT = P.alloc([96, 4096], BF16); cosT = P.alloc([96, 4096], BF16)
    cqnT = P.alloc([128, 2, 4096], BF16); ckvnT = P.alloc([128, 4096], BF16); krT = P.alloc([96, 4096], BF16)
    R_ = slice(64, 96)
    P.mark()
    posi = P.alloc([96, 4096], I32); load(posi[R_, :], pos.ap(), "posi")
    invf = P.alloc([128, 1], F32); load(invf, c_invf.ap(), "invf")
    ang = P.alloc([96, 4096], F32); kf = P.alloc([96, 4096], F32); fw = P.alloc([96, 4096], F32)
    cp("dve", ang[R_], posi[R_], ["posi"], ["ang"])
    ts("dve", ang[R_], ang[R_], invf[R_, 0:1], 1.0 / (2 * np.pi), ALU.mult, ALU.mult, ["ang", "invf"], ["ang"])
    cp("dve", posi[R_], ang[R_], ["ang"], ["posi"])
    cp("dve", kf[R_], posi[R_], ["posi"], ["kf"])
    tt("dve", ang[R_], ang[R_], kf[R_], ALU.subtract, ["ang", "kf"], ["ang"])
    for dstT, shift, dk_ in ((sinT, 0.0, "sinT"), (cosT, 0.25, "cosT")):
        ts("dve", fw[R_], ang[R_], shift, None, ALU.add, None, ["ang"], ["fw"])
        ts("dve", kf[R_], fw[R_], 0.5, None, ALU.is_gt, None, ["fw"], ["kf"])
        tt("dve", fw[R_], fw[R_], kf[R_], ALU.subtract, ["fw", "kf"], ["fw"])
        ts("dve", kf[R_], fw[R_], -0.5, None, ALU.is_lt, None, ["fw"], ["kf"])
        tt("dve", fw[R_], fw[R_], kf[R_], ALU.add, ["fw", "kf"], ["fw"])
        actf(dstT[R_], fw[R_], AF.Sin, ["fw"], [dk_], scale=6.283185)
    P.release()
    if stop <= 2.3:
        tap("sinT", sinT[64:96], [32, 4096], BF16, ["sinT"]); tap("cosT", cosT[64:96], [32, 4096], BF16, ["cosT"])
        return finish(nc, P, out, tapd)
    def wload(shape, src, key):
        t_ = P.alloc(shape, BF16); load(t_, src, key, q="pool"); return t_
    wcq = wload([128, 8, 256], wcq_d.ap().rearrange("(k p) n -> p k n", p=128), "wcq")
    wckv = wload([128, 8, 128], wckv_d.ap().rearrange("(k p) n -> p k n", p=128), "wckv")
    wkr = wload([128, 8, 96], wkr_d.ap().rearrange("(k p) n -> p k n", p=128), "wkr")
    wkrr = wload([128, 8, 96], wkrr_d.ap().rearrange("(k p) n -> p k n", p=128), "wkrr")
    wqb = wload([128, 2, 768], wqb_d.ap().rearrange("(k p) n -> p k n", p=128), "wqb")
    wqbr = wload([128, 2, 768], wqbr_d.ap().rearrange("(k p) n -> p k n", p=128), "wqbr")
    wkn = wload([128, 512], wkn_d.ap(), "wkn"); wv2 = wload([128, 512], wv2_d.ap(), "wv2")
    ts("dve", wkrr[:, :, 64:80], wkrr[:, :, 64:80], -1.0, None, ALU.mult, None, ["wkrr"], ["wkrr"])
    wqbr4 = wqbr.rearrange("p k (h c) -> p k h c", c=96)
    for k2 in range(2):
        ts("dve", wqbr4[:, k2, :, 64:80], wqbr4[:, k2, :, 64:80], -1.0, None, ALU.mult, None, ["wqbr"], ["wqbr"])
    gqa = P.alloc([128, 2], F32); load(gqa, gqa_d.ap(), "gqa")
    gkva = P.alloc([128, 1], F32); load(gkva, gkva_d.ap(), "gkva")
    gha = P.alloc([64, 8], F32); load(gha, gha_d.ap(), "gha")
    nsel = P.alloc([128, 64], BF16); load(nsel, c_nsel.ap(), "nsel", q="pool")
    v_allf = P.alloc([128, 32, 592], BF16)
    v_all = v_allf[:, :, 0:528].rearrange("p j (h c) -> p j h c", c=66)
    memset("pool", v_allf, 0.0, ["v_all"]); memset("dve", v_all[:, :, :, 64:65], 1.0, ["v_all"])
    t1 = P.alloc([96, 512], F32); t2 = P.alloc([96, 512], F32)
    P.mark()
    sqb = P.alloc([128, 2, 512], BF16); cqf = P.alloc([128, 2, 512], F32); rstd = P.alloc([128, 512], F32)
    sq2 = P.alloc([128, 512], BF16); ckf = P.alloc([128, 512], F32); rstd2 = P.alloc([128, 512], F32)
    for s in range(8):
        sl = slice(s * 512, (s + 1) * 512)
        for blk in range(2):
            for kc in range(8):
                mm(B[blk][:, :], wcq[:, kc, blk * 128:(blk + 1) * 128], xnT[:, kc, sl], kc == 0, kc == 7, ["wcq"] + XNT, [BK[blk]])
            actf(sqb[:, blk, :], B[blk][:, :], AF.Square, [BK[blk]], [("sqb", blk)])
            actf(cqf[:, blk, :], B[blk][:, :], AF.Copy, [BK[blk]], [("cqf", blk)])
        mm(B[2][:, :], onesb, sqb[:, 0, :], True, False, ["onesb", ("sqb", 0)], [BK[2]])
        mm(B[2][:, :], onesb, sqb[:, 1, :], False, True, ["onesb", ("sqb", 1)], [BK[2]])
        actf(rstd, B[2][:, :], AF.Sqrt, [BK[2]], ["rstd"], bias=EPS, scale=1.0 / 256)
        recip(rstd, rstd, ["rstd"], ["rstd"])
        for blk in range(2):
            stt("dve", cqnT[:, blk, sl], cqf[:, blk, :], gqa[:, blk:blk + 1], rstd, ALU.mult, ALU.mult, [("cqf", blk), "gqa", "rstd"], ["cqnT"])
        for kc in range(8):
            mm(B[3][:, :], wckv[:, kc, :], xnT[:, kc, sl], kc == 0, kc == 7, ["wckv"] + XNT, [BK[3]])
        actf(sq2, B[3][:, :], AF.Square, [BK[3]], ["sq2"])
        actf(ckf, B[3][:, :], AF.Copy, [BK[3]], ["ckf"])
        mm(B[4][:, :], onesb, sq2, True, True, ["onesb", "sq2"], [BK[4]])
        actf(rstd2, B[4][:, :], AF.Sqrt, [BK[4]], ["rstd2"], bias=EPS, scale=1.0 / 128)
        recip(rstd2, rstd2, ["rstd2"], ["rstd2"])
        stt("dve", ckvnT[:, sl], ckf, gkva[:, 0:1], rstd2, ALU.mult, ALU.mult, ["ckf", "gkva", "rstd2"], ["ckvnT"])
        for kc in range(8):
            mm(B[5][0:96, :], wkr[:, kc, :], xnT[:, kc, sl], kc == 0, kc == 7, ["wkr"] + XNT, [BK[5]])
        for kc in range(8):
            mm(B[6][0:96, :], wkrr[:, kc, :], xnT[:, kc, sl], kc == 0, kc == 7, ["wkrr"] + XNT, [BK[6]])
        tt("dve", t1[R_], B[5][R_, :], cosT[R_, sl], ALU.mult, [BK[5], "cosT"], ["t1"])
        tt("dve", t2[R_], B[6][R_, :], sinT[R_, sl], ALU.mult, [BK[6], "sinT"], ["t2"])
        tt("dve", krT[R_, sl], t1[R_], t2[R_], ALU.add, ["t1", "t2"], ["krT"])
    for j in range(32):
        bk = j % 2
        mm(B[bk][:, :], ckvnT[:, j * 128:(j + 1) * 128], wv2, True, True, ["ckvnT", "wv2"], [BK[bk]])
        cp("dve", v_all[:, j, :, 0:64], B[bk][:, :].rearrange("p (h d) -> p h d", h=8), [BK[bk]], ["v_all"])
    P.release()
    if stop <= 2.6:
        tap("cqnT", cqnT, [128, 2, 4096], BF16, ["cqnT"]); tap("ckvnT", ckvnT, [128, 4096], BF16, ["ckvnT"]); tap("krT", krT[64:96], [32, 4096], BF16, ["krT"])
        return finish(nc, P, out, tapd, extra_out=[("ymT_d", ymT_d, [512, 4096], BF16)])
    P.barrier()
    qTh_ = [P.alloc([128, 4096], BF16), xnT[:, 0, :]]
    kTh_ = [P.alloc([128, 4096], BF16), xnT[:, 1, :]]
    vh_ = [P.alloc([128, 32, 128], BF16), xnT[:, 2, :].rearrange("p (j c) -> p j c", c=128)]
    for pb in range(2):
        memset("dve", qTh_[pb], 0.0, [("qTh", pb)]); memset("pool", kTh_[pb], 0.0, [("kTh", pb)])
        memset("pool", vh_[pb], 0.0, [("vh", pb)]); memset("dve", vh_[pb][:, :, 64:65], 1.0, [("vh", pb)])
    PT2 = [P.alloc([128, 1024], BF16) for _ in range(2)]
    osq = P.alloc([128, 512], BF16); memset("dve", osq, 0.0, ["osq"]); ocp = P.alloc([64, 512], F32); rden = P.alloc([64, 512], F32)
    yab = [P.alloc([64, 512], BF16) for _ in range(2)]

    def pro_groups(h, s):
        pb = h % 2
        qTh, kTh = qTh_[pb], kTh_[pb]
        hc = slice(h * 96, (h + 1) * 96)
        sl = slice(s * 512, (s + 1) * 512)

        def g0():
            ph = P.phase; P.phase = "p1c_pro"
            for k2 in range(2):
                mm(B[5][0:96, :], wqb[:, k2, hc], cqnT[:, k2, sl], k2 == 0, k2 == 1, ["wqb", "cqnT"], [BK[5]])
            cp("dve", qTh[0:64, sl], B[5][0:64, :], [BK[5]], [("qTh", pb)])
            tt("dve", t1[R_], B[5][R_, :], cosT[R_, sl], ALU.mult, [BK[5], "cosT"], ["t1"])
            P.phase = ph

        def g1():
            ph = P.phase; P.phase = "p1c_pro"
            for k2 in range(2):
                mm(B[5][0:96, :], wqbr[:, k2, hc], cqnT[:, k2, sl], k2 == 0, k2 == 1, ["wqbr", "cqnT"], [BK[5]])
            tt("dve", t2[R_], B[5][R_, :], sinT[R_, sl], ALU.mult, [BK[5], "sinT"], ["t2"])
            tt("dve", qTh[R_, sl], t1[R_], t2[R_], ALU.add, ["t1", "t2"], [("qTh", pb)])
            P.phase = ph

        def g2():
            ph = P.phase; P.phase = "p1c_pro"
            mm(B[5][0:64, :], wkn[:, h * 64:(h + 1) * 64], ckvnT[:, sl], True, True, ["wkn", "ckvnT"], [BK[5]])
            cp("dve", kTh[0:64, sl], B[5][0:64, :], [BK[5]], [("kTh", pb)])
            P.phase = ph
        return [g0, g1, g2]

    def pro_step(h, s):
        for g in pro_groups(h, s):
            g()

    def pro_fin(h):
        pb = h % 2
        cp("dve", kTh_[pb][R_, :], krT[R_, :], ["krT"], [("kTh", pb)])
        cp("dve", vh_[pb][:, :, 0:64], v_all[:, :, h, 0:64], ["v_all"], [("vh", pb)])

    def attn(h, Q, inserts=()):
        inserts = list(inserts)
        pb_ = h % 2
        qTh, kTh, vh = qTh_[pb_], kTh_[pb_], vh_[pb_]
        ql = slice(Q * 512, (Q + 1) * 512)
        ob = OB_BASE + Q % 2

        def st_(kp):
            pb = kp % 2
            for u in range(2):
                kt = 2 * kp + u
                mm(B[2 * pb + u][:, :], kTh[:, kt * 128:(kt + 1) * 128], qTh[:, ql], True, True, [("kTh", pb_), ("qTh", pb_)], [BK[2 * pb + u]])
            actf(PT2[pb], TP[pb][:, :], AF.Exp, [BK[2 * pb], BK[2 * pb + 1]], [("PT2", pb)], scale=96 ** -0.5)
        st_(0)
        for kp in range(16):
            if kp + 1 < 16:
                st_(kp + 1)
            for u in range(2):
                kt = 2 * kp + u
                mm(B[ob][:, :], vh[:, kt, :], PT2[kp % 2][:, u * 512:(u + 1) * 512], kt == 0, kt == 31, [("vh", pb_), ("PT2", kp % 2)], [BK[ob]])
            if kp in (3, 8, 13) and inserts:
                inserts.pop(0)()
        actf(osq, B[ob][:, :], AF.Square, [BK[ob]], ["osq"])
        cp("dve", ocp, B[ob][0:64, :], [BK[ob]], ["ocp"])
        mm(B[4][0:64, :], nsel[:, :], osq[:, :], True, True, ["nsel", "osq"], [BK[4]])
        actf(rden, B[4][0:64, :], AF.Sqrt, [BK[4]], ["rden"])
        recip(rden, rden, ["rden"], ["rden"])
        yb = yab[Q % 2]; yk = ("yab", Q % 2)
        stt("dve", yb, ocp, gha[:, h:h + 1], rden, ALU.mult, ALU.mult, ["ocp", "gha", "rden"], [yk])
        P.dma(yaT_d[h, :, ql], yb, reads=[yk], writes=[("yaT_d", h, Q)])

    for s_ in range(8):
        pro_step(0, s_)
    pro_fin(0)
    P.phase = "p1c_attn"
    for h in range(NHEADS_MLA):
        for Q in range(8):
            attn(h, Q, pro_groups(h + 1, Q) if h + 1 < NHEADS_MLA else ())
        if h + 1 < NHEADS_MLA:
            pro_fin(h + 1)
    P.release()
    P.barrier()
    P.top = 0
    identb = P.alloc([128, 128], BF16); load(identb, c_identb.ap(), "identb")
    identf = P.alloc([128, 128], F32); load(identf, c_identf.ap(), "identf")
    if stop <= 3:
        return finish(nc, P, out, tapd, extra_out=[("ymT_d", ymT_d, [512, 4096], BF16), ("yaT_d", yaT_d, [8, 64, 4096], BF16)])

    P.phase = "p2"
    AFF = P.alloc([128, 32, 16], F32)
    P.mark()
    woutm = wload([128, 4, 1024], woutm_d.ap().rearrange("(k p) n -> p k n", p=128), "woutm")
    wouta = wload([128, 4, 1024], wouta_d.ap().rearrange("h p n -> (h p) n").rearrange("(k p) n -> p k n", p=128), "wouta")
    wmq = wload([128, 8, 1024], wmq_d.ap().rearrange("(k p) n -> p k n", p=128), "wmq")
    wmo = wload([128, 8, 1024], wmo_d.ap().rearrange("(k p) n -> p k n", p=128), "wmo")
    gmx = P.alloc([128, 8], F32); load(gmx, gmx_d.ap(), "gmx")
    gffn_b = P.alloc([128, 1024], F32); load(gffn_b, gffn_d.ap().to_broadcast([128, 1024]), "gffn_b")
    gffn_p = P.alloc([128, 8], F32); load(gffn_p, gffnp_d.ap(), "gffn_p")
    wr = P.alloc([128, 8, 16], F32); load(wr, wr_d.ap().rearrange("(k p) n -> p k n", p=128), "wr")
    memKT = P.alloc([128, 8, 256], BF16); memV = P.alloc([128, 2, 4, 257], BF16)
    memset("dve", memV[:, :, :, 256:257], 1.0, ["memV"])
    P.mark()
    wmk = wload([128, 8, 1024], wmk_d.ap().rearrange("(k p) n -> p k n", p=128), "wmk")
    wmv = wload([128, 8, 1024], wmv_d.ap().rearrange("(k p) n -> p k n", p=128), "wmv")
    gmkv = P.alloc([128, 8], F32); load(gmkv, gmkv_d.ap(), "gmkv")
    mx_ = P.alloc([128, 2, 1024], F32); load(mx_, mem.ap().rearrange("(j p) d -> p j d", p=128), "mx")
    mnb = P.alloc([128, 2, 1024], BF16); memnT = P.alloc([128, 8, 256], BF16)
    mss = P.alloc([128, 2], F32); mjunk = P.alloc([128, 1024], BF16)
    for j in range(2):
        actf(mjunk, mx_[:, j, :], AF.Square, ["mx"], ["mjunk", ("mss", j)], accum=mss[:, j:j + 1])
        actf(mss[:, j:j + 1], mss[:, j:j + 1], AF.Sqrt, [("mss", j)], [("mss", j)], bias=EPS, scale=1.0 / 1024)
        recip(mss[:, j:j + 1], mss[:, j:j + 1], [("mss", j)], [("mss", j)])
        ts("dve", mnb[:, j, :], mx_[:, j, :], mss[:, j:j + 1], None, ALU.mult, None, ["mx", ("mss", j)], [("mnb", j)])
    for kc in range(8):
        bk = 6 + kc % 2
        for j in range(2):
            tr(Bb[bk][:, j * 128:(j + 1) * 128], mnb[:, j, kc * 128:(kc + 1) * 128], identb, [("mnb", j), "identb"], [BK[bk]])
        ts("dve", memnT[:, kc, :], Bb[bk][:, 0:256], gmkv[:, kc:kc + 1], None, ALU.mult, None, [BK[bk], "gmkv"], ["memnT"])
    for blk in range(8):
        bk = blk % 2
        for kc in range(8):
            mm(B[bk][:, 0:256], wmk[:, kc, blk * 128:(blk + 1) * 128], memnT[:, kc, :], kc == 0, kc == 7, ["wmk", "memnT"], [BK[bk]])
        cp("dve", memKT[:, blk, :], B[bk][:, 0:256], [BK[bk]], ["memKT"])
    for mt in range(2):
        for hf in range(2):
            bk = 2 + hf
            for kc in range(8):
                mm(B[bk][:, :], memnT[:, kc, mt * 128:(mt + 1) * 128], wmv[:, kc, hf * 512:(hf + 1) * 512], kc == 0, kc == 7, ["wmv", "memnT"], [BK[bk]])
            cp("dve", memV[:, mt, 2 * hf:2 * hf + 2, 0:256], B[bk][:, :].rearrange("p (h d) -> p h d", h=2), [BK[bk]], ["memV"])
    P.release()
    xs2_ = [P.alloc([128, 4, 1024], F32) for _ in range(2)]; ymTs_ = [P.alloc([128, 4, 512], BF16) for _ in range(2)]; yaTs_ = [P.alloc([128, 4, 512], BF16) for _ in range(2)]
    xn2 = P.alloc([128, 4, 1024], BF16); xn2T = P.alloc([128, 8, 512], BF16); qmT = P.alloc([128, 8, 512], BF16)
    PmT = P.alloc([128, 4, 2, 512], BF16); om = P.alloc([128, 4, 1024], BF16); omT = P.alloc([128, 8, 512], BF16)
    x2T = P.alloc([128, 8, 128], F32); xn3 = P.alloc([128, 4, 1024], BF16)
    st2_ = [P.alloc([128, 16], F32) for _ in range(2)]; rtmp = P.alloc([128, 16], F32); lg = P.alloc([128, 16], F32); ex = P.alloc([128, 16], F32)
    junk2 = P.alloc([128, 1024], BF16)
    ymT_v = ymT_d.ap().rearrange("(k p) t -> p k t", p=128)
    yaT_v = yaT_d.ap().rearrange("h p t -> (h p) t").rearrange("(k p) t -> p k t", p=128)
    YD = [("ymT_d", h_) for h_ in range(4)]
    def stageA(s):
        pb = s % 2
        xs2, ymTs, yaTs, st2 = xs2_[pb], ymTs_[pb], yaTs_[pb], st2_[pb]
        sl = slice(s * 512, (s + 1) * 512)
        P.dma(xs2, x[s * 512:(s + 1) * 512, :].rearrange("(j p) d -> p j d", p=128), writes=[("xs2", pb)])
        P.dma(ymTs, ymT_v[:, :, sl], reads=YD, writes=[("ymTs", pb)])
        P.dma(yaTs, yaT_v[:, :, sl], reads=[("yaT_d", h_, s) for h_ in range(8)], writes=[("yaTs", pb)])
        for j in range(4):
            tl = slice(j * 128, (j + 1) * 128)
            for hf in range(2):
                fl = slice(hf * 512, (hf + 1) * 512)
                bk = (2 * j + hf) % 4
                for blk in range(4):
                    mm(B[bk][:, :], ymTs[:, blk, tl], woutm[:, blk, fl], blk == 0, False, [("ymTs", pb), "woutm"], [BK[bk]])
                for blk in range(4):
                    mm(B[bk][:, :], yaTs[:, blk, tl], wouta[:, blk, fl], False, blk == 3, [("yaTs", pb), "wouta"], [BK[bk]])
                tt("dve", xs2[:, j, fl], xs2[:, j, fl], B[bk][:, :], ALU.add, [("xs2", pb), BK[bk]], [("xs2", pb)])
        for j in range(4):
            actf(junk2, xs2[:, j, :], AF.Square, [("xs2", pb)], ["junk2", ("st2", pb, j)], accum=st2[:, j:j + 1])
            actf(st2[:, j:j + 1], st2[:, j:j + 1], AF.Sqrt, [("st2", pb, j)], [("st2", pb, j)], bias=EPS, scale=1.0 / 1024)
            recip(st2[:, j:j + 1], st2[:, j:j + 1], [("st2", pb, j)], [("st2", pb, j)])
            ts("dve", xn2[:, j, :], xs2[:, j, :], st2[:, j:j + 1], None, ALU.mult, None, [("xs2", pb), ("st2", pb, j)], ["xn2"])
        for kc in range(8):
            bk = 6 + kc % 2
            for j in range(4):
                tr(Bb[bk][:, j * 128:(j + 1) * 128], xn2[:, j, kc * 128:(kc + 1) * 128], identb, ["xn2", "identb"], [BK[bk]])
            if kc % 2:
                ts("dve", xn2T[:, kc, :], Bb[bk][:, 0:512], gmx[:, kc:kc + 1], None, ALU.mult, None, [BK[bk], "gmx"], ["xn2T"])
            else:
                actf(xn2T[:, kc, :], Bb[bk][:, 0:512], AF.Copy, [BK[bk], "gmx"], ["xn2T"], scale=gmx[:, kc:kc + 1])
        for blk in range(8):
            bk = blk % 2
            for kc in range(8):
                mm(B[bk][:, :], wmq[:, kc, blk * 128:(blk + 1) * 128], xn2T[:, kc, :], kc == 0, kc == 7, ["wmq", "xn2T"], [BK[bk]])
            if blk % 2:
                cp("dve", qmT[:, blk, :], B[bk][:, :], [BK[bk]], ["qmT"])
            else:
                actf(qmT[:, blk, :], B[bk][:, :], AF.Copy, [BK[bk]], ["qmT"])
    def stageB(s):
        pb = s % 2
        xs2, ymTs, yaTs, st2 = xs2_[pb], ymTs_[pb], yaTs_[pb], st2_[pb]
        for h_ in range(4):
            for mc in range(2):
                bk = 2 + (2 * h_ + mc) % 2
                ml = slice(mc * 128, (mc + 1) * 128)
                mm(B[bk][:, :], memKT[:, 2 * h_, ml], qmT[:, 2 * h_, :], True, False, ["memKT", "qmT"], [BK[bk]])
                mm(B[bk][:, :], memKT[:, 2 * h_ + 1, ml], qmT[:, 2 * h_ + 1, :], False, True, ["memKT", "qmT"], [BK[bk]])
                actf(PmT[:, h_, mc, :], B[bk][:, :], AF.Exp, [BK[bk]], ["PmT"], scale=1.0 / 16)
        for j in range(4):
            tl = slice(j * 128, (j + 1) * 128)
            for h_ in range(4):
                i_ = 4 * j + h_
                bk = 4 + i_ % 2
                mm(B[bk][:, 0:257], PmT[:, h_, 0, tl], memV[:, 0, h_, :], True, False, ["PmT", "memV"], [BK[bk]])
                mm(B[bk][:, 0:257], PmT[:, h_, 1, tl], memV[:, 1, h_, :], False, True, ["PmT", "memV"], [BK[bk]])
                recip(rtmp[:, i_:i_ + 1], B[bk][:, 256:257], [BK[bk]], [("rtmp", i_)])
                ts("dve", om[:, j, h_ * 256:(h_ + 1) * 256], B[bk][:, 0:256], rtmp[:, i_:i_ + 1], None, ALU.mult, None, [BK[bk], ("rtmp", i_)], ["om"])
        for kc in range(8):
            bk = 6 + kc % 2
            for j in range(4):
                tr(Bb[bk][:, j * 128:(j + 1) * 128], om[:, j, kc * 128:(kc + 1) * 128], identb, ["om", "identb"], [BK[bk]])
            if kc % 2:
                cp("dve", omT[:, kc, :], Bb[bk][:, 0:512], [BK[bk]], ["omT"])
            else:
                actf(omT[:, kc, :], Bb[bk][:, 0:512], AF.Copy, [BK[bk]], ["omT"])
        for j in range(4):
            tl = slice(j * 128, (j + 1) * 128)
            for hf in range(2):
                fl = slice(hf * 512, (hf + 1) * 512)
                bk = (2 * j + hf) % 4
                for kc in range(8):
                    mm(B[bk][:, :], omT[:, kc, tl], wmo[:, kc, fl], kc == 0, kc == 7, ["omT", "wmo"], [BK[bk]])
                tt("dve", xs2[:, j, fl], xs2[:, j, fl], B[bk][:, :], ALU.add, [("xs2", pb), BK[bk]], [("xs2", pb)])
    def stageC(s):
        pb = s % 2
        xs2, ymTs, yaTs, st2 = xs2_[pb], ymTs_[pb], yaTs_[pb], st2_[pb]
        P.dma(acc_d[s * 512:(s + 1) * 512, :].rearrange("(j p) d -> p j d", p=128), xs2, reads=[("xs2", pb)], writes=["acc_d"])
        for j in range(4):
            t = s * 4 + j
            q_ = 4 + j
            actf(junk2, xs2[:, j, :], AF.Square, [("xs2", pb)], ["junk2", ("st2", pb, q_)], accum=st2[:, q_:q_ + 1])
            actf(st2[:, q_:q_ + 1], st2[:, q_:q_ + 1], AF.Sqrt, [("st2", pb, q_)], [("st2", pb, q_)], bias=EPS, scale=1.0 / 1024)
            recip(st2[:, q_:q_ + 1], st2[:, q_:q_ + 1], [("st2", pb, q_)], [("st2", pb, q_)])
            stt("dve", xn3[:, j, :], xs2[:, j, :], st2[:, q_:q_ + 1], gffn_b, ALU.mult, ALU.mult, [("xs2", pb), ("st2", pb, q_), "gffn_b"], ["xn3"])
            for kc in range(8):
                bk = kc % 2
                tr(B[bk][:, 0:128], xs2[:, j, kc * 128:(kc + 1) * 128], identf, [("xs2", pb), "identf"], [BK[bk]])
                if kc % 2:
                    ts("dve", x2T[:, kc, :], B[bk][:, 0:128], gffn_p[:, kc:kc + 1], None, ALU.mult, None, [BK[bk], "gffn_p"], [("x2T", kc)])
                else:
                    actf(x2T[:, kc, :], B[bk][:, 0:128], AF.Copy, [BK[bk], "gffn_p"], [("x2T", kc)], scale=gffn_p[:, kc:kc + 1])
            for kc in range(8):
                mm(B[2][:, 0:16], x2T[:, kc, :], wr[:, kc, :], kc == 0, kc == 7, [("x2T", kc), "wr"], [BK[2]])
            ts("dve", lg, B[2][:, 0:16], st2[:, q_:q_ + 1], None, ALU.mult, None, [BK[2], ("st2", pb, q_)], ["lg"])
            P.dve(lambda e: e.tensor_reduce(out=ex[:, 0:1], in_=lg, axis=AX.X, op=ALU.max), ["lg"], ["exm"])
            ts("dve", ex[:, 0:1], ex[:, 0:1], -1.0, None, ALU.mult, None, ["exm"], ["exm"])
            actf(lg, lg, AF.Exp, ["lg", "exm"], ["lg", "exs"], bias=ex[:, 0:1], accum=ex[:, 1:2])
            recip(ex[:, 1:2], ex[:, 1:2], ["exs"], ["exs"])
            ts("dve", AFF[:, t, :], lg, ex[:, 1:2], None, ALU.mult, None, ["lg", "exs"], ["AFF"])
        P.dma(h_d[s * 512:(s + 1) * 512, :].rearrange("(j p) d -> p j d", p=128), xn3, reads=["xn3"], writes=["h_d"])
    stageA(0)
    for s in range(8):
        stageB(s)
        if s + 1 < 8:
            stageA(s + 1)
        stageC(s)
    tap("AFF", AFF, [128, 32, 16], F32, ["AFF"])
    P.release()
    if stop <= 4:
        return finish(nc, P, out, tapd, extra_out=[("acc_d", acc_d, [4096, 1024], F32)])

    P.phase = "p3"
    P.mark()
    iota = P.alloc([128, 512], F32); load(iota, c_iota.ap(), "iota")
    posm = P.alloc([128, 32, 16], F32)
    RH = P.alloc([128, 32, 16, 4], BF16)
    P.mark()
    onesf = P.alloc([128, 128], F32); load(onesf, c_onesf.ap(), "onesf")
    Uf = P.alloc([128, 128], F32); load(Uf, c_U.ap(), "Uf")
    jp = P.alloc([128, 32, 2], F32); load(jp, c_jp.ap(), "jp")
    lo = P.alloc([128, 16], F32); mid = P.alloc([128, 16], F32); tot = P.alloc([128, 16], F32); tq = P.alloc([128, 16], F32)
    sel = P.alloc([128, 32, 16], F32); cum = P.alloc([128, 32, 16], F32)
    ones32 = P.alloc([128, 32], F32); memset("dve", ones32, 1.0, ["ones32"])
    afh = P.alloc([128, 32, 16], BF16); afl = P.alloc([128, 32, 16], F32)
    memset("dve", lo, 0.0, ["lo"])
    mid3 = mid.rearrange("p (o e) -> p o e", o=1).to_broadcast([128, 32, 16])
    lo3 = lo.rearrange("p (o e) -> p o e", o=1).to_broadcast([128, 32, 16])
    self2 = sel.rearrange("p j e -> p (j e)")
    for it in range(24):
        step = 0.5 ** (it + 1)
        ts("dve", mid, lo, step, None, ALU.add, None, ["lo"], ["mid"])
        tt("dve", sel, AFF, mid3, ALU.is_ge, ["AFF", "mid"], ["sel"])
        mm(B[0][:, :], onesf, self2, True, True, ["onesf", "sel"], [BK[0]])
        P.dve(lambda e: e.tensor_reduce(out=tot, in_=B[0][:, :].rearrange("p (j e) -> p e j", e=16), axis=AX.X, op=ALU.add), [BK[0]], ["tot"])
        stt("dve", tq, tot, 512.0, mid, ALU.is_ge, ALU.mult, ["tot", "mid"], ["tq"])
        tt("dve", lo, lo, tq, ALU.max, ["lo", "tq"], ["lo"])
    tt("dve", sel, AFF, lo3, ALU.is_ge, ["AFF", "lo"], ["sel"])
    for e_ in range(16):
        P.dve(lambda e, e_=e_: e.tensor_tensor_scan(out=cum[:, :, e_], data0=ones32, data1=sel[:, :, e_], initial=0.0, op0=ALU.mult, op1=ALU.add), ["ones32", "sel"], ["cum"])
    tt("dve", cum, cum, sel, ALU.subtract, ["cum", "sel"], ["cum"])
    mm(B[1][:, :], onesf, cum.rearrange("p j e -> p (j e)"), True, False, ["onesf", "cum"], [BK[1]])
    mm(B[1][:, :], Uf, self2, False, True, ["Uf", "sel"], [BK[1]])
    stt("dve", posm.rearrange("p j e -> p (j e)"), B[1][:, :], 1.0, self2, ALU.add, ALU.mult, [BK[1], "sel"], ["posm"])
    ts("dve", posm, posm, -1.0, None, ALU.add, None, ["posm"], ["posm"])
    cp("dve", afh, AFF, ["AFF"], ["afh"])
    tt("dve", afl, AFF, afh, ALU.subtract, ["AFF", "afh"], ["afl"])
    cp("dve", RH[:, :, :, 2], afh, ["afh"], ["RH"])
    cp("dve", RH[:, :, :, 3], afl, ["afl", "RH"], ["RH"])
    for e_ in range(16):
        cp("dve", RH[:, :, e_, 0:2], jp, ["jp", "RH"], ["RH"])
    P.release()
    P.phase = "p3_alloc"
    OH = P.alloc([128, 32, 512], BF16)
    res = P.alloc([128, 16], F32); idxf = [P.alloc([128, 4], F32) for _ in range(2)]; idxi = [P.alloc([128, 4], I32) for _ in range(2)]
    gate_ = [P.alloc([128, 4], F32) for _ in range(2)]
    xe = [P.alloc([128, 4, 1024], BF16) for _ in range(2)]
    xeT = P.alloc([128, 8, 512], BF16); hidT = P.alloc([128, 16, 512], BF16)
    sg = [P.alloc([128, 512], F32) for _ in range(2)]
    yacc = [P.alloc([128, 1024], F32) for _ in range(4)]
    NGU, ND = 4, 4
    gub = [P.alloc([128, 2, 8, 512], BF16) for _ in range(NGU)]
    dbf = [P.alloc([128, 4, 1024], BF16) for _ in range(ND)]

    def route_oh(e_, j0, j1):
        ph = P.phase; P.phase = "p3_route"
        for j in range(j0, j1):
            ts("dve", OH[:, j, :], iota, posm[:, j, e_:e_ + 1], None, ALU.is_equal, None, ["iota", "posm"], [("OH", j)])
        P.phase = ph

    def route_rest(e_):
        ph = P.phase; P.phase = "p3_route"
        p_ = e_ % 2
        for sc in range(4):
            for j in range(32):
                mm(B[0][:, sc * 4:(sc + 1) * 4], OH[:, j, sc * 128:(sc + 1) * 128], RH[:, j, e_, :], j == 0, j == 31, [("OH", j), "RH"], [BK[0]])
        cp("dve", res, B[0][:, 0:16], [BK[0]], ["res"])
        r3 = res.rearrange("p (s c) -> p s c", c=4)
        stt("dve", idxf[p_], r3[:, :, 0], 128.0, r3[:, :, 1], ALU.mult, ALU.add, ["res"], [("idxf", p_)])
        tt("dve", gate_[p_], r3[:, :, 2], r3[:, :, 3], ALU.add, ["res"], [("gate", p_)])
        cp("dve", idxi[p_], idxf[p_], [("idxf", p_)], [("idxi", p_)])
        for sc in range(4):
            P.op("pool", lambda e, sc=sc, p_=p_: e.indirect_dma_start(out=xe[p_][:, sc, :], out_offset=None, in_=h_d[:, :],
                                                                in_offset=bass.IndirectOffsetOnAxis(ap=idxi[p_][:, sc:sc + 1], axis=0)),
                 reads=[("idxi", p_), "h_d"], writes=[("xe", p_, sc)], dma=True)
        P.phase = ph

    def wload_gu(e_, q4):
        ph = P.phase; P.phase = "p3_wload"
        g_ = gub[q4 % NGU]
        P.dma(g_[:, 0, :, :], weg_d[e_, :, q4 * 512:(q4 + 1) * 512].rearrange("(k p) n -> p k n", p=128), writes=[("gub", q4 % NGU, 0)], q="pool")
        P.dma(g_[:, 1, :, :], weu_d[e_, :, q4 * 512:(q4 + 1) * 512].rearrange("(k p) n -> p k n", p=128), writes=[("gub", q4 % NGU, 1)], q="pool")
        P.phase = ph

    def wload_d(e_, q4):
        ph = P.phase; P.phase = "p3_wload"
        P.dma(dbf[q4 % ND], wed_d[e_, q4 * 512:(q4 + 1) * 512, :].rearrange("(k p) n -> p k n", p=128), writes=[("dbf", q4 % ND)], q="pool")
        P.phase = ph

    def wloads(e_):
        for q4 in range(4):
            wload_gu(e_, q4)
        for q4 in range(4):
            wload_d(e_, q4)

    def compute(e_):
        P.phase = "p3_xT"
        p_ = e_ % 2
        for kc in range(8):
            for sc in range(4):
                tr(Bb[7][:, sc * 128:(sc + 1) * 128], xe[p_][:, sc, kc * 128:(kc + 1) * 128], identb, [("xe", p_, sc), "identb"], [BK[7]])
            if kc % 2:
                cp("dve", xeT[:, kc, :], Bb[7][:, 0:512], [BK[7]], ["xeT"])
            else:
                actf(xeT[:, kc, :], Bb[7][:, 0:512], AF.Copy, [BK[7]], ["xeT"])
        P.phase = "p3_gu"
        for fc in range(16):
            q4, f4 = fc // 4, fc % 4
            g_ = gub[q4 % NGU]
            bg, bu = 1 + (fc % 2) * 2, 2 + (fc % 2) * 2
            for kc in range(8):
                mm(B[bg][:, :], g_[:, 0, kc, f4 * 128:(f4 + 1) * 128], xeT[:, kc, :], kc == 0, kc == 7, [("gub", q4 % NGU, 0), "xeT"], [BK[bg]])
            for kc in range(8):
                mm(B[bu][:, :], g_[:, 1, kc, f4 * 128:(f4 + 1) * 128], xeT[:, kc, :], kc == 0, kc == 7, [("gub", q4 % NGU, 1), "xeT"], [BK[bu]])
            actf(sg[fc % 2], B[bg][:, :], AF.Silu, [BK[bg]], [("sg", fc % 2)])
            tt("dve", hidT[:, fc, :], sg[fc % 2], B[bu][:, :], ALU.mult, [("sg", fc % 2), BK[bu]], [("hidT", fc)])
            if e_ + 1 < 16:
                route_oh(e_ + 1, 2 * fc, 2 * fc + 2)
            if f4 == 3 and e_ + 1 < 16:
                wload_gu(e_ + 1, q4)
        if e_ + 1 < 16:
            route_rest(e_ + 1)
        P.phase = "p3_down"
        gi = 0
        for q4 in range(4):
            for st_ in range(4):
                for hf in range(2):
                    bk = 5 + gi % 2
                    gi += 1
                    fl = slice(hf * 512, (hf + 1) * 512)
                    for f4 in range(4):
                        fc = q4 * 4 + f4
                        mm(B[bk][:, :], hidT[:, fc, st_ * 128:(st_ + 1) * 128], dbf[q4 % ND][:, f4, fl], f4 == 0, f4 == 3,
                           [("hidT", fc), ("dbf", q4 % ND)], [BK[bk]])
                    yk = ("ya", st_, hf)
                    if q4 == 0:
                        actf(yacc[st_][:, fl], B[bk][:, :], AF.Copy, [BK[bk], ("gate", p_)], [yk], scale=gate_[p_][:, st_:st_ + 1])
                    else:
                        stt("dve", yacc[st_][:, fl], B[bk][:, :], gate_[p_][:, st_:st_ + 1], yacc[st_][:, fl], ALU.mult, ALU.add,
                            [BK[bk], ("gate", p_), yk], [yk])
            if e_ + 1 < 16:
                wload_d(e_ + 1, q4)
        for st_ in range(4):
            P.op("pool", lambda e, st_=st_, p_=p_: e.indirect_dma_start(out=acc_d[:, :], out_offset=bass.IndirectOffsetOnAxis(ap=idxi[p_][:, st_:st_ + 1], axis=0),
                                                                in_=yacc[st_], in_offset=None, compute_op=ALU.add),
                 reads=[("idxi", p_), ("ya", st_, 0), ("ya", st_, 1), "acc_d"], writes=["acc_d"], dma=True)

    route_oh(0, 0, 32)
    route_rest(0)
    wloads(0)
    for e_ in range(16):
        compute(e_)
    P.release()
    if stop <= 5:
        return finish(nc, P, out, tapd, extra_out=[("acc_d", acc_d, [4096, 1024], F32)])

    P.phase = "p4"
    gfin_b = P.alloc([128, 1024], F32); load(gfin_b, gfin_d.ap().to_broadcast([128, 1024]), "gfin_b")
    xf = [P.alloc([128, 4, 1024], F32) for _ in range(2)]
    of = [P.alloc([128, 4, 1024], F32) for _ in range(2)]
    fs = P.alloc([128, 32], F32); junk3 = P.alloc([128, 1024], BF16)
    for s in range(8):
        b = s % 2
        P.dma(xf[b], acc_d[s * 512:(s + 1) * 512, :].rearrange("(j p) d -> p j d", p=128), reads=["acc_d"], writes=[("xf", b)])
        for j in range(4):
            t = s * 4 + j
            actf(junk3, xf[b][:, j, :], AF.Square, [("xf", b)], ["junk3", ("fs", t)], accum=fs[:, t:t + 1])
            actf(fs[:, t:t + 1], fs[:, t:t + 1], AF.Sqrt, [("fs", t)], [("fs", t)], bias=EPS, scale=1.0 / 1024)
            recip(fs[:, t:t + 1], fs[:, t:t + 1], [("fs", t)], [("fs", t)])
            stt("dve", of[b][:, j, :], xf[b][:, j, :], fs[:, t:t + 1], gfin_b, ALU.mult, ALU.mult, [("xf", b), ("fs", t), "gfin_b"], [("of", b)])
        P.dma(out[s * 512:(s + 1) * 512, :].rearrange("(j p) d -> p j d", p=128), of[b], reads=[("of", b)], writes=[("out", s)])
    return finish(nc, P, out, tapd)


def finish(nc, P, out, tapd, extra_out=()):
    for name, src, shp, dt in extra_out:
        t = nc.dram_tensor("tap_" + name, list(shp), dt, kind="ExternalOutput")
        tapd[name] = t
        P.barrier()
        P.dma(t.ap(), src.ap(), writes=[("tap", name)])
    P.barrier()
    P.emit()
    return nc, P, tapd


def _bf(a):
    return np.ascontiguousarray(a).astype(ml_dtypes.bfloat16)


def _pk(v, k):
    return np.ascontiguousarray(np.asarray(v, np.float32).reshape(k, 128).T)


def const_inputs():
    c = {}
    c["c_identb"] = _bf(np.eye(128, dtype=np.float32))
    c["c_identf"] = np.eye(128, dtype=np.float32)
    c["c_onesb"] = _bf(np.ones((128, 128), np.float32))
    c["c_onesf"] = np.ones((128, 128), np.float32)
    s = np.arange(128)[:, None]; t = np.arange(128)[None, :]
    same = (s // 64) == (t // 64)
    bm = np.zeros((128, 2, 128), np.float32)
    bm[:, 0, :] = (same & (s <= t)); bm[:, 1, :] = (same & (s >= t))
    bm *= np.float32(128 ** -0.5)
    c["c_bmask"] = bm
    sel = np.zeros((36, 8, 128), np.float32)
    for l, p in enumerate(LANE_PART):
        sel[p, l, :] = 1.0
    c["c_sel36"] = sel
    ns = np.zeros((128, 64), np.float32); ns[0:64, :] = 1.0 / 64; ns[64, :] = EPS
    c["c_nsel"] = ns
    invf = np.zeros((128, 1), np.float32)
    f = (10000.0 ** (-np.arange(0, 32, 2, dtype=np.float32) / 32)).astype(np.float32)
    invf[64:80, 0] = f; invf[80:96, 0] = f
    c["c_invf"] = invf
    r = np.ones((36, 4096), np.float32); r[:, ::64] = 0.0
    c["c_reset"] = r
    c["c_iota"] = np.tile(np.arange(512, dtype=np.float32)[None, :], (128, 1))
    c["c_tokid"] = (np.arange(32)[None, :] * 128 + np.arange(128)[:, None]).astype(np.float32)
    c["c_U"] = (s < t).astype(np.float32)
    jp = np.zeros((128, 32, 2), np.float32); jp[:, :, 0] = np.arange(32)[None, :]; jp[:, :, 1] = np.arange(128)[:, None]
    c["c_jp"] = jp
    return c


def weight_inputs(I):
    g = lambda k: np.asarray(I[k], np.float32)
    w = {}
    w_in = g("w_in")[0]
    w["gmix"] = _pk(g("g_mix")[0], 8)
    w["w_q"] = np.ascontiguousarray(w_in[:, 0:512]); w["w_k"] = np.ascontiguousarray(w_in[:, 512:1024])
    w["w_v"] = np.ascontiguousarray(w_in[:, 1024:1536]); w["w_o"] = np.ascontiguousarray(w_in[:, 1536:2048])
    gt = w_in[:, 2048:2064]; bg = g("b_gates")[0]
    wI = np.zeros((1024, 36), np.float32); wF = np.zeros((1024, 36), np.float32)
    bI = np.zeros((36, 1), np.float32); bF = np.zeros((36, 1), np.float32)
    wI[:, 0:4] = gt[:, 0:4]; wI[:, 32:36] = gt[:, 8:12]; wF[:, 0:4] = gt[:, 4:8]; wF[:, 32:36] = gt[:, 12:16]
    bI[0:4, 0] = bg[0:4]; bI[32:36, 0] = bg[8:12]; bF[0:4, 0] = bg[4:8]; bF[32:36, 0] = bg[12:16]
    w["w_gI"], w["w_gF"], w["b_I"], w["b_F"] = wI, wF, bI, bF
    w["w_cq"] = np.ascontiguousarray(w_in[:, 2064:2320]); w["w_ckv"] = np.ascontiguousarray(w_in[:, 2320:2448])
    kr = w_in[:, 2448:2480]
    wkr = np.zeros((1024, 96), np.float32); wkr[:, 64:96] = kr
    wkrr = np.zeros((1024, 96), np.float32); wkrr[:, 64:80] = kr[:, 16:32]; wkrr[:, 80:96] = kr[:, 0:16]
    w["w_kr"], w["w_krr"] = wkr, wkrr
    cv = g("conv_qk")[0]
    w["convq"] = np.ascontiguousarray(cv[:, 0:512].reshape(3, 4, 128).transpose(2, 1, 0))
    w["convk"] = np.ascontiguousarray(cv[:, 512:1024].reshape(3, 4, 128).transpose(2, 1, 0))
    w["g_qa"] = _pk(g("g_q_a")[0], 2); w["g_kva"] = _pk(g("g_kv_a")[0], 1)
    wqb = g("w_q_b")[0]
    w["w_qb"] = wqb
    wqbr = np.zeros_like(wqb).reshape(256, 8, 96); q3 = wqb.reshape(256, 8, 96)
    wqbr[:, :, 64:80] = q3[:, :, 80:96]; wqbr[:, :, 80:96] = q3[:, :, 64:80]
    w["w_qbr"] = np.ascontiguousarray(wqbr.reshape(256, 768))
    kv3 = g("w_kv_b")[0].reshape(128, 8, 128)
    w["w_kn"] = np.ascontiguousarray(kv3[:, :, 0:64].reshape(128, 512)); w["w_v2"] = np.ascontiguousarray(kv3[:, :, 64:128].reshape(128, 512))
    w["g_hm"] = _pk(g("g_head_mlstm")[0], 4)
    w["g_ha"] = np.ascontiguousarray(g("g_head_mla")[0].reshape(8, 64).T)
    wout = g("w_out")[0]
    w["w_outm"] = np.ascontiguousarray(wout[0:512]); w["w_outa"] = np.ascontiguousarray(wout[512:1024].reshape(8, 64, 1024))
    w["g_mx"] = _pk(g("g_mem_x")[0], 8); w["g_mkv"] = _pk(g("g_mem_kv")[0], 8)
    w["w_mq"], w["w_mk"], w["w_mv"], w["w_mo"] = g("w_mem_q")[0], g("w_mem_k")[0], g("w_mem_v")[0], g("w_mem_o")[0]
    w["g_ffnp"] = _pk(g("g_ffn")[0], 8); w["g_ffn"] = g("g_ffn")[0].reshape(1, 1024); w["g_fin"] = g("g_final").reshape(1, 1024)
    w["w_r"] = g("w_router")[0]
    w["w_eg"], w["w_eu"], w["w_ed"] = g("w_exp_gate")[0], g("w_exp_up")[0], g("w_exp_down")[0]
    return w


def core_inputs(I, b, shared):
    m = dict(shared)
    m["x"] = np.ascontiguousarray(np.asarray(I["x"], np.float32)[b])
    m["mem"] = np.ascontiguousarray(np.asarray(I["mem"], np.float32)[b])
    m["pos"] = np.ascontiguousarray(np.tile(np.asarray(I["positions"], np.int32)[b][None, :], (32, 1)))
    return m


_CACHE = {}


def kernel(**inputs):
    if "nc" not in _CACHE:
        _CACHE["nc"] = build()[0]
    nc = _CACHE["nc"]
    shared = const_inputs()
    shared.update(weight_inputs(inputs))
    in_maps = [core_inputs(inputs, c % 4, shared) for c in range(8)]
    res = run_bass_kernel_spmd(nc, in_maps, core_ids=list(range(8)))
    return np.stack([np.asarray(res.results[b]["out"], np.float32) for b in range(4)], axis=0)
```

```python
from contextlib import ExitStack
import numpy as np
import ml_dtypes
import concourse.bass as bass
import concourse.mybir as mybir
from concourse.bass_utils import run_bass_kernel_spmd

F32 = mybir.dt.float32
BF16 = mybir.dt.bfloat16
I32 = mybir.dt.int32
AF = mybir.ActivationFunctionType
ALU = mybir.AluOpType
AX = mybir.AxisListType

SEM_CH = 1000
NDMA_SEM = 12
ARENA_WORDS = 53000
EPS = 1e-6


class Op:
    __slots__ = ("eng", "fn", "deps", "signal", "semval", "idx", "dma", "dsem", "dval", "phase")


class Prog:
    ENGS = ("pe", "act", "dve", "pool", "sp")

    def __init__(self, nc):
        self.nc = nc
        self.streams = {e: [] for e in self.ENGS}
        self.last_w = {}
        self.readers = {}
        self.ndma = {"sp": 0, "pool": 0}
        self.dma_ops = {"sp": [], "pool": []}
        self.seen = {e: {p: -1 for p in self.ENGS} for e in self.ENGS}
        self.seen_dma = {e: {} for e in self.ENGS}
        self.stack = ExitStack()
        self.n_ops = 0
        self.arena = self.stack.enter_context(nc.sbuf_tensor("arena", [128, ARENA_WORDS], F32))
        self.top = 0
        self.marks = []
        self.peak = 0
        self.phase = "p0"
        self.scopes = False

    def alloc(self, shape, dtype):
        esz = 2 if dtype == BF16 else 4
        n = 1
        for s in shape[1:]:
            n *= s
        words = (n * esz + 3) // 4
        off = self.top
        self.top += (words + 7) // 8 * 8
        self.peak = max(self.peak, self.top)
        assert self.top <= ARENA_WORDS, f"SBUF arena overflow {self.top}"
        ap = self.arena[0:shape[0], off:off + words]
        if dtype != F32:
            ap = ap.bitcast(dtype)
        if ap.shape[1] != n:
            ap = ap[:, 0:n]
        if len(shape) == 3:
            ap = ap.rearrange("p (a b) -> p a b", a=shape[1])
        elif len(shape) == 4:
            ap = ap.rearrange("p (a b c) -> p a b c", a=shape[1], b=shape[2])
        return ap

    def mark(self):
        self.marks.append(self.top)

    def release(self):
        self.barrier()
        self.top = self.marks.pop()

    def psum(self, name, shape, dtype=F32):
        return self.stack.enter_context(self.nc.psum_tensor(name, list(shape), dtype))

    def op(self, eng, fn, reads=(), writes=(), dma=False, extra=()):
        xr = [k for k in reads if isinstance(k, tuple) and k[0] == "B"]
        if xr:
            writes = list(writes) + [k for k in xr if k not in writes]
        o = Op()
        o.eng, o.fn, o.dma, o.signal, o.semval = eng, fn, dma, False, 0
        o.dsem = o.dval = None
        o.phase = self.phase
        stream = self.streams[eng]
        o.idx = len(stream)
        deps = {}
        cand = list(extra)
        for k in reads:
            w = self.last_w.get(k)
            if w is not None:
                cand.append(w)
        for k in writes:
            w = self.last_w.get(k)
            if w is not None:
                cand.append(w)
            cand.extend(self.readers.get(k, ()))
        if dma:
            q = eng
            n = self.ndma[q]
            self.ndma[q] = n + 1
            o.dsem = n % NDMA_SEM
            o.dval = 16 * (n // NDMA_SEM + 1)
            if n >= NDMA_SEM:
                cand.append(self.dma_ops[q][n - NDMA_SEM])
            self.dma_ops[q].append(o)
        for d in cand:
            if d is o or d.fn is None:
                continue
            if d.dma:
                key = (d.eng, d.dsem)
                if self.seen_dma[eng].get(key, 0) >= d.dval:
                    continue
                self.seen_dma[eng][key] = d.dval
                deps[("dma",) + key] = d
            else:
                if d.eng == "pe" and eng == "pe" and not dma:
                    continue
                if self.seen[eng][d.eng] >= d.idx:
                    continue
                cur = deps.get(("c", d.eng))
                if cur is None or cur.idx < d.idx:
                    deps[("c", d.eng)] = d
        for k, d in deps.items():
            if k[0] == "c":
                self.seen[eng][d.eng] = d.idx
                d.signal = True
        o.deps = list(deps.values())
        stream.append(o)
        self.n_ops += 1
        for k in reads:
            lst = self.readers.setdefault(k, [])
            if not dma:
                lst[:] = [r for r in lst if r.dma or r.eng != eng]
            lst.append(o)
        for k in writes:
            self.last_w[k] = o
            self.readers[k] = []
        return o

    def pe(self, fn, reads=(), writes=()):
        return self.op("pe", fn, reads, writes)

    def act(self, fn, reads=(), writes=()):
        return self.op("act", fn, reads, writes)

    def dve(self, fn, reads=(), writes=()):
        return self.op("dve", fn, reads, writes)

    def pool(self, fn, reads=(), writes=()):
        return self.op("pool", fn, reads, writes)

    def dma(self, out, in_, reads=(), writes=(), q="sp", **kw):
        return self.op(q, lambda e: e.dma_start(out=out, in_=in_, **kw), reads, writes, dma=True)

    def barrier(self):
        allops = set()
        for w in self.last_w.values():
            allops.add(w)
        for lst in self.readers.values():
            allops.update(lst)
        for q in self.dma_ops:
            allops.update(self.dma_ops[q][-NDMA_SEM:])
        allops = [o for o in allops if o.fn is not None]
        for e in self.ENGS:
            self.op(e, None, extra=allops)
        self.last_w.clear()
        self.readers.clear()

    def emit(self):
        nc = self.nc
        st = self.stack
        csem = {}
        for e in ("pe", "act", "dve", "pool"):
            cnt = 0
            for o in self.streams[e]:
                if o.signal and not o.dma:
                    cnt += 1
                    o.semval = cnt
            nsem = max(1, (cnt + SEM_CH - 1) // SEM_CH)
            csem[e] = [st.enter_context(nc.semaphore(f"s_{e}{i}")) for i in range(nsem)]
        dsem = {}
        for q in ("sp", "pool"):
            dsem[q] = [st.enter_context(nc.semaphore(f"d_{q}{i}")) for i in range(NDMA_SEM)]

        def emit_stream(ename, eng):
            if self.scopes:
                cur = None
                cm = None
                for o in self.streams[ename]:
                    if o.phase != cur:
                        if cm is not None:
                            cm.__exit__(None, None, None)
                        cur = o.phase
                        cm = nc.named_scope(cur)
                        cm.__enter__()
                    emit_one(ename, eng, o)
                if cm is not None:
                    cm.__exit__(None, None, None)
                return
            for o in self.streams[ename]:
                emit_one(ename, eng, o)

        def emit_one(ename, eng, o):
            if True:
                for d in o.deps:
                    if d.dma:
                        eng.wait_ge(dsem[d.eng][d.dsem], d.dval)
                    else:
                        v = d.semval - 1
                        eng.wait_ge(csem[d.eng][v // SEM_CH], v % SEM_CH + 1)
                if o.fn is None:
                    return
                ins = o.fn(eng)
                if o.dma:
                    ins.then_inc(dsem[ename][o.dsem], 16)
                elif o.signal:
                    v = o.semval - 1
                    ins.then_inc(csem[ename][v // SEM_CH], 1)

        with nc.Block() as block:
            @block.tensor
            def _(eng):
                emit_stream("pe", eng)

            @block.scalar
            def _(eng):
                emit_stream("act", eng)

            @block.vector
            def _(eng):
                emit_stream("dve", eng)

            @block.gpsimd
            def _(eng):
                emit_stream("pool", eng)

            @block.sync
            def _(eng):
                emit_stream("sp", eng)


LANE_PART = [0, 1, 2, 3, 32, 33, 34, 35]
NHEADS_MLA = 8
PV_NOACC = False
OB_BASE = 6
SCOPES = False


def build(stop=99, taps=False):
    nc = bass.Bass("TRN2", target_bir_lowering=False)
    P = Prog(nc)
    P.scopes = SCOPES

    def din(n, shp, dt=F32):
        return nc.dram_tensor(n, list(shp), dt, kind="ExternalInput")

    def dscr(n, shp, dt):
        return nc.dram_tensor(n, list(shp), dt, kind="Internal")

    x = din("x", [4096, 1024]); mem = din("mem", [256, 1024]); pos = din("pos", [32, 4096], I32)
    c_identb = din("c_identb", [128, 128], BF16); c_identf = din("c_identf", [128, 128])
    c_onesb = din("c_onesb", [128, 128], BF16)
    c_bmask = din("c_bmask", [128, 2, 128]); c_sel36 = din("c_sel36", [36, 8, 128])
    c_nsel = din("c_nsel", [128, 64]); c_invf = din("c_invf", [128, 1])
    c_reset = din("c_reset", [36, 4096])
    c_iota = din("c_iota", [128, 512]); c_tokid = din("c_tokid", [128, 32]); c_U = din("c_U", [128, 128])
    c_onesf = din("c_onesf", [128, 128])
    gmix_d = din("gmix", [128, 8])
    wq_d = din("w_q", [1024, 512]); wk_d = din("w_k", [1024, 512]); wv_d = din("w_v", [1024, 512]); wo_d = din("w_o", [1024, 512])
    wgI_d = din("w_gI", [1024, 36]); wgF_d = din("w_gF", [1024, 36]); bI_d = din("b_I", [36, 1]); bF_d = din("b_F", [36, 1])
    wcq_d = din("w_cq", [1024, 256]); wckv_d = din("w_ckv", [1024, 128])
    wkr_d = din("w_kr", [1024, 96]); wkrr_d = din("w_krr", [1024, 96])
    convq_d = din("convq", [128, 4, 3]); convk_d = din("convk", [128, 4, 3])
    gqa_d = din("g_qa", [128, 2]); gkva_d = din("g_kva", [128, 1])
    wqb_d = din("w_qb", [256, 768]); wqbr_d = din("w_qbr", [256, 768])
    wkn_d = din("w_kn", [128, 512]); wv2_d = din("w_v2", [128, 512])
    ghm_d = din("g_hm", [128, 4]); gha_d = din("g_ha", [64, 8])
    woutm_d = din("w_outm", [512, 1024]); wouta_d = din("w_outa", [8, 64, 1024])
    gmx_d = din("g_mx", [128, 8]); gmkv_d = din("g_mkv", [128, 8])
    wmq_d = din("w_mq", [1024, 1024]); wmk_d = din("w_mk", [1024, 1024]); wmv_d = din("w_mv", [1024, 1024]); wmo_d = din("w_mo", [1024, 1024])
    gffn_d = din("g_ffn", [1, 1024]); gfin_d = din("g_fin", [1, 1024]); gffnp_d = din("g_ffnp", [128, 8]); c_jp = din("c_jp", [128, 32, 2])
    wr_d = din("w_r", [1024, 16])
    if stop > 4:
        weg_d = din("w_eg", [16, 1024, 2048]); weu_d = din("w_eu", [16, 1024, 2048]); wed_d = din("w_ed", [16, 2048, 1024])
    else:
        weg_d = din("w_eg", [1, 1, 1]); weu_d = din("w_eu", [1, 1, 1]); wed_d = din("w_ed", [1, 1, 1])
    out = nc.dram_tensor("out", [4096, 1024], F32, kind="ExternalOutput")
    ymT_d = dscr("ymT_d", [512, 4096], BF16)
    yaT_d = dscr("yaT_d", [8, 64, 4096], BF16)
    acc_d = dscr("acc_d", [4096, 1024], F32)
    h_d = dscr("h_d", [4096, 1024], BF16)
    tapd = {}

    def tap(name, ap_sb, shape, dt, reads):
        if not taps:
            return
        t = nc.dram_tensor("tap_" + name, list(shape), dt, kind="ExternalOutput")
        tapd[name] = t
        P.dma(t.ap(), ap_sb, reads=reads, writes=[("tap", name)])

    def mm(o, lhsT, rhs, st, sp, R, W):
        P.pe(lambda e: e.matmul(o, lhsT=lhsT, rhs=rhs, start=st, stop=sp), R, W)

    def tr(o, i, idn, R, W):
        P.pe(lambda e: e.transpose(out=o, in_=i, identity=idn), R, W)

    def actf(o, i, func, R, W, bias=None, scale=None, accum=None):
        kw = {}
        if bias is not None:
            kw["bias"] = bias
        if scale is not None:
            kw["scale"] = scale
        if accum is not None:
            kw["accum_out"] = accum
        P.act(lambda e: e.activation(out=o, in_=i, func=func, **kw), R, W)

    def ts(eng, o, i0, s1, s2, op0, op1, R, W):
        if op1 is None:
            P.op(eng, lambda e: e.tensor_scalar(out=o, in0=i0, scalar1=s1, scalar2=None, op0=op0), R, W)
        else:
            P.op(eng, lambda e: e.tensor_scalar(out=o, in0=i0, scalar1=s1, scalar2=s2, op0=op0, op1=op1), R, W)

    def tt(eng, o, i0, i1, op, R, W):
        P.op(eng, lambda e: e.tensor_tensor(out=o, in0=i0, in1=i1, op=op), R, W)

    def stt(eng, o, i0, sc, i1, op0, op1, R, W):
        P.op(eng, lambda e: e.scalar_tensor_tensor(out=o, in0=i0, scalar=sc, in1=i1, op0=op0, op1=op1), R, W)

    def cp(eng, o, i, R, W):
        P.op(eng, lambda e: e.tensor_copy(out=o, in_=i), R, W)

    def recip(o, i, R, W):
        P.dve(lambda e: e.reciprocal(out=o, in_=i), R, W)

    def memset(eng, o, v, W):
        P.op(eng, lambda e: e.memset(o, v), (), W)

    def load(dst, src, key, q="sp"):
        P.dma(dst, src, writes=[key], q=q)

    TP = [P.psum(f"pp{i}", [128, 1024], F32) for i in range(4)]
    B = []
    for t_ in TP:
        B += [t_[:, 0:512], t_[:, 512:1024]]
    Bb = [b.bitcast(BF16) for b in B]
    BK = [("B", i) for i in range(8)]

    identb = P.alloc([128, 128], BF16); load(identb, c_identb.ap(), "identb")
    identf = P.alloc([128, 128], F32); load(identf, c_identf.ap(), "identf")
    onesb = P.alloc([128, 128], BF16); load(onesb, c_onesb.ap(), "onesb")

    def rms_rstd(ssum, rs, n, key):
        actf(rs, ssum, AF.Sqrt, [key + "ss"], [key + "rs"], bias=EPS, scale=1.0 / n)
        recip(rs, rs, [key + "rs"], [key + "rs"])

    xnT = P.alloc([128, 8, 4096], BF16)
    P.mark()
    gmix = P.alloc([128, 8], F32); load(gmix, gmix_d.ap(), "gmix")
    xs = [P.alloc([128, 4, 1024], F32) for _ in range(2)]
    xnb = [P.alloc([128, 4, 1024], BF16) for _ in range(2)]
    junk = P.alloc([128, 1024], BF16)
    ss = P.alloc([128, 32], F32); rs = P.alloc([128, 32], F32)
    for s in range(8):
        b = s % 2
        P.dma(xs[b], x[s * 512:(s + 1) * 512, :].rearrange("(j p) d -> p j d", p=128), writes=[("xs", b)])
        for j in range(4):
            t = s * 4 + j
            actf(junk, xs[b][:, j, :], AF.Square, [("xs", b)], ["junk", ("ss", t)], accum=ss[:, t:t + 1])
            actf(rs[:, t:t + 1], ss[:, t:t + 1], AF.Sqrt, [("ss", t)], [("rs", t)], bias=EPS, scale=1.0 / 1024)
            recip(rs[:, t:t + 1], rs[:, t:t + 1], [("rs", t)], [("rs", t)])
            ts("dve", xnb[b][:, j, :], xs[b][:, j, :], rs[:, t:t + 1], None, ALU.mult, None, [("xs", b), ("rs", t)], [("xnb", b, j)])
        for kc in range(8):
            bk = 6 + kc % 2
            for j in range(4):
                tr(Bb[bk][:, j * 128:(j + 1) * 128], xnb[b][:, j, kc * 128:(kc + 1) * 128], identb, [("xnb", b, j), "identb"], [BK[bk]])
            if kc % 2 == 0:
                ts("dve", xnT[:, kc, s * 512:(s + 1) * 512], Bb[bk][:, 0:512], gmix[:, kc:kc + 1], None, ALU.mult, None, [BK[bk], "gmix"], [("xnT", s)])
            else:
                actf(xnT[:, kc, s * 512:(s + 1) * 512], Bb[bk][:, 0:512], AF.Copy, [BK[bk], "gmix"], [("xnT", s)], scale=gmix[:, kc:kc + 1])
    XNT = [("xnT", s) for s in range(8)]
    tap("xnT", xnT, [128, 8, 4096], BF16, XNT)
    P.release()
    if stop <= 0:
        return finish(nc, P, out, tapd)

    P.phase = "p1a"
    P.mark()
    WS128 = P.alloc([128, 32, 36], F32)
    LB64 = P.alloc([64, 64, 36], F32)
    DEC = P.alloc([128, 8, 64], F32)
    P.mark()
    wgI = P.alloc([128, 8, 36], BF16); load(wgI, wgI_d.ap().rearrange("(k p) n -> p k n", p=128), "wgI", q="pool")
    wgF = P.alloc([128, 8, 36], BF16); load(wgF, wgF_d.ap().rearrange("(k p) n -> p k n", p=128), "wgF", q="pool")
    bI = P.alloc([36, 1], F32); load(bI, bI_d.ap(), "bI")
    nbF = P.alloc([36, 1], F32); load(nbF, bF_d.ap(), "nbF")
    ts("dve", nbF, nbF, -1.0, None, ALU.mult, None, ["nbF"], ["nbF"])
    reset = P.alloc([36, 4096], F32); load(reset, c_reset.ap(), "reset")
    sel36 = P.alloc([36, 8, 128], F32); load(sel36, c_sel36.ap(), "sel36")
    IT = P.alloc([36, 4096], F32); SP = P.alloc([36, 4096], F32); NB = P.alloc([36, 4096], F32)
    Gt = P.alloc([36, 64], F32); Gn = P.alloc([36, 64], F32); amax = P.alloc([36, 64], F32)
    Mm = P.alloc([36, 64], F32); Mend = P.alloc([36, 64], F32); MP = P.alloc([36, 64], F32); DECs = P.alloc([36, 64], F32)
    for s in range(8):
        sl = slice(s * 512, (s + 1) * 512)
        for kc in range(8):
            mm(B[0][0:36, :], wgI[:, kc, :], xnT[:, kc, sl], kc == 0, kc == 7, ["wgI"] + XNT, [BK[0]])
        actf(IT[:, sl], B[0][0:36, :], AF.Identity, [BK[0], "bI"], ["IT"], bias=bI[:, 0:1])
        for kc in range(8):
            mm(B[1][0:36, :], wgF[:, kc, :], xnT[:, kc, sl], kc == 0, kc == 7, ["wgF"] + XNT, [BK[1]])
        actf(SP[:, sl], B[1][0:36, :], AF.Exp, [BK[1], "nbF"], ["SP"], bias=nbF[:, 0:1], scale=-1.0)
    actf(SP, SP, AF.Ln, ["SP"], ["SP"], bias=1.0)
    P.dve(lambda e: e.tensor_tensor_scan(out=NB, data0=reset, data1=SP, initial=0.0, op0=ALU.mult, op1=ALU.add), ["reset", "SP"], ["NB"])
    NB3 = NB.rearrange("p (c t) -> p c t", t=64); IT3 = IT.rearrange("p (c t) -> p c t", t=64); SP3 = SP.rearrange("p (c t) -> p c t", t=64)
    cp("dve", Gt, NB3[:, :, 63], ["NB"], ["Gt"])
    Gt3 = Gt.rearrange("p (c o) -> p c o", o=1)
    tt("dve", NB3[32:36], Gt3[32:36].to_broadcast([4, 64, 64]), NB3[32:36], ALU.subtract, ["Gt", "NB"], ["NB"])
    tt("dve", NB3[32:36], NB3[32:36], SP3[32:36], ALU.add, ["NB", "SP"], ["NB"])
    tt("dve", IT, IT, NB, ALU.add, ["IT", "NB"], ["IT"])
    P.dve(lambda e: e.tensor_reduce(out=amax, in_=IT3, axis=AX.X, op=ALU.max), ["IT"], ["amax"])
    ts("dve", Gn, Gt, -1.0, None, ALU.mult, None, ["Gt"], ["Gn"])
    memset("dve", Mm, 0.0, ["Mm"])
    P.dve(lambda e: e.tensor_tensor_scan(out=Mm[0:4, :], data0=amax[0:4, :], data1=Gn[0:4, :], initial=0.0, op0=ALU.max, op1=ALU.add), ["amax", "Gn"], ["Mm"])
    P.dve(lambda e: e.tensor_tensor_scan(out=Mm[32:36, ::-1], data0=amax[32:36, ::-1], data1=Gn[32:36, ::-1], initial=0.0, op0=ALU.max, op1=ALU.add), ["amax", "Gn"], ["Mm"])
    tt("dve", Mend, Mm, Gt, ALU.add, ["Mm", "Gt"], ["Mend"])
    memset("dve", MP, 0.0, ["MP"])
    cp("dve", MP[0:4, 1:64], Mm[0:4, 0:63], ["Mm", "MP"], ["MP"])
    cp("dve", MP[32:36, 0:63], Mm[32:36, 1:64], ["Mm", "MP"], ["MP"])
    tt("dve", DECs, MP, Mend, ALU.subtract, ["MP", "Mend"], ["DECs"])
    actf(DECs, DECs, AF.Exp, ["DECs"], ["DECs"])
    Mend3 = Mend.rearrange("p (c o) -> p c o", o=1).to_broadcast([36, 64, 64])
    tt("dve", IT3, IT3, Mend3, ALU.subtract, ["IT", "Mend"], ["IT"])
    actf(IT, IT, AF.Exp, ["IT"], ["IT"])
    tt("dve", NB3, NB3, Mend3, ALU.subtract, ["NB", "Mend"], ["NB"])
    actf(NB, NB, AF.Exp, ["NB"], ["NB"])
    for g8 in range(4):
        bk = g8 % 2
        for jj in range(8):
            j = g8 * 8 + jj
            tr(B[bk][:, jj * 36:(jj + 1) * 36], IT[0:36, j * 128:(j + 1) * 128], identf[0:36, 0:36], ["IT", "identf"], [BK[bk]])
        cp("dve", WS128[:, g8 * 8:(g8 + 1) * 8, :], B[bk][:, 0:288].rearrange("p (a b) -> p a b", a=8), [BK[bk]], ["WS128"])
    for g8 in range(8):
        bk = 2 + g8 % 2
        for cc in range(8):
            c = g8 * 8 + cc
            tr(B[bk][0:64, cc * 36:(cc + 1) * 36], NB[0:36, c * 64:(c + 1) * 64], identf[0:36, 0:36], ["NB", "identf"], [BK[bk]])
        cp("dve", LB64[:, g8 * 8:(g8 + 1) * 8, :], B[bk][0:64, 0:288].rearrange("p (a b) -> p a b", a=8), [BK[bk]], ["LB64"])
    for l in range(8):
        mm(B[4][:, l * 64:(l + 1) * 64], sel36[:, l, :], DECs, True, True, ["sel36", "DECs"], [BK[4]])
    cp("dve", DEC, B[4][:, 0:512].rearrange("p (a b) -> p a b", a=8), [BK[4]], ["DEC"])
    tap("WS128", WS128, [128, 32, 36], F32, ["WS128"])
    tap("LB64", LB64, [64, 64, 36], F32, ["LB64"])
    tap("DEC", DEC, [128, 8, 64], F32, ["DEC"])
    P.release()
    if stop <= 1:
        return finish(nc, P, out, tapd)

    P.phase = "p1b"
    P.mark()
    wq = P.alloc([128, 8, 128], BF16); wk = P.alloc([128, 8, 128], BF16)
    wv = P.alloc([128, 8, 128], BF16); wo = P.alloc([128, 8, 128], BF16)
    convq = P.alloc([128, 4, 3], F32); load(convq, convq_d.ap(), "convq")
    convk = P.alloc([128, 4, 3], F32); load(convk, convk_d.ap(), "convk")
    ghm = P.alloc([128, 4], F32); load(ghm, ghm_d.ap(), "ghm")
    bmask = P.alloc([128, 2, 128], F32); load(bmask, c_bmask.ap(), "bmask")
    preq = P.alloc([128, 4098], BF16); prek = P.alloc([128, 4098], BF16)
    ctmp = [P.alloc([128, 512], F32) for _ in range(2)]
    qT = P.alloc([128, 4096], BF16); kT = P.alloc([128, 4096], BF16)
    k_tm = P.alloc([128, 32, 128], BF16); v_tm = P.alloc([128, 32, 129], BF16)
    sigT = P.alloc([128, 4096], BF16)
    PT = P.alloc([128, 2, 32, 128], BF16)
    hacc = P.alloc([64, 64, 128], F32)
    hn = [P.alloc([64, 8, 128], BF16) for _ in range(2)]
    ymT = qT
    C32 = [[P.alloc([128, 129], F32) for _ in range(2)] for _ in range(2)]
    Cbf = [[P.alloc([128, 129], BF16) for _ in range(2)] for _ in range(2)]
    vp = [P.alloc([128, 129], BF16) for _ in range(4)]
    dtmp = [P.alloc([64, 2], F32) for _ in range(4)]
    hsq = P.alloc([64, 128], BF16)
    hss = P.alloc([64, 64], F32)
    memset("dve", v_tm[:, :, 128:129], 1.0, ["v_tm"])
    for h in range(4):
        hs = slice(0, 128)
        P.phase = "p1b_proj"
        for wt, wd, wkey_ in ((wq, wq_d, "wq"), (wk, wk_d, "wk"), (wv, wv_d, "wv"), (wo, wo_d, "wo")):
            P.dma(wt, wd[:, h * 128:(h + 1) * 128].rearrange("(k p) n -> p k n", p=128), writes=[wkey_], q="pool")
        qk = (("convq", wq, "wq", convq, qT, "qT", preq, "preq"), ("convk", wk, "wk", convk, kT, "kT", prek, "prek"))
        for cwk, wmat, wkey, cw, dst, dkey, pre, pk in qk:
            if h == 0:
                memset("dve", pre[:, 0:1], 0.0, [pk]); memset("dve", pre[:, 4097:4098], 0.0, [pk])
            for s in range(8):
                sl = slice(s * 512, (s + 1) * 512)
                bk = s % 2
                for kc in range(8):
                    mm(B[bk][:, :], wmat[:, kc, hs], xnT[:, kc, sl], kc == 0, kc == 7, [wkey] + XNT, [BK[bk]])
                actf(pre[:, 1 + s * 512:1 + (s + 1) * 512], B[bk][:, :], AF.Copy, [BK[bk]], [pk])
        for s in range(8):
            sl = slice(s * 512, (s + 1) * 512)
            bk = s % 2
            for kc in range(8):
                mm(B[bk][:, :], wo[:, kc, hs], xnT[:, kc, sl], kc == 0, kc == 7, ["wo"] + XNT, [BK[bk]])
            actf(sigT[:, sl], B[bk][:, :], AF.Sigmoid, [BK[bk]], ["sigT"])
        for cwk, wmat, wkey, cw, dst, dkey, pre, pk in qk:
            for s in range(8):
                c0 = s * 512
                tb = ctmp[s % 2]
                tk = ("ctmp", s % 2)
                ts("dve", tb, pre[:, c0:c0 + 512], cw[:, h, 0:1], None, ALU.mult, None, [pk, cwk], [tk])
                stt("dve", tb, pre[:, c0 + 1:c0 + 513], cw[:, h, 1:2], tb, ALU.mult, ALU.add, [pk, tk], [tk])
                stt("dve", tb, pre[:, c0 + 2:c0 + 514], cw[:, h, 2:3], tb, ALU.mult, ALU.add, [pk, tk], [tk])
                actf(dst[:, c0:c0 + 512], tb, AF.Silu, [tk], [dkey])
        for j in range(32):
            bk = 2 + j % 2
            for kc in range(8):
                mm(B[bk][:, 0:128], xnT[:, kc, j * 128:(j + 1) * 128], wv[:, kc, hs], kc == 0, kc == 7, ["wv"] + XNT, [BK[bk]])
            cp("dve", v_tm[:, j, 0:128], B[bk][:, 0:128], [BK[bk]], ["v_tm"])
        for j in range(32):
            bk = 6 + j % 2
            tr(Bb[bk][:, 0:128], kT[:, j * 128:(j + 1) * 128], identb, ["kT", "identb"], [BK[bk]])
            ts("dve", k_tm[:, j, :], Bb[bk][:, 0:128], 128 ** -0.5, None, ALU.mult, None, [BK[bk]], ["k_tm"])
        P.phase = "p1b_scan"
        for j in range(32):
            bk = j % 2
            mm(B[bk][:, 0:128], kT[:, j * 128:(j + 1) * 128], qT[:, j * 128:(j + 1) * 128], True, True, ["kT", "qT"], [BK[bk]])
            for d in range(2):
                lc = h + 32 * d
                stt("dve", PT[:, d, j, :], B[bk][:, 0:128], WS128[:, j, lc:lc + 1], bmask[:, d, :], ALU.mult, ALU.mult,
                    [BK[bk], "WS128", "bmask"], [("PT", d, j)])
        for d in range(2):
            memset("dve", C32[d][0], 0.0, [("C32", d, 0)])
        units = []
        for i in range(64):
            for d in range(2):
                units.append((i, d, i if d == 0 else 63 - i))

        def emit_dc(n):
            i, d, c = units[n]
            j, r = c // 2, c % 2
            rs_ = slice(r * 64, (r + 1) * 64)
            lc = h + 32 * d
            vpb = vp[n % 4]; vk = ("vp", n % 4)
            actf(vpb[rs_, :], v_tm[rs_, j, :], AF.Copy, ["v_tm", "WS128"], [vk], scale=WS128[rs_, j, lc:lc + 1])
            db = 4 + n % 2
            mm(B[db][:, 0:129], k_tm[rs_, j, :], vpb[rs_, :], True, True, ["k_tm", vk], [BK[db]])

        def emit_norm(n):
            i, d, c = units[n]
            lc = h + 32 * d
            nb_ = n % 4
            dt_ = dtmp[n % 4]; dk = ("dtmp", n % 4)
            actf(dt_[:, 0:1], B[nb_][0:64, 128:129], AF.Abs, [BK[nb_]], [dk])
            ts("dve", dt_[:, 0:1], dt_[:, 0:1], LB64[:, c, lc:lc + 1], None, ALU.max, None, [dk, "LB64"], [dk])
            recip(dt_[:, 1:2], dt_[:, 0:1], [dk], [dk])
            if i < 32:
                ts("dve", hacc[:, c, :], B[nb_][0:64, 0:128], dt_[:, 1:2], None, ALU.mult, None, [BK[nb_], dk], [("hacc", c)])
            else:
                stt("dve", hacc[:, c, :], B[nb_][0:64, 0:128], dt_[:, 1:2], hacc[:, c, :], ALU.mult, ALU.add, [BK[nb_], dk, ("hacc", c)], [("hacc", c)])

        emit_dc(0)
        for n in range(128):
            i, d, c = units[n]
            j, r = c // 2, c % 2
            rs_ = slice(r * 64, (r + 1) * 64)
            l = h + 4 * d
            if n >= 4:
                emit_norm(n - 4)
            if n + 1 < 128:
                emit_dc(n + 1)
            cur, nxt = C32[d][i % 2], C32[d][(i + 1) % 2]
            ck, nk = ("C32", d, i % 2), ("C32", d, (i + 1) % 2)
            cb = Cbf[d][i % 2]; cbk = ("Cbf", d, i % 2)
            actf(cb, cur, AF.Copy, [ck, "DEC"], [cbk], scale=DEC[:, l, c:c + 1])
            db = 4 + n % 2
            stt("dve", nxt, cur, DEC[:, l, c:c + 1], B[db][:, 0:129], ALU.mult, ALU.add, [ck, "DEC", BK[db]], [nk])
            nb_ = n % 4
            mm(B[nb_][0:64, 0:129], qT[:, c * 64:(c + 1) * 64], cb, True, False, ["qT", cbk], [BK[nb_]])
            mm(B[nb_][0:64, 0:129], PT[rs_, d, j, rs_], v_tm[rs_, j, :], False, True, [("PT", d, j), "v_tm"], [BK[nb_]])
        for n in range(124, 128):
            emit_norm(n)
        P.phase = "p1b_post"
        HA = [("hacc", c) for c in range(64)]
        for c in range(64):
            actf(hsq, hacc[:, c, :], AF.Square, [("hacc", c)], ["hsq", "hssss"], accum=hss[:, c:c + 1])
        rms_rstd(hss, hss, 128, "hss")
        for g8 in range(8):
            hb = hn[g8 % 2]; hk = ("hn", g8 % 2)
            for cc in range(8):
                c = g8 * 8 + cc
                ts("dve", hb[:, cc, :], hacc[:, c, :], hss[:, c:c + 1], None, ALU.mult, None, [("hacc", c), "hssrs"], [hk])
            bk = 6 + g8 % 2
            for cc in range(8):
                tr(Bb[bk][:, cc * 64:(cc + 1) * 64], hb[:, cc, :], identb[0:64, 0:64], [hk, "identb"], [BK[bk]])
            sl = slice(g8 * 512, (g8 + 1) * 512)
            stt("dve", ymT[:, sl], Bb[bk][:, 0:512], ghm[:, h:h + 1], sigT[:, sl], ALU.mult, ALU.mult, [BK[bk], "ghm", "sigT"], ["qT"])
        P.dma(ymT_d[h * 128:(h + 1) * 128, :], ymT, reads=["qT"], writes=[("ymT_d", h)])
    P.release()
    P.release()
    if stop <= 2:
        return finish(nc, P, out, tapd, extra_out=[("ymT_d", ymT_d, [512, 4096], BF16)])
    P.phase = "p1c"
    P.mark()
    sinT = P.alloc([96, 4096], BF16); cosT = P.alloc([96, 4096], BF16)
    cqnT = P.alloc([128, 2, 4096], BF16); ckvnT = P.alloc([128, 4096], BF16); krT = P.alloc([96, 4096], BF16)
    R_ = slice(64, 96)
    P.mark()
    posi = P.alloc([96, 4096], I32); load(posi[R_, :], pos.ap(), "posi")
    invf = P.alloc([128, 1], F32); load(invf, c_invf.ap(), "invf")
    ang = P.alloc([96, 4096], F32); kf = P.alloc([96, 4096], F32); fw = P.alloc([96, 4096], F32)
    cp("dve", ang[R_], posi[R_], ["posi"], ["ang"])
    ts("dve", ang[R_], ang[R_], invf[R_, 0:1], 1.0 / (2 * np.pi), ALU.mult, ALU.mult, ["ang", "invf"], ["ang"])
    cp("dve", posi[R_], ang[R_], ["ang"], ["posi"])
    cp("dve", kf[R_], posi[R_], ["posi"], ["kf"])
    tt("dve", ang[R_], ang[R_], kf[R_], ALU.subtract, ["ang", "kf"], ["ang"])
    for dstT, shift, dk_ in ((sinT, 0.0, "sinT"), (cosT, 0.25, "cosT")):
        ts("dve", fw[R_], ang[R_], shift, None, ALU.add, None, ["ang"], ["fw"])
        ts("dve", kf[R_], fw[R_], 0.5, None, ALU.is_gt, None, ["fw"], ["kf"])
        tt("dve", fw[R_], fw[R_], kf[R_], ALU.subtract, ["fw", "kf"], ["fw"])
        ts("dve", kf[R_], fw[R_], -0.5, None, ALU.is_lt, None, ["fw"], ["kf"])
        tt("dve", fw[R_], fw[R_], kf[R_], ALU.add, ["fw", "kf"], ["fw"])
        actf(dstT[R_], fw[R_], AF.Sin, ["fw"], [dk_], scale=6.283185)
    P.release()
    if stop <= 2.3:
        tap("sinT", sinT[64:96], [32, 4096], BF16, ["sinT"]); tap("cosT", cosT[64:96], [32, 4096], BF16, ["cosT"])
        return finish(nc, P, out, tapd)
    def wload(shape, src, key):
        t_ = P.alloc(shape, BF16); load(t_, src, key, q="pool"); return t_
    wcq = wload([128, 8, 256], wcq_d.ap().rearrange("(k p) n -> p k n", p=128), "wcq")
    wckv = wload([128, 8, 128], wckv_d.ap().rearrange("(k p) n -> p k n", p=128), "wckv")
    wkr = wload([128, 8, 96], wkr_d.ap().rearrange("(k p) n -> p k n", p=128), "wkr")
    wkrr = wload([128, 8, 96], wkrr_d.ap().rearrange("(k p) n -> p k n", p=128), "wkrr")
    wqb = wload([128, 2, 768], wqb_d.ap().rearrange("(k p) n -> p k n", p=128), "wqb")
    wqbr = wload([128, 2, 768], wqbr_d.ap().rearrange("(k p) n -> p k n", p=128), "wqbr")
    wkn = wload([128, 512], wkn_d.ap(), "wkn"); wv2 = wload([128, 512], wv2_d.ap(), "wv2")
    ts("dve", wkrr[:, :, 64:80], wkrr[:, :, 64:80], -1.0, None, ALU.mult, None, ["wkrr"], ["wkrr"])
    wqbr4 = wqbr.rearrange("p k (h c) -> p k h c", c=96)
    for k2 in range(2):
        ts("dve", wqbr4[:, k2, :, 64:80], wqbr4[:, k2, :, 64:80], -1.0, None, ALU.mult, None, ["wqbr"], ["wqbr"])
    gqa = P.alloc([128, 2], F32); load(gqa, gqa_d.ap(), "gqa")
    gkva = P.alloc([128, 1], F32); load(gkva, gkva_d.ap(), "gkva")
    gha = P.alloc([64, 8], F32); load(gha, gha_d.ap(), "gha")
    nsel = P.alloc([128, 64], BF16); load(nsel, c_nsel.ap(), "nsel", q="pool")
    v_allf = P.alloc([128, 32, 592], BF16)
    v_all = v_allf[:, :, 0:528].rearrange("p j (h c) -> p j h c", c=66)
    memset("pool", v_allf, 0.0, ["v_all"]); memset("dve", v_all[:, :, :, 64:65], 1.0, ["v_all"])
    t1 = P.alloc([96, 512], F32); t2 = P.alloc([96, 512], F32)
    P.mark()
    sqb = P.alloc([128, 2, 512], BF16); cqf = P.alloc([128, 2, 512], F32); rstd = P.alloc([128, 512], F32)
    sq2 = P.alloc([128, 512], BF16); ckf = P.alloc([128, 512], F32); rstd2 = P.alloc([128, 512], F32)
    for s in range(8):
        sl = slice(s * 512, (s + 1) * 512)
        for blk in range(2):
            for kc in range(8):
                mm(B[blk][:, :], wcq[:, kc, blk * 128:(blk + 1) * 128], xnT[:, kc, sl], kc == 0, kc == 7, ["wcq"] + XNT, [BK[blk]])
            actf(sqb[:, blk, :], B[blk][:, :], AF.Square, [BK[blk]], [("sqb", blk)])
            actf(cqf[:, blk, :], B[blk][:, :], AF.Copy, [BK[blk]], [("cqf", blk)])
        mm(B[2][:, :], onesb, sqb[:, 0, :], True, False, ["onesb", ("sqb", 0)], [BK[2]])
        mm(B[2][:, :], onesb, sqb[:, 1, :], False, True, ["onesb", ("sqb", 1)], [BK[2]])
        actf(rstd, B[2][:, :], AF.Sqrt, [BK[2]], ["rstd"], bias=EPS, scale=1.0 / 256)
        recip(rstd, rstd, ["rstd"], ["rstd"])
        for blk in range(2):
            stt("dve", cqnT[:, blk, sl], cqf[:, blk, :], gqa[:, blk:blk + 1], rstd, ALU.mult, ALU.mult, [("cqf", blk), "gqa", "rstd"], ["cqnT"])
        for kc in range(8):
            mm(B[3][:, :], wckv[:, kc, :], xnT[:, kc, sl], kc == 0, kc == 7, ["wckv"] + XNT, [BK[3]])
        actf(sq2, B[3][:, :], AF.Square, [BK[3]], ["sq2"])
        actf(ckf, B[3][:, :], AF.Copy, [BK[3]], ["ckf"])
        mm(B[4][:, :], onesb, sq2, True, True, ["onesb", "sq2"], [BK[4]])
        actf(rstd2, B[4][:, :], AF.Sqrt, [BK[4]], ["rstd2"], bias=EPS, scale=1.0 / 128)
        recip(rstd2, rstd2, ["rstd2"], ["rstd2"])
        stt("dve", ckvnT[:, sl], ckf, gkva[:, 0:1], rstd2, ALU.mult, ALU.mult, ["ckf", "gkva", "rstd2"], ["ckvnT"])
        for kc in range(8):
            mm(B[5][0:96, :], wkr[:, kc, :], xnT[:, kc, sl], kc == 0, kc == 7, ["wkr"] + XNT, [BK[5]])
        for kc in range(8):
            mm(B[6][0:96, :], wkrr[:, kc, :], xnT[:, kc, sl], kc == 0, kc == 7, ["wkrr"] + XNT, [BK[6]])
        tt("dve", t1[R_], B[5][R_, :], cosT[R_, sl], ALU.mult, [BK[5], "cosT"], ["t1"])
        tt("dve", t2[R_], B[6][R_, :], sinT[R_, sl], ALU.mult, [BK[6], "sinT"], ["t2"])
        tt("dve", krT[R_, sl], t1[R_], t2[R_], ALU.add, ["t1", "t2"], ["krT"])
    for j in range(32):
        bk = j % 2
        mm(B[bk][:, :], ckvnT[:, j * 128:(j + 1) * 128], wv2, True, True, ["ckvnT", "wv2"], [BK[bk]])
        cp("dve", v_all[:, j, :, 0:64], B[bk][:, :].rearrange("p (h d) -> p h d", h=8), [BK[bk]], ["v_all"])
    P.release()
    if stop <= 2.6:
        tap("cqnT", cqnT, [128, 2, 4096], BF16, ["cqnT"]); tap("ckvnT", ckvnT, [128, 4096], BF16, ["ckvnT"]); tap("krT", krT[64:96], [32, 4096], BF16, ["krT"])
        return finish(nc, P, out, tapd, extra_out=[("ymT_d", ymT_d, [512, 4096], BF16)])
    P.barrier()
    qTh_ = [P.alloc([128, 4096], BF16), xnT[:, 0, :]]
    kTh_ = [P.alloc([128, 4096], BF16), xnT[:, 1, :]]
    vh_ = [P.alloc([128, 32, 128], BF16), xnT[:, 2, :].rearrange("p (j c) -> p j c", c=128)]
    for pb in range(2):
        memset("dve", qTh_[pb], 0.0, [("qTh", pb)]); memset("pool", kTh_[pb], 0.0, [("kTh", pb)])
        memset("pool", vh_[pb], 0.0, [("vh", pb)]); memset("dve", vh_[pb][:, :, 64:65], 1.0, [("vh", pb)])
    PT2 = [P.alloc([128, 1024], BF16) for _ in range(2)]
    osq = P.alloc([128, 512], BF16); memset("dve", osq, 0.0, ["osq"]); ocp = P.alloc([64, 512], F32); rden = P.alloc([64, 512], F32)
    yab = [P.alloc([64, 512], BF16) for _ in range(2)]

    def pro_groups(h, s):
        pb = h % 2
        qTh, kTh = qTh_[pb], kTh_[pb]
        hc = slice(h * 96, (h + 1) * 96)
        sl = slice(s * 512, (s + 1) * 512)

        def g0():
            ph = P.phase; P.phase = "p1c_pro"
            for k2 in range(2):
                mm(B[5][0:96, :], wqb[:, k2, hc], cqnT[:, k2, sl], k2 == 0, k2 == 1, ["wqb", "cqnT"], [BK[5]])
            cp("dve", qTh[0:64, sl], B[5][0:64, :], [BK[5]], [("qTh", pb)])
            tt("dve", t1[R_], B[5][R_, :], cosT[R_, sl], ALU.mult, [BK[5], "cosT"], ["t1"])
            P.phase = ph

        def g1():
            ph = P.phase; P.phase = "p1c_pro"
            for k2 in range(2):
                mm(B[5][0:96, :], wqbr[:, k2, hc], cqnT[:, k2, sl], k2 == 0, k2 == 1, ["wqbr", "cqnT"], [BK[5]])
            tt("dve", t2[R_], B[5][R_, :], sinT[R_, sl], ALU.mult, [BK[5], "sinT"], ["t2"])
            tt("dve", qTh[R_, sl], t1[R_], t2[R_], ALU.add, ["t1", "t2"], [("qTh", pb)])
            P.phase = ph

        def g2():
            ph = P.phase; P.phase = "p1c_pro"
            mm(B[5][0:64, :], wkn[:, h * 64:(h + 1) * 64], ckvnT[:, sl], True, True, ["wkn", "ckvnT"], [BK[5]])
            cp("dve", kTh[0:64, sl], B[5][0:64, :], [BK[5]], [("kTh", pb)])
            P.phase = ph
        return [g0, g1, g2]

    def pro_step(h, s):
        for g in pro_groups(h, s):
            g()

    def pro_fin(h):
        pb = h % 2
        cp("dve", kTh_[pb][R_, :], krT[R_, :], ["krT"], [("kTh", pb)])
        cp("dve", vh_[pb][:, :, 0:64], v_all[:, :, h, 0:64], ["v_all"], [("vh", pb)])

    def attn(h, Q, inserts=()):
        inserts = list(inserts)
        pb_ = h % 2
        qTh, kTh, vh = qTh_[pb_], kTh_[pb_], vh_[pb_]
        ql = slice(Q * 512, (Q + 1) * 512)
        ob = OB_BASE + Q % 2

        def st_(kp):
            pb = kp % 2
            for u in range(2):
                kt = 2 * kp + u
                mm(B[2 * pb + u][:, :], kTh[:, kt * 128:(kt + 1) * 128], qTh[:, ql], True, True, [("kTh", pb_), ("qTh", pb_)], [BK[2 * pb + u]])
            actf(PT2[pb], TP[pb][:, :], AF.Exp, [BK[2 * pb], BK[2 * pb + 1]], [("PT2", pb)], scale=96 ** -0.5)
        st_(0)
        for kp in range(16):
            if kp + 1 < 16:
                st_(kp + 1)
            for u in range(2):
                kt = 2 * kp + u
                mm(B[ob][:, :], vh[:, kt, :], PT2[kp % 2][:, u * 512:(u + 1) * 512], kt == 0, kt == 31, [("vh", pb_), ("PT2", kp % 2)], [BK[ob]])
            if kp in (3, 8, 13) and inserts:
                inserts.pop(0)()
        actf(osq, B[ob][:, :], AF.Square, [BK[ob]], ["osq"])
        cp("dve", ocp, B[ob][0:64, :], [BK[ob]], ["ocp"])
        mm(B[4][0:64, :], nsel[:, :], osq[:, :], True, True, ["nsel", "osq"], [BK[4]])
        actf(rden, B[4][0:64, :], AF.Sqrt, [BK[4]], ["rden"])
        recip(rden, rden, ["rden"], ["rden"])
        yb = yab[Q % 2]; yk = ("yab", Q % 2)
        stt("dve", yb, ocp, gha[:, h:h + 1], rden, ALU.mult, ALU.mult, ["ocp", "gha", "rden"], [yk])
        P.dma(yaT_d[h, :, ql], yb, reads=[yk], writes=[("yaT_d", h, Q)])

    for s_ in range(8):
        pro_step(0, s_)
    pro_fin(0)
    P.phase = "p1c_attn"
    for h in range(NHEADS_MLA):
        for Q in range(8):
            attn(h, Q, pro_groups(h + 1, Q) if h + 1 < NHEADS_MLA else ())
        if h + 1 < NHEADS_MLA:
            pro_fin(h + 1)
    P.release()
    P.barrier()
    P.top = 0
    identb = P.alloc([128, 128], BF16); load(identb, c_identb.ap(), "identb")
    identf = P.alloc([128, 128], F32); load(identf, c_identf.ap(), "identf")
    if stop <= 3:
        return finish(nc, P, out, tapd, extra_out=[("ymT_d", ymT_d, [512, 4096], BF16), ("yaT_d", yaT_d, [8, 64, 4096], BF16)])

    P.phase = "p2"
    AFF = P.alloc([128, 32, 16], F32)
    P.mark()
    woutm = wload([128, 4, 1024], woutm_d.ap().rearrange("(k p) n -> p k n", p=128), "woutm")
    wouta = wload([128, 4, 1024], wouta_d.ap().rearrange("h p n -> (h p) n").rearrange("(k p) n -> p k n", p=128), "wouta")
    wmq = wload([128, 8, 1024], wmq_d.ap().rearrange("(k p) n -> p k n", p=128), "wmq")
    wmo = wload([128, 8, 1024], wmo_d.ap().rearrange("(k p) n -> p k n", p=128), "wmo")
    gmx = P.alloc([128, 8], F32); load(gmx, gmx_d.ap(), "gmx")
    gffn_b = P.alloc([128, 1024], F32); load(gffn_b, gffn_d.ap().to_broadcast([128, 1024]), "gffn_b")
    gffn_p = P.alloc([128, 8], F32); load(gffn_p, gffnp_d.ap(), "gffn_p")
    wr = P.alloc([128, 8, 16], F32); load(wr, wr_d.ap().rearrange("(k p) n -> p k n", p=128), "wr")
    memKT = P.alloc([128, 8, 256], BF16); memV = P.alloc([128, 2, 4, 257], BF16)
    memset("dve", memV[:, :, :, 256:257], 1.0, ["memV"])
    P.mark()
    wmk = wload([128, 8, 1024], wmk_d.ap().rearrange("(k p) n -> p k n", p=128), "wmk")
    wmv = wload([128, 8, 1024], wmv_d.ap().rearrange("(k p) n -> p k n", p=128), "wmv")
    gmkv = P.alloc([128, 8], F32); load(gmkv, gmkv_d.ap(), "gmkv")
    mx_ = P.alloc([128, 2, 1024], F32); load(mx_, mem.ap().rearrange("(j p) d -> p j d", p=128), "mx")
    mnb = P.alloc([128, 2, 1024], BF16); memnT = P.alloc([128, 8, 256], BF16)
    mss = P.alloc([128, 2], F32); mjunk = P.alloc([128, 1024], BF16)
    for j in range(2):
        actf(mjunk, mx_[:, j, :], AF.Square, ["mx"], ["mjunk", ("mss", j)], accum=mss[:, j:j + 1])
        actf(mss[:, j:j + 1], mss[:, j:j + 1], AF.Sqrt, [("mss", j)], [("mss", j)], bias=EPS, scale=1.0 / 1024)
        recip(mss[:, j:j + 1], mss[:, j:j + 1], [("mss", j)], [("mss", j)])
        ts("dve", mnb[:, j, :], mx_[:, j, :], mss[:, j:j + 1], None, ALU.mult, None, ["mx", ("mss", j)], [("mnb", j)])
    for kc in range(8):
        bk = 6 + kc % 2
        for j in range(2):
            tr(Bb[bk][:, j * 128:(j + 1) * 128], mnb[:, j, kc * 128:(kc + 1) * 128], identb, [("mnb", j), "identb"], [BK[bk]])
        ts("dve", memnT[:, kc, :], Bb[bk][:, 0:256], gmkv[:, kc:kc + 1], None, ALU.mult, None, [BK[bk], "gmkv"], ["memnT"])
    for blk in range(8):
        bk = blk % 2
        for kc in range(8):
            mm(B[bk][:, 0:256], wmk[:, kc, blk * 128:(blk + 1) * 128], memnT[:, kc, :], kc == 0, kc == 7, ["wmk", "memnT"], [BK[bk]])
        cp("dve", memKT[:, blk, :], B[bk][:, 0:256], [BK[bk]], ["memKT"])
    for mt in range(2):
        for hf in range(2):
            bk = 2 + hf
            for kc in range(8):
                mm(B[bk][:, :], memnT[:, kc, mt * 128:(mt + 1) * 128], wmv[:, kc, hf * 512:(hf + 1) * 512], kc == 0, kc == 7, ["wmv", "memnT"], [BK[bk]])
            cp("dve", memV[:, mt, 2 * hf:2 * hf + 2, 0:256], B[bk][:, :].rearrange("p (h d) -> p h d", h=2), [BK[bk]], ["memV"])
    P.release()
    xs2_ = [P.alloc([128, 4, 1024], F32) for _ in range(2)]; ymTs_ = [P.alloc([128, 4, 512], BF16) for _ in range(2)]; yaTs_ = [P.alloc([128, 4, 512], BF16) for _ in range(2)]
    xn2 = P.alloc([128, 4, 1024], BF16); xn2T = P.alloc([128, 8, 512], BF16); qmT = P.alloc([128, 8, 512], BF16)
    PmT = P.alloc([128, 4, 2, 512], BF16); om = P.alloc([128, 4, 1024], BF16); omT = P.alloc([128, 8, 512], BF16)
    x2T = P.alloc([128, 8, 128], F32); xn3 = P.alloc([128, 4, 1024], BF16)
    st2_ = [P.alloc([128, 16], F32) for _ in range(2)]; rtmp = P.alloc([128, 16], F32); lg = P.alloc([128, 16], F32); ex = P.alloc([128, 16], F32)
    junk2 = P.alloc([128, 1024], BF16)
    ymT_v = ymT_d.ap().rearrange("(k p) t -> p k t", p=128)
    yaT_v = yaT_d.ap().rearrange("h p t -> (h p) t").rearrange("(k p) t -> p k t", p=128)
    YD = [("ymT_d", h_) for h_ in range(4)]
    def stageA(s):
        pb = s % 2
        xs2, ymTs, yaTs, st2 = xs2_[pb], ymTs_[pb], yaTs_[pb], st2_[pb]
        sl = slice(s * 512, (s + 1) * 512)
        P.dma(xs2, x[s * 512:(s + 1) * 512, :].rearrange("(j p) d -> p j d", p=128), writes=[("xs2", pb)])
        P.dma(ymTs, ymT_v[:, :, sl], reads=YD, writes=[("ymTs", pb)])
        P.dma(yaTs, yaT_v[:, :, sl], reads=[("yaT_d", h_, s) for h_ in range(8)], writes=[("yaTs", pb)])
        for j in range(4):
            tl = slice(j * 128, (j + 1) * 128)
            for hf in range(2):
                fl = slice(hf * 512, (hf + 1) * 512)
                bk = (2 * j + hf) % 4
                for blk in range(4):
                    mm(B[bk][:, :], ymTs[:, blk, tl], woutm[:, blk, fl], blk == 0, False, [("ymTs", pb), "woutm"], [BK[bk]])
                for blk in range(4):
                    mm(B[bk][:, :], yaTs[:, blk, tl], wouta[:, blk, fl], False, blk == 3, [("yaTs", pb), "wouta"], [BK[bk]])
                tt("dve", xs2[:, j, fl], xs2[:, j, fl], B[bk][:, :], ALU.add, [("xs2", pb), BK[bk]], [("xs2", pb)])
        for j in range(4):
            actf(junk2, xs2[:, j, :], AF.Square, [("xs2", pb)], ["junk2", ("st2", pb, j)], accum=st2[:, j:j + 1])
            actf(st2[:, j:j + 1], st2[:, j:j + 1], AF.Sqrt, [("st2", pb, j)], [("st2", pb, j)], bias=EPS, scale=1.0 / 1024)
            recip(st2[:, j:j + 1], st2[:, j:j + 1], [("st2", pb, j)], [("st2", pb, j)])
            ts("dve", xn2[:, j, :], xs2[:, j, :], st2[:, j:j + 1], None, ALU.mult, None, [("xs2", pb), ("st2", pb, j)], ["xn2"])
        for kc in range(8):
            bk = 6 + kc % 2
            for j in range(4):
                tr(Bb[bk][:, j * 128:(j + 1) * 128], xn2[:, j, kc * 128:(kc + 1) * 128], identb, ["xn2", "identb"], [BK[bk]])
            if kc % 2:
                ts("dve", xn2T[:, kc, :], Bb[bk][:, 0:512], gmx[:, kc:kc + 1], None, ALU.mult, None, [BK[bk], "gmx"], ["xn2T"])
            else:
                actf(xn2T[:, kc, :], Bb[bk][:, 0:512], AF.Copy, [BK[bk], "gmx"], ["xn2T"], scale=gmx[:, kc:kc + 1])
        for blk in range(8):
            bk = blk % 2
            for kc in range(8):
                mm(B[bk][:, :], wmq[:, kc, blk * 128:(blk + 1) * 128], xn2T[:, kc, :], kc == 0, kc == 7, ["wmq", "xn2T"], [BK[bk]])
            if blk % 2:
                cp("dve", qmT[:, blk, :], B[bk][:, :], [BK[bk]], ["qmT"])
            else:
                actf(qmT[:, blk, :], B[bk][:, :], AF.Copy, [BK[bk]], ["qmT"])
    def stageB(s):
        pb = s % 2
        xs2, ymTs, yaTs, st2 = xs2_[pb], ymTs_[pb], yaTs_[pb], st2_[pb]
        for h_ in range(4):
            for mc in range(2):
                bk = 2 + (2 * h_ + mc) % 2
                ml = slice(mc * 128, (mc + 1) * 128)
                mm(B[bk][:, :], memKT[:, 2 * h_, ml], qmT[:, 2 * h_, :], True, False, ["memKT", "qmT"], [BK[bk]])
                mm(B[bk][:, :], memKT[:, 2 * h_ + 1, ml], qmT[:, 2 * h_ + 1, :], False, True, ["memKT", "qmT"], [BK[bk]])
                actf(PmT[:, h_, mc, :], B[bk][:, :], AF.Exp, [BK[bk]], ["PmT"], scale=1.0 / 16)
        for j in range(4):
            tl = slice(j * 128, (j + 1) * 128)
            for h_ in range(4):
                i_ = 4 * j + h_
                bk = 4 + i_ % 2
                mm(B[bk][:, 0:257], PmT[:, h_, 0, tl], memV[:, 0, h_, :], True, False, ["PmT", "memV"], [BK[bk]])
                mm(B[bk][:, 0:257], PmT[:, h_, 1, tl], memV[:, 1, h_, :], False, True, ["PmT", "memV"], [BK[bk]])
                recip(rtmp[:, i_:i_ + 1], B[bk][:, 256:257], [BK[bk]], [("rtmp", i_)])
                ts("dve", om[:, j, h_ * 256:(h_ + 1) * 256], B[bk][:, 0:256], rtmp[:, i_:i_ + 1], None, ALU.mult, None, [BK[bk], ("rtmp", i_)], ["om"])
        for kc in range(8):
            bk = 6 + kc % 2
            for j in range(4):
                tr(Bb[bk][:, j * 128:(j + 1) * 128], om[:, j, kc * 128:(kc + 1) * 128], identb, ["om", "identb"], [BK[bk]])
            if kc % 2:
                cp("dve", omT[:, kc, :], Bb[bk][:, 0:512], [BK[bk]], ["omT"])
            else:
                actf(omT[:, kc, :], Bb[bk][:, 0:512], AF.Copy, [BK[bk]], ["omT"])
        for j in range(4):
            tl = slice(j * 128, (j + 1) * 128)
            for hf in range(2):
                fl = slice(hf * 512, (hf + 1) * 512)
                bk = (2 * j + hf) % 4
                for kc in range(8):
                    mm(B[bk][:, :], omT[:, kc, tl], wmo[:, kc, fl], kc == 0, kc == 7, ["omT", "wmo"], [BK[bk]])
                tt("dve", xs2[:, j, fl], xs2[:, j, fl], B[bk][:, :], ALU.add, [("xs2", pb), BK[bk]], [("xs2", pb)])
    def stageC(s):
        pb = s % 2
        xs2, ymTs, yaTs, st2 = xs2_[pb], ymTs_[pb], yaTs_[pb], st2_[pb]
        P.dma(acc_d[s * 512:(s + 1) * 512, :].rearrange("(j p) d -> p j d", p=128), xs2, reads=[("xs2", pb)], writes=["acc_d"])
        for j in range(4):
            t = s * 4 + j
            q_ = 4 + j
            actf(junk2, xs2[:, j, :], AF.Square, [("xs2", pb)], ["junk2", ("st2", pb, q_)], accum=st2[:, q_:q_ + 1])
            actf(st2[:, q_:q_ + 1], st2[:, q_:q_ + 1], AF.Sqrt, [("st2", pb, q_)], [("st2", pb, q_)], bias=EPS, scale=1.0 / 1024)
            recip(st2[:, q_:q_ + 1], st2[:, q_:q_ + 1], [("st2", pb, q_)], [("st2", pb, q_)])
            stt("dve", xn3[:, j, :], xs2[:, j, :], st2[:, q_:q_ + 1], gffn_b, ALU.mult, ALU.mult, [("xs2", pb), ("st2", pb, q_), "gffn_b"], ["xn3"])
            for kc in range(8):
                bk = kc % 2
                tr(B[bk][:, 0:128], xs2[:, j, kc * 128:(kc + 1) * 128], identf, [("xs2", pb), "identf"], [BK[bk]])
                if kc % 2:
                    ts("dve", x2T[:, kc, :], B[bk][:, 0:128], gffn_p[:, kc:kc + 1], None, ALU.mult, None, [BK[bk], "gffn_p"], [("x2T", kc)])
                else:
                    actf(x2T[:, kc, :], B[bk][:, 0:128], AF.Copy, [BK[bk], "gffn_p"], [("x2T", kc)], scale=gffn_p[:, kc:kc + 1])
            for kc in range(8):
                mm(B[2][:, 0:16], x2T[:, kc, :], wr[:, kc, :], kc == 0, kc == 7, [("x2T", kc), "wr"], [BK[2]])
            ts("dve", lg, B[2][:, 0:16], st2[:, q_:q_ + 1], None, ALU.mult, None, [BK[2], ("st2", pb, q_)], ["lg"])
            P.dve(lambda e: e.tensor_reduce(out=ex[:, 0:1], in_=lg, axis=AX.X, op=ALU.max), ["lg"], ["exm"])
            ts("dve", ex[:, 0:1], ex[:, 0:1], -1.0, None, ALU.mult, None, ["exm"], ["exm"])
            actf(lg, lg, AF.Exp, ["lg", "exm"], ["lg", "exs"], bias=ex[:, 0:1], accum=ex[:, 1:2])
            recip(ex[:, 1:2], ex[:, 1:2], ["exs"], ["exs"])
            ts("dve", AFF[:, t, :], lg, ex[:, 1:2], None, ALU.mult, None, ["lg", "exs"], ["AFF"])
        P.dma(h_d[s * 512:(s + 1) * 512, :].rearrange("(j p) d -> p j d", p=128), xn3, reads=["xn3"], writes=["h_d"])
    stageA(0)
    for s in range(8):
        stageB(s)
        if s + 1 < 8:
            stageA(s + 1)
        stageC(s)
    tap("AFF", AFF, [128, 32, 16], F32, ["AFF"])
    P.release()
    if stop <= 4:
        return finish(nc, P, out, tapd, extra_out=[("acc_d", acc_d, [4096, 1024], F32)])

    P.phase = "p3"
    P.mark()
    iota = P.alloc([128, 512], F32); load(iota, c_iota.ap(), "iota")
    posm = P.alloc([128, 32, 16], F32)
    RH = P.alloc([128, 32, 16, 4], BF16)
    P.mark()
    onesf = P.alloc([128, 128], F32); load(onesf, c_onesf.ap(), "onesf")
    Uf = P.alloc([128, 128], F32); load(Uf, c_U.ap(), "Uf")
    jp = P.alloc([128, 32, 2], F32); load(jp, c_jp.ap(), "jp")
    lo = P.alloc([128, 16], F32); mid = P.alloc([128, 16], F32); tot = P.alloc([128, 16], F32); tq = P.alloc([128, 16], F32)
    sel = P.alloc([128, 32, 16], F32); cum = P.alloc([128, 32, 16], F32)
    ones32 = P.alloc([128, 32], F32); memset("dve", ones32, 1.0, ["ones32"])
    afh = P.alloc([128, 32, 16], BF16); afl = P.alloc([128, 32, 16], F32)
    memset("dve", lo, 0.0, ["lo"])
    mid3 = mid.rearrange("p (o e) -> p o e", o=1).to_broadcast([128, 32, 16])
    lo3 = lo.rearrange("p (o e) -> p o e", o=1).to_broadcast([128, 32, 16])
    self2 = sel.rearrange("p j e -> p (j e)")
    for it in range(20):
        step = 0.5 ** (it + 1)
        ts("dve", mid, lo, step, None, ALU.add, None, ["lo"], ["mid"])
        tt("dve", sel, AFF, mid3, ALU.is_ge, ["AFF", "mid"], ["sel"])
        mm(B[0][:, :], onesf, self2, True, True, ["onesf", "sel"], [BK[0]])
        P.dve(lambda e: e.tensor_reduce(out=tot, in_=B[0][:, :].rearrange("p (j e) -> p e j", e=16), axis=AX.X, op=ALU.add), [BK[0]], ["tot"])
        stt("dve", tq, tot, 512.0, mid, ALU.is_ge, ALU.mult, ["tot", "mid"], ["tq"])
        tt("dve", lo, lo, tq, ALU.max, ["lo", "tq"], ["lo"])
    tt("dve", sel, AFF, lo3, ALU.is_ge, ["AFF", "lo"], ["sel"])
    for e_ in range(16):
        P.dve(lambda e, e_=e_: e.tensor_tensor_scan(out=cum[:, :, e_], data0=ones32, data1=sel[:, :, e_], initial=0.0, op0=ALU.mult, op1=ALU.add), ["ones32", "sel"], ["cum"])
    tt("dve", cum, cum, sel, ALU.subtract, ["cum", "sel"], ["cum"])
    mm(B[1][:, :], onesf, cum.rearrange("p j e -> p (j e)"), True, False, ["onesf", "cum"], [BK[1]])
    mm(B[1][:, :], Uf, self2, False, True, ["Uf", "sel"], [BK[1]])
    stt("dve", posm.rearrange("p j e -> p (j e)"), B[1][:, :], 1.0, self2, ALU.add, ALU.mult, [BK[1], "sel"], ["posm"])
    ts("dve", posm, posm, -1.0, None, ALU.add, None, ["posm"], ["posm"])
    cp("dve", afh, AFF, ["AFF"], ["afh"])
    tt("dve", afl, AFF, afh, ALU.subtract, ["AFF", "afh"], ["afl"])
    cp("dve", RH[:, :, :, 2], afh, ["afh"], ["RH"])
    cp("dve", RH[:, :, :, 3], afl, ["afl", "RH"], ["RH"])
    for e_ in range(16):
        cp("dve", RH[:, :, e_, 0:2], jp, ["jp", "RH"], ["RH"])
    P.release()
    P.phase = "p3_alloc"
    OH = P.alloc([128, 32, 512], BF16)
    res = P.alloc([128, 16], F32); idxf = [P.alloc([128, 4], F32) for _ in range(2)]; idxi = [P.alloc([128, 4], I32) for _ in range(2)]
    gate_ = [P.alloc([128, 4], F32) for _ in range(2)]
    xe = [P.alloc([128, 4, 1024], BF16) for _ in range(2)]
    xeT = P.alloc([128, 8, 512], BF16); hidT = P.alloc([128, 16, 512], BF16)
    sg = [P.alloc([128, 512], F32) for _ in range(2)]
    yacc = [P.alloc([128, 1024], F32) for _ in range(4)]
    NGU, ND = 4, 4
    gub = [P.alloc([128, 2, 8, 512], BF16) for _ in range(NGU)]
    dbf = [P.alloc([128, 4, 1024], BF16) for _ in range(ND)]

    def route_oh(e_, j0, j1):
        ph = P.phase; P.phase = "p3_route"
        for j in range(j0, j1):
            ts("dve", OH[:, j, :], iota, posm[:, j, e_:e_ + 1], None, ALU.is_equal, None, ["iota", "posm"], [("OH", j)])
        P.phase = ph

    def route_rest(e_):
        ph = P.phase; P.phase = "p3_route"
        p_ = e_ % 2
        for sc in range(4):
            for j in range(32):
                mm(B[0][:, sc * 4:(sc + 1) * 4], OH[:, j, sc * 128:(sc + 1) * 128], RH[:, j, e_, :], j == 0, j == 31, [("OH", j), "RH"], [BK[0]])
        cp("dve", res, B[0][:, 0:16], [BK[0]], ["res"])
        r3 = res.rearrange("p (s c) -> p s c", c=4)
        stt("dve", idxf[p_], r3[:, :, 0], 128.0, r3[:, :, 1], ALU.mult, ALU.add, ["res"], [("idxf", p_)])
        tt("dve", gate_[p_], r3[:, :, 2], r3[:, :, 3], ALU.add, ["res"], [("gate", p_)])
        cp("dve", idxi[p_], idxf[p_], [("idxf", p_)], [("idxi", p_)])
        for sc in range(4):
            P.op("pool", lambda e, sc=sc, p_=p_: e.indirect_dma_start(out=xe[p_][:, sc, :], out_offset=None, in_=h_d[:, :],
                                                                in_offset=bass.IndirectOffsetOnAxis(ap=idxi[p_][:, sc:sc + 1], axis=0)),
                 reads=[("idxi", p_), "h_d"], writes=[("xe", p_, sc)], dma=True)
        P.phase = ph

    def wload_gu(e_, q4):
        ph = P.phase; P.phase = "p3_wload"
        g_ = gub[q4 % NGU]
        P.dma(g_[:, 0, :, :], weg_d[e_, :, q4 * 512:(q4 + 1) * 512].rearrange("(k p) n -> p k n", p=128), writes=[("gub", q4 % NGU, 0)], q="pool")
        P.dma(g_[:, 1, :, :], weu_d[e_, :, q4 * 512:(q4 + 1) * 512].rearrange("(k p) n -> p k n", p=128), writes=[("gub", q4 % NGU, 1)], q="pool")
        P.phase = ph

    def wload_d(e_, q4):
        ph = P.phase; P.phase = "p3_wload"
        P.dma(dbf[q4 % ND], wed_d[e_, q4 * 512:(q4 + 1) * 512, :].rearrange("(k p) n -> p k n", p=128), writes=[("dbf", q4 % ND)], q="pool")
        P.phase = ph

    def wloads(e_):
        for q4 in range(4):
            wload_gu(e_, q4)
        for q4 in range(4):
            wload_d(e_, q4)

    def compute(e_):
        P.phase = "p3_xT"
        p_ = e_ % 2
        for kc in range(8):
            for sc in range(4):
                tr(Bb[7][:, sc * 128:(sc + 1) * 128], xe[p_][:, sc, kc * 128:(kc + 1) * 128], identb, [("xe", p_, sc), "identb"], [BK[7]])
            if kc % 2:
                cp("dve", xeT[:, kc, :], Bb[7][:, 0:512], [BK[7]], ["xeT"])
            else:
                actf(xeT[:, kc, :], Bb[7][:, 0:512], AF.Copy, [BK[7]], ["xeT"])
        P.phase = "p3_gu"
        for fc in range(16):
            q4, f4 = fc // 4, fc % 4
            g_ = gub[q4 % NGU]
            bg, bu = 1 + (fc % 2) * 2, 2 + (fc % 2) * 2
            for kc in range(8):
                mm(B[bg][:, :], g_[:, 0, kc, f4 * 128:(f4 + 1) * 128], xeT[:, kc, :], kc == 0, kc == 7, [("gub", q4 % NGU, 0), "xeT"], [BK[bg]])
            for kc in range(8):
                mm(B[bu][:, :], g_[:, 1, kc, f4 * 128:(f4 + 1) * 128], xeT[:, kc, :], kc == 0, kc == 7, [("gub", q4 % NGU, 1), "xeT"], [BK[bu]])
            actf(sg[fc % 2], B[bg][:, :], AF.Silu, [BK[bg]], [("sg", fc % 2)])
            tt("dve", hidT[:, fc, :], sg[fc % 2], B[bu][:, :], ALU.mult, [("sg", fc % 2), BK[bu]], [("hidT", fc)])
            if e_ + 1 < 16:
                route_oh(e_ + 1, 2 * fc, 2 * fc + 2)
            if f4 == 3 and e_ + 1 < 16:
                wload_gu(e_ + 1, q4)
        if e_ + 1 < 16:
            route_rest(e_ + 1)
        P.phase = "p3_down"
        gi = 0
        for q4 in range(4):
            for st_ in range(4):
                for hf in range(2):
                    bk = 5 + gi % 2
                    gi += 1
                    fl = slice(hf * 512, (hf + 1) * 512)
                    for f4 in range(4):
                        fc = q4 * 4 + f4
                        mm(B[bk][:, :], hidT[:, fc, st_ * 128:(st_ + 1) * 128], dbf[q4 % ND][:, f4, fl], f4 == 0, f4 == 3,
                           [("hidT", fc), ("dbf", q4 % ND)], [BK[bk]])
                    yk = ("ya", st_, hf)
                    if q4 == 0:
                        actf(yacc[st_][:, fl], B[bk][:, :], AF.Copy, [BK[bk], ("gate", p_)], [yk], scale=gate_[p_][:, st_:st_ + 1])
                    else:
                        stt("dve", yacc[st_][:, fl], B[bk][:, :], gate_[p_][:, st_:st_ + 1], yacc[st_][:, fl], ALU.mult, ALU.add,
                            [BK[bk], ("gate", p_), yk], [yk])
            if e_ + 1 < 16:
                wload_d(e_ + 1, q4)
        for st_ in range(4):
            P.op("pool", lambda e, st_=st_, p_=p_: e.indirect_dma_start(out=acc_d[:, :], out_offset=bass.IndirectOffsetOnAxis(ap=idxi[p_][:, st_:st_ + 1], axis=0),
                                                                in_=yacc[st_], in_offset=None, compute_op=ALU.add),
                 reads=[("idxi", p_), ("ya", st_, 0), ("ya", st_, 1), "acc_d"], writes=["acc_d"], dma=True)

    route_oh(0, 0, 32)
    route_rest(0)
    wloads(0)
    for e_ in range(16):
        compute(e_)
    P.release()
    if stop <= 5:
        return finish(nc, P, out, tapd, extra_out=[("acc_d", acc_d, [4096, 1024], F32)])

    P.phase = "p4"
    gfin_b = P.alloc([128, 1024], F32); load(gfin_b, gfin_d.ap().to_broadcast([128, 1024]), "gfin_b")
    xf = [P.alloc([128, 4, 1024], F32) for _ in range(2)]
    of = [P.alloc([128, 4, 1024], F32) for _ in range(2)]
    fs = P.alloc([128, 32], F32); junk3 = P.alloc([128, 1024], BF16)
    for s in range(8):
        b = s % 2
        P.dma(xf[b], acc_d[s * 512:(s + 1) * 512, :].rearrange("(j p) d -> p j d", p=128), reads=["acc_d"], writes=[("xf", b)])
        for j in range(4):
            t = s * 4 + j
            actf(junk3, xf[b][:, j, :], AF.Square, [("xf", b)], ["junk3", ("fs", t)], accum=fs[:, t:t + 1])
            actf(fs[:, t:t + 1], fs[:, t:t + 1], AF.Sqrt, [("fs", t)], [("fs", t)], bias=EPS, scale=1.0 / 1024)
            recip(fs[:, t:t + 1], fs[:, t:t + 1], [("fs", t)], [("fs", t)])
            stt("dve", of[b][:, j, :], xf[b][:, j, :], fs[:, t:t + 1], gfin_b, ALU.mult, ALU.mult, [("xf", b), ("fs", t), "gfin_b"], [("of", b)])
        P.dma(out[s * 512:(s + 1) * 512, :].rearrange("(j p) d -> p j d", p=128), of[b], reads=[("of", b)], writes=[("out", s)])
    return finish(nc, P, out, tapd)


def finish(nc, P, out, tapd, extra_out=()):
    for name, src, shp, dt in extra_out:
        t = nc.dram_tensor("tap_" + name, list(shp), dt, kind="ExternalOutput")
        tapd[name] = t
        P.barrier()
        P.dma(t.ap(), src.ap(), writes=[("tap", name)])
    P.barrier()
    P.emit()
    return nc, P, tapd


def _bf(a):
    return np.ascontiguousarray(a).astype(ml_dtypes.bfloat16)


def _pk(v, k):
    return np.ascontiguousarray(np.asarray(v, np.float32).reshape(k, 128).T)


def const_inputs():
    c = {}
    c["c_identb"] = _bf(np.eye(128, dtype=np.float32))
    c["c_identf"] = np.eye(128, dtype=np.float32)
    c["c_onesb"] = _bf(np.ones((128, 128), np.float32))
    c["c_onesf"] = np.ones((128, 128), np.float32)
    s = np.arange(128)[:, None]; t = np.arange(128)[None, :]
    same = (s // 64) == (t // 64)
    bm = np.zeros((128, 2, 128), np.float32)
    bm[:, 0, :] = (same & (s <= t)); bm[:, 1, :] = (same & (s >= t))
    bm *= np.float32(128 ** -0.5)
    c["c_bmask"] = bm
    sel = np.zeros((36, 8, 128), np.float32)
    for l, p in enumerate(LANE_PART):
        sel[p, l, :] = 1.0
    c["c_sel36"] = sel
    ns = np.zeros((128, 64), np.float32); ns[0:64, :] = 1.0 / 64; ns[64, :] = EPS
    c["c_nsel"] = ns
    invf = np.zeros((128, 1), np.float32)
    f = (10000.0 ** (-np.arange(0, 32, 2, dtype=np.float32) / 32)).astype(np.float32)
    invf[64:80, 0] = f; invf[80:96, 0] = f
    c["c_invf"] = invf
    r = np.ones((36, 4096), np.float32); r[:, ::64] = 0.0
    c["c_reset"] = r
    c["c_iota"] = np.tile(np.arange(512, dtype=np.float32)[None, :], (128, 1))
    c["c_tokid"] = (np.arange(32)[None, :] * 128 + np.arange(128)[:, None]).astype(np.float32)
    c["c_U"] = (s < t).astype(np.float32)
    jp = np.zeros((128, 32, 2), np.float32); jp[:, :, 0] = np.arange(32)[None, :]; jp[:, :, 1] = np.arange(128)[:, None]
    c["c_jp"] = jp
    return c


def weight_inputs(I):
    g = lambda k: np.asarray(I[k], np.float32)
    w = {}
    w_in = g("w_in")[0]
    w["gmix"] = _pk(g("g_mix")[0], 8)
    w["w_q"] = np.ascontiguousarray(w_in[:, 0:512]); w["w_k"] = np.ascontiguousarray(w_in[:, 512:1024])
    w["w_v"] = np.ascontiguousarray(w_in[:, 1024:1536]); w["w_o"] = np.ascontiguousarray(w_in[:, 1536:2048])
    gt = w_in[:, 2048:2064]; bg = g("b_gates")[0]
    wI = np.zeros((1024, 36), np.float32); wF = np.zeros((1024, 36), np.float32)
    bI = np.zeros((36, 1), np.float32); bF = np.zeros((36, 1), np.float32)
    wI[:, 0:4] = gt[:, 0:4]; wI[:, 32:36] = gt[:, 8:12]; wF[:, 0:4] = gt[:, 4:8]; wF[:, 32:36] = gt[:, 12:16]
    bI[0:4, 0] = bg[0:4]; bI[32:36, 0] = bg[8:12]; bF[0:4, 0] = bg[4:8]; bF[32:36, 0] = bg[12:16]
    w["w_gI"], w["w_gF"], w["b_I"], w["b_F"] = wI, wF, bI, bF
    w["w_cq"] = np.ascontiguousarray(w_in[:, 2064:2320]); w["w_ckv"] = np.ascontiguousarray(w_in[:, 2320:2448])
    kr = w_in[:, 2448:2480]
    wkr = np.zeros((1024, 96), np.float32); wkr[:, 64:96] = kr
    wkrr = np.zeros((1024, 96), np.float32); wkrr[:, 64:80] = kr[:, 16:32]; wkrr[:, 80:96] = kr[:, 0:16]
    w["w_kr"], w["w_krr"] = wkr, wkrr
    cv = g("conv_qk")[0]
    w["convq"] = np.ascontiguousarray(cv[:, 0:512].reshape(3, 4, 128).transpose(2, 1, 0))
    w["convk"] = np.ascontiguousarray(cv[:, 512:1024].reshape(3, 4, 128).transpose(2, 1, 0))
    w["g_qa"] = _pk(g("g_q_a")[0], 2); w["g_kva"] = _pk(g("g_kv_a")[0], 1)
    wqb = g("w_q_b")[0]
    w["w_qb"] = wqb
    wqbr = np.zeros_like(wqb).reshape(256, 8, 96); q3 = wqb.reshape(256, 8, 96)
    wqbr[:, :, 64:80] = q3[:, :, 80:96]; wqbr[:, :, 80:96] = q3[:, :, 64:80]
    w["w_qbr"] = np.ascontiguousarray(wqbr.reshape(256, 768))
    kv3 = g("w_kv_b")[0].reshape(128, 8, 128)
    w["w_kn"] = np.ascontiguousarray(kv3[:, :, 0:64].reshape(128, 512)); w["w_v2"] = np.ascontiguousarray(kv3[:, :, 64:128].reshape(128, 512))
    w["g_hm"] = _pk(g("g_head_mlstm")[0], 4)
    w["g_ha"] = np.ascontiguousarray(g("g_head_mla")[0].reshape(8, 64).T)
    wout = g("w_out")[0]
    w["w_outm"] = np.ascontiguousarray(wout[0:512]); w["w_outa"] = np.ascontiguousarray(wout[512:1024].reshape(8, 64, 1024))
    w["g_mx"] = _pk(g("g_mem_x")[0], 8); w["g_mkv"] = _pk(g("g_mem_kv")[0], 8)
    w["w_mq"], w["w_mk"], w["w_mv"], w["w_mo"] = g("w_mem_q")[0], g("w_mem_k")[0], g("w_mem_v")[0], g("w_mem_o")[0]
    w["g_ffnp"] = _pk(g("g_ffn")[0], 8); w["g_ffn"] = g("g_ffn")[0].reshape(1, 1024); w["g_fin"] = g("g_final").reshape(1, 1024)
    w["w_r"] = g("w_router")[0]
    w["w_eg"], w["w_eu"], w["w_ed"] = g("w_exp_gate")[0], g("w_exp_up")[0], g("w_exp_down")[0]
    return w


def core_inputs(I, b, shared):
    m = dict(shared)
    m["x"] = np.ascontiguousarray(np.asarray(I["x"], np.float32)[b])
    m["mem"] = np.ascontiguousarray(np.asarray(I["mem"], np.float32)[b])
    m["pos"] = np.ascontiguousarray(np.tile(np.asarray(I["positions"], np.int32)[b][None, :], (32, 1)))
    return m


_CACHE = {}


def kernel(**inputs):
    if "nc" not in _CACHE:
        _CACHE["nc"] = build()[0]
    nc = _CACHE["nc"]
    shared = const_inputs()
    shared.update(weight_inputs(inputs))
    in_maps = [core_inputs(inputs, c % 4, shared) for c in range(8)]
    res = run_bass_kernel_spmd(nc, in_maps, core_ids=list(range(8)))
    return np.stack([np.asarray(res.results[b]["out"], np.float32) for b in range(4)], axis=0)
```
